# Optimizing a Trainium2 kernel written in Bass

```python
import math
import jax, jax.numpy as jnp
from jax import lax
import numpy as np

D_MODEL = 4096
BATCH = 1
SEQ = 16384
DEPTH = 2

F32 = jnp.float32
EPS = 1e-6
N_MOD = 6
D_MIX = D_MODEL

A_HEADS = 8
A_HEAD_DIM = 128
A_WIDTH = A_HEADS * A_HEAD_DIM
CONV_W = 4
LRU_C = 8.0

B_HEADS = 12
B_HEAD_DIM = 128
B_WIDTH = B_HEADS * B_HEAD_DIM
DN_CHUNK = 64

C_HEADS = 12
C_NOPE = 128
C_ROPE = 64
C_VDIM = 128
C_WIDTH = C_HEADS * C_VDIM
Q_LORA = 1024
KV_LORA = 512
ROPE_THETA = 10000.0
ATTN_BLOCK = 128

PEER_HEADS = 8
PEER_KEYS = 128
PEER_N = PEER_KEYS * PEER_KEYS
PEER_QDIM = 256
PEER_HALF = PEER_QDIM // 2
PEER_TOPK = 16
PEER_CHUNK = 64

IN_SPLITS = (A_WIDTH, A_WIDTH,
             B_WIDTH, B_WIDTH, B_WIDTH, B_WIDTH,
             B_HEADS, B_HEADS,
             Q_LORA, KV_LORA, C_ROPE)
D_IN = 2 * A_WIDTH + 4 * B_WIDTH + 2 * B_HEADS + Q_LORA + KV_LORA + C_ROPE

kernel_name = "hybrid_rglru_gdn_mla_peer_adaln"


def rmsnorm(x, w):
    xf = x.astype(F32)
    y = xf * lax.rsqrt(jnp.mean(xf * xf, axis=-1, keepdims=True) + EPS)
    return (y * w.astype(F32)).astype(x.dtype)


def l2norm(x):
    return x * lax.rsqrt(jnp.sum(x * x, axis=-1, keepdims=True) + EPS)


def split_columns(p):
    outs, start = [], 0
    for width in IN_SPLITS:
        outs.append(p[..., start:start + width])
        start += width
    return outs


def causal_dwconv(x, w, b=None):
    ch = x.shape[-1]
    y = lax.conv_general_dilated(x, w[:, None, :].astype(x.dtype), window_strides=(1,),
                                 padding=[(w.shape[0] - 1, 0)],
                                 dimension_numbers=('NWC', 'WIO', 'NWC'),
                                 feature_group_count=ch)
    return y if b is None else y + b


def rglru_group(x_rec, x_gate, conv_w, conv_b, w_a, b_a, w_x, b_x, lam):
    bsz, s, _ = x_rec.shape
    u = causal_dwconv(x_rec, conv_w, conv_b)
    uh = u.reshape(bsz, s, A_HEADS, A_HEAD_DIM)
    r = jax.nn.sigmoid(jnp.einsum('bshi,hij->bshj', uh, w_a).reshape(bsz, s, A_WIDTH) + b_a)
    i = jax.nn.sigmoid(jnp.einsum('bshi,hij->bshj', uh, w_x).reshape(bsz, s, A_WIDTH) + b_x)
    log_a = -LRU_C * r.astype(F32) * jax.nn.softplus(-lam.astype(F32))
    a = jnp.exp(log_a)
    bterm = jnp.sqrt(jnp.maximum(-jnp.expm1(2.0 * log_a), 0.0)) * (i * u).astype(F32)

    def combine(lhs, rhs):
        a1, b1 = lhs
        a2, b2 = rhs
        return a1 * a2, a2 * b1 + b2

    _, h = lax.associative_scan(combine, (a, bterm), axis=1)
    return jax.nn.gelu(x_gate) * h.astype(x_rec.dtype)


def chunk_gated_delta_rule(q, k, v, g, beta):
    bsz, s, h, d = q.shape
    c = DN_CHUNK
    n = s // c

    def chunks(t):
        return t.reshape(bsz, n, c, h, -1).transpose(0, 1, 3, 2, 4)

    q, k, v = chunks(q), chunks(k), chunks(v)
    g = g.reshape(bsz, n, c, h).transpose(0, 1, 3, 2)
    beta = beta.reshape(bsz, n, c, h).transpose(0, 1, 3, 2)
    gc = jnp.cumsum(g, axis=-1)
    lower = jnp.tril(jnp.ones((c, c), bool))
    lower_strict = jnp.tril(jnp.ones((c, c), bool), -1)
    diff = gc[..., :, None] - gc[..., None, :]
    decay = jnp.where(lower, jnp.exp(jnp.where(lower, diff, 0.0)), 0.0)
    kb = k * beta[..., None]
    lmat = jnp.where(lower_strict, jnp.einsum('bnhid,bnhjd->bnhij', kb, k) * decay, 0.0)
    amat = lmat + jnp.eye(c, dtype=F32)
    rhs = jnp.concatenate([v * beta[..., None], kb * jnp.exp(gc)[..., None]], axis=-1)
    sol = lax.linalg.triangular_solve(amat, rhs, left_side=True, lower=True, unit_diagonal=True)
    u, w = sol[..., :d], sol[..., d:]
    qk = jnp.where(lower, jnp.einsum('bnhid,bnhjd->bnhij', q, k) * decay, 0.0)
    q_dec = q * jnp.exp(gc)[..., None]
    k_tail = k * jnp.exp(gc[..., -1:] - gc)[..., None]
    g_last = jnp.exp(gc[..., -1])

    def step(state, inp):
        q_d, qk_i, u_i, w_i, k_t, gl = inp
        v_new = u_i - jnp.einsum('bhcd,bhde->bhce', w_i, state)
        o = jnp.einsum('bhcd,bhde->bhce', q_d, state) + jnp.einsum('bhij,bhje->bhie', qk_i, v_new)
        state = state * gl[..., None, None] + jnp.einsum('bhcd,bhce->bhde', k_t, v_new)
        return state, o

    xs = tuple(t.swapaxes(0, 1) for t in (q_dec, qk, u, w, k_tail, g_last))
    s0 = jnp.zeros((bsz, h, d, d), F32)
    _, o = lax.scan(step, s0, xs)
    return o.transpose(1, 0, 3, 2, 4).reshape(bsz, s, h, d)


def gated_deltanet_group(q, k, v, z, beta_raw, alpha_raw, conv_w, a_log, dt_bias, norm_w):
    bsz, s, _ = q.shape
    qkv = jax.nn.silu(causal_dwconv(jnp.concatenate([q, k, v], axis=-1), conv_w))
    q, k, v = jnp.split(qkv, 3, axis=-1)

    def heads(t):
        return t.reshape(bsz, s, B_HEADS, B_HEAD_DIM).astype(F32)

    qh = l2norm(heads(q)) * (B_HEAD_DIM ** -0.5)
    kh = l2norm(heads(k))
    vh = heads(v)
    beta = jax.nn.sigmoid(beta_raw.astype(F32))
    g = -jnp.exp(a_log.astype(F32)) * jax.nn.softplus(alpha_raw.astype(F32) + dt_bias.astype(F32))
    o = chunk_gated_delta_rule(qh, kh, vh, g, beta)
    o = rmsnorm(o, norm_w) * jax.nn.silu(heads(z))
    return o.reshape(bsz, s, B_WIDTH).astype(q.dtype)


def rope_cos_sin(positions):
    inv = ROPE_THETA ** (-jnp.arange(0, C_ROPE, 2, dtype=F32) / C_ROPE)
    ang = positions.astype(F32)[..., None] * inv
    return jnp.cos(ang), jnp.sin(ang)


def apply_rope(x, cos, sin):
    half = x.shape[-1] // 2
    xf = x.astype(F32)
    x1, x2 = xf[..., :half], xf[..., half:]
    return jnp.concatenate([x1 * cos - x2 * sin, x1 * sin + x2 * cos], axis=-1).astype(x.dtype)


def causal_block_attention(q, k, v, scale):
    bsz, s, h, dq = q.shape
    nb = s // ATTN_BLOCK
    qb = q.reshape(bsz, nb, ATTN_BLOCK, h, dq).transpose(1, 0, 2, 3, 4)
    kpos = jnp.arange(s)
    neg = jnp.finfo(F32).min

    def one(args):
        q_blk, bi = args
        sc = jnp.einsum('bqhd,bkhd->bhqk', q_blk, k).astype(F32) * scale
        qpos = bi * ATTN_BLOCK + jnp.arange(ATTN_BLOCK)
        sc = jnp.where(kpos[None, :] <= qpos[:, None], sc, neg)
        p = jax.nn.softmax(sc, axis=-1).astype(v.dtype)
        return jnp.einsum('bhqk,bkhd->bqhd', p, v)

    o = lax.map(one, (qb, jnp.arange(nb)))
    return o.transpose(1, 0, 2, 3, 4).reshape(bsz, s, h, -1)


def mla_group(c_q, c_kv, k_rope, positions, q_norm_w, w_q_up, kv_norm_w, w_kv_up):
    bsz, s, _ = c_q.shape
    q = jnp.einsum('bsr,rhd->bshd', rmsnorm(c_q, q_norm_w), w_q_up)
    kv = jnp.einsum('bsr,rhd->bshd', rmsnorm(c_kv, kv_norm_w), w_kv_up)
    q_nope, q_pe = q[..., :C_NOPE], q[..., C_NOPE:]
    k_nope, v = kv[..., :C_NOPE], kv[..., C_NOPE:]
    cos, sin = rope_cos_sin(positions)
    q_pe = apply_rope(q_pe, cos[:, :, None, :], sin[:, :, None, :])
    k_pe = apply_rope(k_rope, cos, sin)
    q_full = jnp.concatenate([q_nope, q_pe], axis=-1)
    k_full = jnp.concatenate([k_nope, jnp.broadcast_to(k_pe[:, :, None, :], (bsz, s, C_HEADS, C_ROPE))], axis=-1)
    o = causal_block_attention(q_full, k_full, v, (C_NOPE + C_ROPE) ** -0.5)
    return o.reshape(bsz, s, C_WIDTH)


def peer_ffn(h, w_query, sub_keys, expert_u, expert_v):
    bsz, s, d = h.shape
    ht = h.reshape(bsz * s // PEER_CHUNK, PEER_CHUNK, d)

    def one(xc):
        q = jnp.einsum('td,dhq->thq', xc, w_query).reshape(PEER_CHUNK, PEER_HEADS, 2, PEER_HALF)
        sc = jnp.einsum('thpq,hpnq->thpn', q, sub_keys).astype(F32)
        s_top, i_top = lax.top_k(sc, PEER_TOPK)
        cand = s_top[:, :, 0, :, None] + s_top[:, :, 1, None, :]
        cand_idx = i_top[:, :, 0, :, None] * PEER_KEYS + i_top[:, :, 1, None, :]
        best, pos = lax.top_k(cand.reshape(PEER_CHUNK, PEER_HEADS, -1), PEER_TOPK)
        eidx = jnp.take_along_axis(cand_idx.reshape(PEER_CHUNK, PEER_HEADS, -1), pos, axis=-1)
        gate = jax.nn.softmax(best, axis=-1)
        u = expert_u[eidx]
        act = jax.nn.gelu(jnp.einsum('td,thkd->thk', xc, u).astype(F32))
        coef = (gate * act).astype(xc.dtype)
        return jnp.einsum('thk,thkd->td', coef, expert_v[eidx])

    return lax.map(one, ht).reshape(bsz, s, d)


def setup_inputs(seed: int = 0) -> dict:
    key = jax.random.key(seed)
    ks = iter(jax.random.split(key, 40))
    nrm = lambda shape, scale: jax.random.normal(next(ks), shape, F32) * scale
    gain = lambda shape: 1.0 + 0.05 * jax.random.normal(next(ks), shape, F32)
    L, D = DEPTH, D_MODEL
    x = jax.random.normal(next(ks), (BATCH, SEQ, D), F32)
    c = jax.random.normal(next(ks), (BATCH, D), F32)
    offset = jax.random.randint(next(ks), (BATCH, 1), 0, 4096, dtype=jnp.int32)
    positions = offset + jnp.arange(SEQ, dtype=jnp.int32)[None, :]
    a0 = jax.random.uniform(next(ks), (L, A_WIDTH), F32, 0.9, 0.999) ** (1.0 / LRU_C)
    lru_lambda = jnp.log(a0) - jnp.log1p(-a0)
    dn_a_log = jnp.log(jax.random.uniform(next(ks), (L, B_HEADS), F32, 1.0, 16.0))
    dt = jnp.exp(jax.random.uniform(next(ks), (L, B_HEADS), F32, math.log(1e-3), math.log(1e-1)))
    dn_dt_bias = dt + jnp.log(-jnp.expm1(-dt))
    return {
        "x": x,
        "c": c,
        "positions": positions,
        "mod_w": nrm((D, N_MOD * D), 0.5 * D ** -0.5),
        "mod_layer": nrm((L, N_MOD, D), 0.3),
        "norm_mix_w": gain((L, D)),
        "w_in": nrm((L, D, D_IN), D ** -0.5),
        "lru_conv_w": nrm((L, CONV_W, A_WIDTH), 0.5),
        "lru_conv_b": nrm((L, A_WIDTH), 0.02),
        "lru_wa": nrm((L, A_HEADS, A_HEAD_DIM, A_HEAD_DIM), A_HEAD_DIM ** -0.5),
        "lru_ba": nrm((L, A_WIDTH), 0.1),
        "lru_wx": nrm((L, A_HEADS, A_HEAD_DIM, A_HEAD_DIM), A_HEAD_DIM ** -0.5),
        "lru_bx": nrm((L, A_WIDTH), 0.1),
        "lru_lambda": lru_lambda,
        "dn_conv_w": nrm((L, CONV_W, 3 * B_WIDTH), 0.5),
        "dn_a_log": dn_a_log,
        "dn_dt_bias": dn_dt_bias,
        "dn_norm_w": gain((L, B_HEAD_DIM)),
        "mla_q_norm_w": gain((L, Q_LORA)),
        "mla_w_q_up": nrm((L, Q_LORA, C_HEADS, C_NOPE + C_ROPE), Q_LORA ** -0.5),
        "mla_kv_norm_w": gain((L, KV_LORA)),
        "mla_w_kv_up": nrm((L, KV_LORA, C_HEADS, C_NOPE + C_VDIM), KV_LORA ** -0.5),
        "branch_norm_a": gain((L, A_WIDTH)),
        "branch_norm_c": gain((L, C_WIDTH)),
        "w_out": nrm((L, D_MIX, D), D_MIX ** -0.5),
        "norm_ffn_w": gain((L, D)),
        "peer_w_query": nrm((L, D, PEER_HEADS, PEER_QDIM), D ** -0.5),
        "peer_sub_keys": nrm((L, PEER_HEADS, 2, PEER_KEYS, PEER_HALF), PEER_HALF ** -0.5),
        "peer_u": nrm((L, PEER_N, D), D ** -0.5),
        "peer_v": nrm((L, PEER_N, D), 0.5),
        "final_norm_w": gain((D,)),
    }


def reference(x, c, positions, mod_w, mod_layer, norm_mix_w, w_in, lru_conv_w, lru_conv_b,
              lru_wa, lru_ba, lru_wx, lru_bx, lru_lambda, dn_conv_w, dn_a_log, dn_dt_bias,
              dn_norm_w, mla_q_norm_w, mla_w_q_up, mla_kv_norm_w, mla_w_kv_up, branch_norm_a,
              branch_norm_c, w_out, norm_ffn_w, peer_w_query, peer_sub_keys, peer_u, peer_v,
              final_norm_w):
    bsz = x.shape[0]
    base = (jax.nn.silu(c) @ mod_w).reshape(bsz, N_MOD, D_MODEL)
    for l in range(DEPTH):
        m = base + mod_layer[l]
        sh_a, sc_a, g_a, sh_f, sc_f, g_f = [m[:, i, None, :] for i in range(N_MOD)]

        h = rmsnorm(x, norm_mix_w[l]) * (1.0 + sc_a) + sh_a
        (a_rec, a_gate, b_q, b_k, b_v, b_z, b_beta, b_alpha,
         c_q, c_kv, c_kr) = split_columns(h @ w_in[l])
        ya = rglru_group(a_rec, a_gate, lru_conv_w[l], lru_conv_b[l], lru_wa[l], lru_ba[l],
                         lru_wx[l], lru_bx[l], lru_lambda[l])
        yb = gated_deltanet_group(b_q, b_k, b_v, b_z, b_beta, b_alpha, dn_conv_w[l],
                                  dn_a_log[l], dn_dt_bias[l], dn_norm_w[l])
        yc = mla_group(c_q, c_kv, c_kr, positions, mla_q_norm_w[l], mla_w_q_up[l],
                       mla_kv_norm_w[l], mla_w_kv_up[l])
        y = jnp.concatenate([rmsnorm(ya, branch_norm_a[l]), yb, rmsnorm(yc, branch_norm_c[l])], axis=-1)
        x = x + g_a * (y @ w_out[l])

        h = rmsnorm(x, norm_ffn_w[l]) * (1.0 + sc_f) + sh_f
        x = x + g_f * peer_ffn(h, peer_w_query[l], peer_sub_keys[l], peer_u[l], peer_v[l])
    return rmsnorm(x, final_norm_w)
```

```python
import contextlib
import numpy as np
import concourse.bass as bass
import concourse.mybir as mybir
from concourse.bass_utils import run_bass_kernel_spmd

F32 = mybir.dt.float32
BF16 = mybir.dt.bfloat16
I32 = mybir.dt.int32
AF = mybir.ActivationFunctionType
ALU = mybir.AluOpType
AX = mybir.AxisListType

NCORES = 8
D = 4096
NCH = D // 128
EPS = 1e-6


class TT:
    def __init__(self, h, name):
        self.h = h; self.name = name; self.w = None; self.r = []
    def __getitem__(self, idx):
        return self.h[idx]


class TV:
    def __init__(self, parent, ap, name):
        self.p = parent; self.h = ap; self.name = name; self.excl = getattr(parent, "excl", False)
    def __getitem__(self, idx):
        return self.h[idx]
    @property
    def w(self): return self.p.w
    @w.setter
    def w(self, v): self.p.w = v
    @property
    def r(self): return self.p.r
    @r.setter
    def r(self, v): self.p.r = v


class KB:
    NDMA = 6
    def __init__(self, nc, stack):
        self.nc = nc; self.stack = stack
        self.eng = {"pe": nc.tensor, "act": nc.scalar, "dve": nc.vector, "pool": nc.gpsimd, "sp": nc.sync}
        self.sem = {}; self.cnt = {}; self.seen = {e: {} for e in self.eng}
        for e in self.eng:
            self.sem[e] = stack.enter_context(nc.semaphore("s_" + e)); self.cnt[e] = 0
        self.drr = {}
        for q in ("sp", "pool", "act"):
            for i in range(self.NDMA):
                k = "d_%s%d" % (q, i)
                self.sem[k] = stack.enter_context(nc.semaphore(k)); self.cnt[k] = 0
            self.drr[q] = 0
        self.ntile = 0
        self.last_out = []
    def sb(self, shape, dt=F32, name=None, stack=None):
        self.ntile += 1
        name = "%s_%d" % (name or "t", self.ntile)
        return TT((stack or self.stack).enter_context(self.nc.sbuf_tensor(name, list(shape), dt)), name)
    def ps(self, shape, dt=F32, name=None, stack=None):
        self.ntile += 1
        name = "%s_%d" % (name or "p", self.ntile)
        t = TT((stack or self.stack).enter_context(self.nc.psum_tensor(name, list(shape), dt)), name)
        t.excl = True
        return t
    def _wait(self, e, tok):
        if tok is None: return
        k, v = tok
        if self.seen[e].get(k, 0) >= v: return
        self.eng[e].wait_ge(self.sem[k], v)
        self.seen[e][k] = v
    def _deps(self, e, w, r):
        for t in r:
            self._wait(e, t.w)
            if getattr(t, "excl", False):
                for tok in t.r: self._wait(e, tok)
        for t in w:
            self._wait(e, t.w)
            for tok in t.r: self._wait(e, tok)
    def _mark(self, tok, w, r):
        for t in r:
            if t not in w: t.r.append(tok)
        for t in w:
            t.w = tok; t.r = []
    def op(self, e, fn, w=(), r=()):
        self._deps(e, w, r)
        ins = fn(self.eng[e])
        self.cnt[e] += 1
        ins.then_inc(self.sem[e], 1)
        tok = (e, self.cnt[e])
        self._mark(tok, w, r)
        return tok
    def dma(self, q, out, in_, w=(), r=(), **kw):
        i = self.drr[q]; self.drr[q] = (i + 1) % self.NDMA
        k = "d_%s%d" % (q, i)
        if self.cnt[k] > 0: self._wait(q, (k, self.cnt[k]))
        self._deps(q, w, r)
        ins = self.eng[q].dma_start(out=out, in_=in_, **kw)
        self.cnt[k] += 16
        ins.then_inc(self.sem[k], 16)
        tok = (k, self.cnt[k])
        self._mark(tok, w, r)
        return tok
    def out_dma(self, q, out, in_, r=(), **kw):
        tok = self.dma(q, out, in_, r=r, **kw)
        self.last_out.append(tok)
        return tok
    def finish(self, e="sp"):
        for t in self.last_out: self._wait(e, t)


def _new_nc():
    return bass.Bass("TRN2", target_bir_lowering=False)


def colvec(v):
    v = np.asarray(v, np.float32).reshape(-1, 128)
    return np.ascontiguousarray(v.T)


def build_mod(ncols):
    nc = _new_nc()
    ccol = nc.dram_tensor("ccol", [128, NCH], F32, kind="ExternalInput").ap()
    wm = nc.dram_tensor("wm", [D, ncols], F32, kind="ExternalInput").ap()
    ml = nc.dram_tensor("ml", [2, ncols], F32, kind="ExternalInput").ap()
    out = nc.dram_tensor("out", [2, ncols], F32, kind="ExternalOutput").ap()
    ng = ncols // 512
    with contextlib.ExitStack() as st:
        k = KB(nc, st)
        c_t = k.sb([128, NCH]); s_t = k.sb([128, NCH])
        k.dma("sp", c_t[:], ccol[:, :], w=[c_t])
        k.op("act", lambda e: e.activation(out=s_t[:], in_=c_t[:], func=AF.Silu), w=[s_t], r=[c_t])
        wb = [k.sb([128, ncols], name="wmb%d" % i) for i in range(3)]
        acc = [k.ps([1, 512], name="macc%d" % g) for g in range(ng)]
        for kc in range(NCH):
            b = wb[kc % 3]
            k.dma("sp" if kc % 2 == 0 else "pool", b[:], wm[kc * 128:(kc + 1) * 128, :], w=[b])
            for g in range(ng):
                k.op("pe", lambda e, g=g, b=b, kc=kc: e.matmul(acc[g][:], lhsT=s_t[:, kc:kc + 1], rhs=b[:, g * 512:(g + 1) * 512],
                                                      start=(kc == 0), stop=(kc == NCH - 1)), w=[acc[g]], r=[s_t, b])
        base = k.sb([1, ncols]); mlt = k.sb([1, 2, ncols]); res = k.sb([1, 2, ncols])
        k.dma("sp", mlt[:], ml.rearrange("(o l) n -> o l n", o=1), w=[mlt])
        for g in range(ng):
            k.op("act", lambda e, g=g: e.activation(out=base[:, g * 512:(g + 1) * 512], in_=acc[g][:], func=AF.Copy), w=[base], r=[acc[g]])
        for l in range(2):
            k.op("dve", lambda e, l=l: e.tensor_tensor(out=res[:, l, :], in0=mlt[:, l, :], in1=base[:], op=ALU.add), w=[res], r=[mlt, base])
        k.out_dma("sp", out.rearrange("(o l) n -> o l n", o=1), res[:], r=[res])
        k.finish()
    return nc


def emit_norm_mod(k, ones, src_tile, nchunks, tt, wmod, sh, dst_fn, ps_ssq, scratch, rs, dim, c0=0):
    for c in range(nchunks):
        sq = scratch[c % 2]
        k.op("act", lambda e, c=c, sq=sq: e.activation(out=sq[:], in_=src_tile[:, c0 + c, :], func=AF.Square), w=[sq], r=[src_tile])
        k.op("pe", lambda e, c=c, sq=sq: e.matmul(ps_ssq[:], lhsT=ones[:], rhs=sq[:], start=(c == 0), stop=(c == nchunks - 1)),
             w=[ps_ssq], r=[ones, sq])
    k.op("act", lambda e: e.activation(out=rs[:], in_=ps_ssq[:], func=AF.Sqrt, scale=1.0 / dim, bias=EPS), w=[rs], r=[ps_ssq])
    k.op("dve", lambda e: e.reciprocal(out=rs[:], in_=rs[:]), w=[rs], r=[rs])
    for c in range(nchunks):
        tmp = scratch[c % 2]
        k.op("dve", lambda e, c=c, tmp=tmp: e.scalar_tensor_tensor(out=tmp[:], in0=src_tile[:, c0 + c, :], scalar=wmod[:, c:c + 1], in1=rs[:],
                                                             op0=ALU.mult, op1=ALU.mult), w=[tmp], r=[src_tile, wmod, rs])
        ap, dt_ = dst_fn(c)
        if sh is None:
            k.op("act", lambda e, tmp=tmp, ap=ap: e.activation(out=ap, in_=tmp[:], func=AF.Copy), w=[dt_], r=[tmp])
        else:
            k.op("act", lambda e, c=c, tmp=tmp, ap=ap: e.activation(out=ap, in_=tmp[:], func=AF.Identity, bias=sh[:, c:c + 1], scale=1.0),
                 w=[dt_], r=[tmp, sh])


def build_pre(tc, out_dt):
    nc = _new_nc()
    xT = nc.dram_tensor("xT", [D, tc], F32, kind="ExternalInput").ap()
    wn = nc.dram_tensor("wn", [128, NCH], F32, kind="ExternalInput").ap()
    scc = nc.dram_tensor("scc", [128, NCH], F32, kind="ExternalInput").ap()
    shc = nc.dram_tensor("shc", [128, NCH], F32, kind="ExternalInput").ap()
    hT = nc.dram_tensor("hT", [D, tc], out_dt, kind="ExternalOutput").ap()
    TT_ = 512
    with contextlib.ExitStack() as st:
        k = KB(nc, st)
        ones = k.sb([128, 128]); k.op("dve", lambda e: e.memset(ones[:], 1.0), w=[ones])
        w_t = k.sb([128, NCH]); sc_t = k.sb([128, NCH]); sh_t = k.sb([128, NCH]); wmod = k.sb([128, NCH])
        k.dma("sp", w_t[:], wn[:, :], w=[w_t]); k.dma("sp", sc_t[:], scc[:, :], w=[sc_t]); k.dma("sp", sh_t[:], shc[:, :], w=[sh_t])
        k.op("dve", lambda e: e.scalar_tensor_tensor(out=wmod[:], in0=sc_t[:], scalar=1.0, in1=w_t[:], op0=ALU.add, op1=ALU.mult),
             w=[wmod], r=[sc_t, w_t])
        xt = [k.sb([128, NCH, TT_], name="xt%d" % i) for i in range(2)]
        ht = [k.sb([128, NCH, TT_], out_dt, name="ht%d" % i) for i in range(1)]
        scratch = [k.sb([128, TT_], name="sq%d" % i) for i in range(2)]
        rs = k.sb([128, TT_]); ps_ssq = k.ps([128, TT_])
        xv = xT.rearrange("(c p) t -> p c t", p=128); hv = hT.rearrange("(c p) t -> p c t", p=128)
        for it in range(tc // TT_):
            x_ = xt[it % 2]; h_ = ht[0]
            for half in range(2):
                cs = slice(half * 16, half * 16 + 16)
                k.dma("sp" if half == 0 else "pool", x_[:, cs, :], xv[:, cs, it * TT_:(it + 1) * TT_], w=[x_])
            emit_norm_mod(k, ones, x_, NCH, TT_, wmod, sh_t, lambda c, h_=h_: (h_[:, c, :], h_), ps_ssq, scratch, rs, float(D))
            for half in range(2):
                cs = slice(half * 16, half * 16 + 16)
                k.out_dma("sp" if half == 0 else "pool", hv[:, cs, it * TT_:(it + 1) * TT_], h_[:, cs, :], r=[h_])
        k.finish()
    return nc


def run(nc, in_maps):
    res = run_bass_kernel_spmd(nc, in_maps, core_ids=list(range(len(in_maps))))
    return res.results


ROPE = 64
ATT_SCALE = 192.0 ** -0.5
NEGBIG = -30000.0


def mix_consts():
    i = np.arange(128)
    c = {}
    c["ident"] = np.eye(128, dtype=np.float32)
    c["tri"] = (i[:, None] <= i[None, :]).astype(np.float32)
    c["negl"] = np.where(i[:, None] > i[None, :], 0.0, NEGBIG).astype(np.float32)
    c["negu"] = np.where(i[:, None] <= i[None, :], 0.0, NEGBIG).astype(np.float32)
    c["bd16"] = ((i[:, None] // 16) == (i[None, :] // 16)).astype(np.float32)
    for b in (16, 32, 64):
        off = (((i[:, None] // (2 * b)) == (i[None, :] // (2 * b))) & ((i[:, None] % (2 * b)) >= b) & ((i[None, :] % (2 * b)) < b)).astype(np.float32)
        c["off%d" % b] = off
        if b < 64:
            c["offT%d" % b] = np.ascontiguousarray(off.T)
    rm = np.zeros((64, 64), np.float32)
    for m in range(32):
        rm[m + 32, m] = -1.0
        rm[m, m + 32] = 1.0
    c["rot"] = rm
    inv = (10000.0 ** (-np.arange(0, 64, 2, dtype=np.float32) / 64.0)).astype(np.float32)
    c["invf"] = np.concatenate([inv, inv]).reshape(64, 1).astype(np.float32)
    p = np.arange(128)[:, None, None]; m = np.arange(4)[None, :, None]; q = np.arange(512)[None, None, :]
    c["amask"] = ((128 * m + p) <= q).astype(np.float32)
    return c


def build_mix(T, do_a=True, do_b=True, do_c=True):
    nc = _new_nc()
    BT = 512
    NB = T // BT
    def din(name, shape, dt=F32):
        return nc.dram_tensor(name, list(shape), dt, kind="ExternalInput").ap()
    hT = din("hT", [D, T], BF16)
    w_ab = din("w_ab", [D, 8 * 128]); w_c = din("w_c", [D, 12 * 128 + 64]); w_tm = din("w_tm", [D, 260])
    lru_p = din("lru_p", [128, 8]); lru_w = din("lru_w", [128, 256])
    dn_cw = din("dn_cw", [128, 24]); dn_hp = din("dn_hp", [128, 4]); dn_nw = din("dn_nw", [128, 128])
    qnw = din("qnw", [128, 8]); kvnw = din("kvnw", [128, 4])
    wq = din("wq", [128, 8, 384]); wkv = din("wkv", [128, 4, 512])
    pos = din("pos", [1, T], I32)
    c_ident = din("ident", [128, 128]); c_tri = din("tri", [128, 128]); c_negl = din("negl", [128, 128]); c_negu = din("negu", [128, 128])
    c_blk = {n: din(n, [128, 128]) for n in ("bd16", "off16", "off32", "off64", "offT16", "offT32")}
    c_rot = din("rot", [64, 64]); c_invf = din("invf", [64, 1]); c_amask = din("amask", [128, 4, 512])
    yaT = nc.dram_tensor("yaT", [128, T], F32, kind="ExternalOutput").ap()
    yb = nc.dram_tensor("yb", [T, 2, 128], F32, kind="ExternalOutput").ap()
    yc = nc.dram_tensor("yc", [T, 2, 128], F32, kind="ExternalOutput").ap()
    s_qn = [nc.dram_tensor("s_qn%d" % h, [128, T], BF16, kind="Internal").ap() for h in range(2)]
    s_qp = [nc.dram_tensor("s_qp%d" % h, [65, T], BF16, kind="Internal").ap() for h in range(2)]
    s_kn = [nc.dram_tensor("s_kn%d" % h, [128, T], BF16, kind="Internal").ap() for h in range(2)]
    s_kp = nc.dram_tensor("s_kp", [65, T], BF16, kind="Internal").ap()
    s_v = [nc.dram_tensor("s_v%d" % h, [128, T // 128, 129], BF16, kind="Internal").ap() for h in range(2)]
    s_km = nc.dram_tensor("s_km", [128, 2], F32, kind="Internal").ap()
    hv = hT.rearrange("(c p) t -> p c t", p=128)

    with contextlib.ExitStack() as st:
        k = KB(nc, st)

        def barrier():
            for e in k.eng:
                for key in k.sem:
                    if k.cnt[key] > 0:
                        k._wait(e, (key, k.cnt[key]))

        def load_h(htq, b):
            for qd in range(4):
                k.dma("sp", htq[qd][:], hv[:, qd * 8:(qd + 1) * 8, b * BT:(b + 1) * BT], w=[htq[qd]])

        if do_a or do_b:
          with contextlib.ExitStack() as p1:
            ones = k.sb([128, 128], stack=p1); negones = k.sb([128, 128], stack=p1)
            k.op("dve", lambda e: e.memset(ones[:], 1.0), w=[ones]); k.op("dve", lambda e: e.memset(negones[:], -1.0), w=[negones])
            ident = k.sb([128, 128], stack=p1); tri = k.sb([128, 128], stack=p1); negl = k.sb([128, 128], stack=p1); negu = k.sb([128, 128], stack=p1)
            for t_, s_ in ((ident, c_ident), (tri, c_tri), (negl, c_negl), (negu, c_negu)):
                k.dma("sp", t_[:], s_[:, :], w=[t_])
            cm = {}
            for n_, ap_ in c_blk.items():
                cm[n_] = k.sb([128, 128], stack=p1, name="cm_" + n_)
                k.dma("sp", cm[n_][:], ap_[:, :], w=[cm[n_]])
            htq = [k.sb([128, 8, BT], BF16, stack=p1, name="htq%d" % i) for i in range(4)]
            wst = [k.sb([128, NCH, 128], BF16, stack=p1, name="wst%d" % i) for i in range(3)]
            wtm = k.sb([128, NCH, 260], BF16, stack=p1)
            for half in range(2):
                k.dma("pool", wtm[:, half * 16:(half + 1) * 16, :], w_tm.rearrange("(c p) n -> p c n", p=128)[:, half * 16:(half + 1) * 16, :], w=[wtm])
            big = [k.ps([128, BT], stack=p1, name="big%d" % i) for i in range(3)]
            smb = [k.ps([128, 512], stack=p1, name="smb%d" % i) for i in range(3)]
            small = [TV(smb[i % 3], smb[i % 3].h[:, (i // 3) * 128:(i // 3 + 1) * 128], "sm%d" % i) for i in range(12)]
            ptm = k.ps([128, 512], stack=p1, name="ptm")
            ctr = {"big": 0, "small": 0, "w": 0}
            def nbig():
                ctr["big"] += 1; return big[ctr["big"] % 3]
            def nsm():
                ctr["small"] += 1; return small[ctr["small"] % 12]
            wabv = w_ab.rearrange("(c p) n -> p c n", p=128)
            def p1_chunk(j):
                wt = wst[ctr["w"] % 3]; ctr["w"] += 1
                k.dma("pool", wt[:], wabv[:, :, j * 128:(j + 1) * 128], w=[wt])
                ps = nbig()
                for fc in range(NCH):
                    k.op("pe", lambda e, fc=fc, ps=ps, wt=wt: e.matmul(ps[:], lhsT=wt[:, fc, :], rhs=htq[fc // 8][:, fc % 8, :],
                                                                       start=(fc == 0), stop=(fc == NCH - 1)), w=[ps], r=[wt, htq[fc // 8]])
                return ps
            def conv(dst, xbuf, cw, col0, bias_ap, eng="pool"):
                if bias_ap is None:
                    k.op(eng, lambda e: e.tensor_scalar(out=dst[:], in0=xbuf[:, 0:BT], scalar1=cw[:, col0:col0 + 1], scalar2=None, op0=ALU.mult), w=[dst], r=[xbuf, cw])
                else:
                    k.op(eng, lambda e: e.tensor_scalar(out=dst[:], in0=xbuf[:, 0:BT], scalar1=cw[:, col0:col0 + 1], scalar2=bias_ap, op0=ALU.mult, op1=ALU.add), w=[dst], r=[xbuf, cw])
                for j in range(1, 4):
                    k.op("dve", lambda e, j=j: e.scalar_tensor_tensor(out=dst[:], in0=xbuf[:, j:j + BT], scalar=cw[:, col0 + j:col0 + j + 1], in1=dst[:],
                                                                     op0=ALU.mult, op1=ALU.add), w=[dst], r=[xbuf, cw, dst])
                k.op("pool", lambda e: e.tensor_copy(out=xbuf[:, 0:3], in_=xbuf[:, BT:BT + 3]), w=[xbuf], r=[xbuf])

            lp = k.sb([128, 8], stack=p1); lw = k.sb([128, 256], stack=p1)
            k.dma("sp", lp[:], lru_p[:, :], w=[lp]); k.dma("sp", lw[:], lru_w[:, :], w=[lw])
            c1 = k.sb([128, 2], stack=p1)
            k.op("act", lambda e: e.activation(out=c1[:, 0:1], in_=lp[:, 7:8], func=AF.Exp, scale=-1.0), w=[c1], r=[lp])
            k.op("act", lambda e: e.activation(out=c1[:, 0:1], in_=c1[:, 0:1], func=AF.Ln, bias=1.0, scale=1.0), w=[c1], r=[c1])
            k.op("dve", lambda e: e.tensor_scalar(out=c1[:, 1:2], in0=c1[:, 0:1], scalar1=-16.0, scalar2=None, op0=ALU.mult), w=[c1], r=[c1])
            k.op("dve", lambda e: e.tensor_scalar(out=c1[:, 0:1], in0=c1[:, 0:1], scalar1=-8.0, scalar2=None, op0=ALU.mult), w=[c1], r=[c1])
            xa = k.sb([128, BT + 3], stack=p1); k.op("dve", lambda e: e.memset(xa[:], 0.0), w=[xa])
            hst = [k.sb([128, BT], stack=p1, name="hst%d" % i) for i in range(2)]
            k.op("dve", lambda e: e.memset(hst[1][:], 0.0), w=[hst[1]])
            A_t = {n: k.sb([128, BT], stack=p1, name="A_" + n) for n in ("u", "r", "i", "a", "b", "g")}
            dcw = k.sb([128, 24], stack=p1); dhp = k.sb([128, 4], stack=p1); dnw = k.sb([128, 128], stack=p1)
            k.dma("sp", dcw[:], dn_cw[:, :], w=[dcw]); k.dma("sp", dhp[:], dn_hp[:, :], w=[dhp]); k.dma("sp", dnw[:], dn_nw[:, :], w=[dnw])
            nega = k.sb([128, 2], stack=p1)
            k.op("act", lambda e: e.activation(out=nega[:], in_=dhp[:, 0:2], func=AF.Exp), w=[nega], r=[dhp])
            k.op("dve", lambda e: e.tensor_scalar(out=nega[:], in0=nega[:], scalar1=-1.0, scalar2=None, op0=ALU.mult), w=[nega], r=[nega])
            xb_ = [[k.sb([128, BT + 3], stack=p1, name="xb%d%d" % (h, i)) for i in range(3)] for h in range(2)]
            for h in range(2):
                for i in range(3):
                    k.op("pool", lambda e, h=h, i=i: e.memset(xb_[h][i][:], 0.0), w=[xb_[h][i]])
            S_ = [[k.sb([128, 128], stack=p1, name="S%d%d" % (h, i)) for i in range(2)] for h in range(2)]
            for h in range(2):
                k.op("pool", lambda e, h=h: e.memset(S_[h][0][:], 0.0), w=[S_[h][0]])
            B_t = {n: k.sb([128, BT], stack=p1, name="B_" + n) for n in ("qc", "kc", "vc", "sq", "rsq", "rsk", "qn", "kn")}
            sm_names = ["gt", "dl", "el", "du", "eu", "N", "NT", "qkT", "kbg", "ktail", "vb", "PTa", "PTb", "Qa", "QTa", "Qb", "QTb",
                        "u", "wT", "vnew", "o1", "o", "zs", "junk", "yout",
                        "N16", "No16", "No32", "No64", "NT16", "NoT16", "NoT32", "Xa", "Xb", "Z", "Zp"]
            Bs = {n: k.sb([128, 128], stack=p1, name="Bs_" + n) for n in sm_names}
            cols = {n: k.sb([128, 2], stack=p1, name="Bc_" + n) for n in ("beta", "nbeta", "g", "gc", "gl", "egc", "egl", "etail", "sp", "ssq", "rso")}
            tmsb = k.sb([128, 260], stack=p1)
            k.op("dve", lambda e: e.memset(cols["g"][:], 0.0), w=[cols["g"]])
            sblk = [0, 0]

            for b in range(NB):
                load_h(htq, b)
                if do_a:
                    ps = p1_chunk(0)
                    k.op("act", lambda e, ps=ps: e.activation(out=xa[:, 3:3 + BT], in_=ps[:], func=AF.Copy), w=[xa], r=[ps])
                    u = A_t["u"]
                    conv(u, xa, lp, 0, lp[:, 4:5])
                    for nm, woff, bcol in (("r", 0, 5), ("i", 128, 6)):
                        pg = nbig()
                        k.op("pe", lambda e, pg=pg, woff=woff: e.matmul(pg[:], lhsT=lw[:, woff:woff + 128], rhs=u[:], start=True, stop=True), w=[pg], r=[lw, u])
                        k.op("act", lambda e, pg=pg, nm=nm, bcol=bcol: e.activation(out=A_t[nm][:], in_=pg[:], func=AF.Sigmoid, bias=lp[:, bcol:bcol + 1], scale=1.0),
                             w=[A_t[nm]], r=[pg, lp])
                    k.op("act", lambda e: e.activation(out=A_t["a"][:], in_=A_t["r"][:], func=AF.Exp, scale=c1[:, 0:1]), w=[A_t["a"]], r=[A_t["r"], c1])
                    k.op("act", lambda e: e.activation(out=A_t["b"][:], in_=A_t["r"][:], func=AF.Exp, scale=c1[:, 1:2]), w=[A_t["b"]], r=[A_t["r"], c1])
                    k.op("dve", lambda e: e.tensor_scalar(out=A_t["b"][:], in0=A_t["b"][:], scalar1=-1.0, scalar2=1.0, op0=ALU.mult, op1=ALU.add), w=[A_t["b"]], r=[A_t["b"]])
                    k.op("dve", lambda e: e.tensor_scalar(out=A_t["b"][:], in0=A_t["b"][:], scalar1=1e-30, scalar2=None, op0=ALU.max), w=[A_t["b"]], r=[A_t["b"]])
                    k.op("act", lambda e: e.activation(out=A_t["b"][:], in_=A_t["b"][:], func=AF.Sqrt), w=[A_t["b"]], r=[A_t["b"]])
                    k.op("pool", lambda e: e.tensor_tensor(out=A_t["i"][:], in0=A_t["i"][:], in1=u[:], op=ALU.mult), w=[A_t["i"]], r=[A_t["i"], u])
                    k.op("pool", lambda e: e.tensor_tensor(out=A_t["b"][:], in0=A_t["b"][:], in1=A_t["i"][:], op=ALU.mult), w=[A_t["b"]], r=[A_t["b"], A_t["i"]])
                    hp_, hc_ = hst[(b + 1) % 2], hst[b % 2]
                    k.op("dve", lambda e, hp_=hp_, hc_=hc_: e.tensor_tensor_scan(out=hc_[:], data0=A_t["a"][:], data1=A_t["b"][:], initial=hp_[:, BT - 1:BT],
                                                                               op0=ALU.mult, op1=ALU.add), w=[hc_], r=[A_t["a"], A_t["b"], hp_])
                    ps = p1_chunk(1)
                    k.op("act", lambda e, ps=ps: e.activation(out=A_t["g"][:], in_=ps[:], func=AF.Gelu_apprx_tanh), w=[A_t["g"]], r=[ps])
                    k.op("pool", lambda e, hc_=hc_: e.tensor_tensor(out=A_t["g"][:], in0=A_t["g"][:], in1=hc_[:], op=ALU.mult), w=[A_t["g"]], r=[A_t["g"], hc_])
                    k.out_dma("sp", yaT[:, b * BT:(b + 1) * BT], A_t["g"][:], r=[A_t["g"]])
                if do_b:
                    for h in range(2):
                        dst3 = (B_t["qc"], B_t["kc"], B_t["vc"])
                        for i in range(3):
                            ps = p1_chunk(2 + 3 * h + i)
                            xbuf = xb_[h][i]
                            k.op("act", lambda e, ps=ps, xbuf=xbuf: e.activation(out=xbuf[:, 3:3 + BT], in_=ps[:], func=AF.Copy), w=[xbuf], r=[ps])
                            conv(dst3[i], xbuf, dcw, (h * 3 + i) * 4, None)
                            k.op("act", lambda e, d_=dst3[i]: e.activation(out=d_[:], in_=d_[:], func=AF.Silu), w=[dst3[i]], r=[dst3[i]])
                        for src, rs_, dstn, mul in ((B_t["qc"], B_t["rsq"], B_t["qn"], 128.0 ** -0.5), (B_t["kc"], B_t["rsk"], B_t["kn"], 1.0)):
                            k.op("act", lambda e, src=src: e.activation(out=B_t["sq"][:], in_=src[:], func=AF.Square), w=[B_t["sq"]], r=[src])
                            pg = nbig()
                            k.op("pe", lambda e, pg=pg: e.matmul(pg[:], lhsT=ones[:], rhs=B_t["sq"][:], start=True, stop=True), w=[pg], r=[ones, B_t["sq"]])
                            k.op("act", lambda e, pg=pg, rs_=rs_: e.activation(out=rs_[:], in_=pg[:], func=AF.Sqrt, bias=EPS, scale=1.0), w=[rs_], r=[pg])
                            k.op("dve", lambda e, rs_=rs_: e.reciprocal(out=rs_[:], in_=rs_[:]), w=[rs_], r=[rs_])
                            k.op("dve", lambda e, src=src, rs_=rs_, dstn=dstn, mul=mul: e.scalar_tensor_tensor(out=dstn[:], in0=src[:], scalar=mul, in1=rs_[:],
                                                                                                             op0=ALU.mult, op1=ALU.mult), w=[dstn], r=[src, rs_])
                        qn, kn, vc = B_t["qn"], B_t["kn"], B_t["vc"]
                        for s in range(4):
                            ts_ = slice(s * 128, (s + 1) * 128)
                            if h == 0:
                                pass
                            for fc in range(NCH):
                                k.op("pe", lambda e, fc=fc, ts_=ts_: e.matmul(ptm[:, 0:260], lhsT=htq[fc // 8][:, fc % 8, ts_], rhs=wtm[:, fc, :],
                                                                          start=(fc == 0), stop=(fc == NCH - 1)), w=[ptm], r=[wtm, htq[fc // 8]])
                            k.op("act", lambda e: e.activation(out=tmsb[:], in_=ptm[:, 0:260], func=AF.Copy), w=[tmsb], r=[ptm])
                            C = cols
                            k.op("act", lambda e, h=h: e.activation(out=C["beta"][:, 0:1], in_=tmsb[:, 256 + h:257 + h], func=AF.Sigmoid), w=[C["beta"]], r=[tmsb])
                            k.op("dve", lambda e: e.tensor_scalar(out=C["nbeta"][:, 0:1], in0=C["beta"][:, 0:1], scalar1=-1.0, scalar2=None, op0=ALU.mult), w=[C["nbeta"]], r=[C["beta"]])
                            k.op("act", lambda e, h=h: e.activation(out=C["sp"][:, 0:1], in_=tmsb[:, 258 + h:259 + h], func=AF.Exp, bias=dhp[:, 2 + h:3 + h], scale=1.0), w=[C["sp"]], r=[tmsb, dhp])
                            k.op("act", lambda e: e.activation(out=C["sp"][:, 0:1], in_=C["sp"][:, 0:1], func=AF.Ln, bias=1.0, scale=1.0), w=[C["sp"]], r=[C["sp"]])
                            k.op("dve", lambda e, h=h: e.tensor_scalar(out=C["g"][:, 0:1], in0=C["sp"][:, 0:1], scalar1=nega[:, h:h + 1], scalar2=None, op0=ALU.mult), w=[C["g"]], r=[C["sp"], nega])
                            g = C["g"]
                            k.op("dve", lambda e: e.tensor_scalar(out=Bs["gt"][:], in0=tri[:], scalar1=g[:, 0:1], scalar2=None, op0=ALU.mult), w=[Bs["gt"]], r=[tri, g])
                            pD = nsm()
                            k.op("pe", lambda e, pD=pD: e.matmul(pD[:], lhsT=Bs["gt"][:], rhs=ones[:], start=True, stop=False), w=[pD], r=[Bs["gt"], ones])
                            k.op("pe", lambda e, pD=pD: e.matmul(pD[:], lhsT=negones[:], rhs=Bs["gt"][:], start=False, stop=True), w=[pD], r=[Bs["gt"], negones])
                            pgc = nsm()
                            k.op("pe", lambda e, pgc=pgc: e.matmul(pgc[:, 0:2], lhsT=tri[:], rhs=g[:, 0:2], start=True, stop=True), w=[pgc], r=[tri, g])
                            k.op("pe", lambda e, pgc=pgc: e.matmul(pgc[:, 2:4], lhsT=ones[:], rhs=g[:, 0:2], start=True, stop=True), w=[pgc], r=[ones, g])
                            k.op("act", lambda e, pgc=pgc: e.activation(out=C["egc"][:, 0:1], in_=pgc[:, 0:1], func=AF.Exp), w=[C["egc"]], r=[pgc])
                            k.op("act", lambda e, pgc=pgc: e.activation(out=C["egl"][:, 0:1], in_=pgc[:, 2:3], func=AF.Exp), w=[C["egl"]], r=[pgc])
                            k.op("act", lambda e, pgc=pgc: e.activation(out=C["gl"][:, 0:1], in_=pgc[:, 2:3], func=AF.Copy), w=[C["gl"]], r=[pgc])
                            k.op("act", lambda e, pgc=pgc: e.activation(out=C["etail"][:, 0:1], in_=pgc[:, 0:1], func=AF.Exp, bias=C["gl"][:, 0:1], scale=-1.0), w=[C["etail"]], r=[pgc, C["gl"]])
                            k.op("dve", lambda e, pD=pD: e.tensor_tensor(out=Bs["dl"][:], in0=pD[:], in1=negl[:], op=ALU.add), w=[Bs["dl"]], r=[pD, negl])
                            k.op("act", lambda e: e.activation(out=Bs["el"][:], in_=Bs["dl"][:], func=AF.Exp), w=[Bs["el"]], r=[Bs["dl"]])
                            k.op("dve", lambda e, pD=pD: e.scalar_tensor_tensor(out=Bs["du"][:], in0=pD[:], scalar=-1.0, in1=negu[:], op0=ALU.mult, op1=ALU.add), w=[Bs["du"]], r=[pD, negu])
                            k.op("act", lambda e: e.activation(out=Bs["eu"][:], in_=Bs["du"][:], func=AF.Exp), w=[Bs["eu"]], r=[Bs["du"]])
                            pG = nsm()
                            k.op("pe", lambda e, pG=pG, ts_=ts_: e.matmul(pG[:], lhsT=kn[:, ts_], rhs=kn[:, ts_], start=True, stop=True), w=[pG], r=[kn])
                            k.op("dve", lambda e, pG=pG: e.scalar_tensor_tensor(out=Bs["N"][:], in0=pG[:], scalar=C["nbeta"][:, 0:1], in1=Bs["el"][:], op0=ALU.mult, op1=ALU.mult),
                                 w=[Bs["N"]], r=[pG, C["nbeta"], Bs["el"]])
                            pQ = nsm()
                            k.op("pe", lambda e, pQ=pQ, ts_=ts_: e.matmul(pQ[:], lhsT=kn[:, ts_], rhs=qn[:, ts_], start=True, stop=True), w=[pQ], r=[kn, qn])
                            k.op("dve", lambda e, pQ=pQ: e.tensor_tensor(out=Bs["qkT"][:], in0=pQ[:], in1=Bs["eu"][:], op=ALU.mult), w=[Bs["qkT"]], r=[pQ, Bs["eu"]])
                            pK = nsm()
                            k.op("pe", lambda e, pK=pK, ts_=ts_: e.matmul(pK[:], lhsT=kn[:, ts_], rhs=ident[:], start=True, stop=True), w=[pK], r=[kn, ident])
                            k.op("dve", lambda e, pK=pK: e.tensor_scalar(out=Bs["kbg"][:], in0=pK[:], scalar1=C["beta"][:, 0:1], scalar2=C["egc"][:, 0:1], op0=ALU.mult, op1=ALU.mult),
                                 w=[Bs["kbg"]], r=[pK, C["beta"], C["egc"]])
                            k.op("dve", lambda e, pK=pK: e.tensor_scalar(out=Bs["ktail"][:], in0=pK[:], scalar1=C["etail"][:, 0:1], scalar2=None, op0=ALU.mult), w=[Bs["ktail"]], r=[pK, C["etail"]])
                            pV = nsm()
                            k.op("pe", lambda e, pV=pV, ts_=ts_: e.matmul(pV[:], lhsT=vc[:, ts_], rhs=ident[:], start=True, stop=True), w=[pV], r=[vc, ident])
                            k.op("dve", lambda e, pV=pV: e.tensor_scalar(out=Bs["vb"][:], in0=pV[:], scalar1=C["beta"][:, 0:1], scalar2=None, op0=ALU.mult), w=[Bs["vb"]], r=[pV, C["beta"]])
                            N_ = Bs["N"]
                            k.op("pool", lambda e: e.tensor_tensor(out=Bs["N16"][:], in0=N_[:], in1=cm["bd16"][:], op=ALU.mult), w=[Bs["N16"]], r=[N_, cm["bd16"]])
                            for b_ in (16, 32, 64):
                                k.op("pool", lambda e, b_=b_: e.tensor_tensor(out=Bs["No%d" % b_][:], in0=N_[:], in1=cm["off%d" % b_][:], op=ALU.mult), w=[Bs["No%d" % b_]], r=[N_, cm["off%d" % b_]])
                            pT = nsm()
                            k.op("pe", lambda e, pT=pT: e.matmul(pT[:], lhsT=N_[:], rhs=ident[:], start=True, stop=True), w=[pT], r=[N_, ident])
                            k.op("dve", lambda e, pT=pT: e.tensor_tensor(out=Bs["NT16"][:], in0=pT[:], in1=cm["bd16"][:], op=ALU.mult), w=[Bs["NT16"]], r=[pT, cm["bd16"]])
                            for b_ in (16, 32):
                                k.op("dve", lambda e, pT=pT, b_=b_: e.tensor_tensor(out=Bs["NoT%d" % b_][:], in0=pT[:], in1=cm["offT%d" % b_][:], op=ALU.mult), w=[Bs["NoT%d" % b_]], r=[pT, cm["offT%d" % b_]])
                            Xc, Xn = Bs["Xa"], Bs["Xb"]; Yc, Yn = Bs["PTa"], Bs["PTb"]
                            k.op("pool", lambda e, Xc=Xc: e.tensor_tensor(out=Xc[:], in0=Bs["N16"][:], in1=ident[:], op=ALU.add), w=[Xc], r=[Bs["N16"], ident])
                            k.op("pool", lambda e, Yc=Yc: e.tensor_tensor(out=Yc[:], in0=Bs["NT16"][:], in1=ident[:], op=ALU.add), w=[Yc], r=[Bs["NT16"], ident])
                            Qc, QTc = Bs["N16"], Bs["NT16"]
                            for s_ in range(3):
                                Qn_, QTn_ = (Bs["Qa"], Bs["QTa"]) if s_ % 2 == 0 else (Bs["Qb"], Bs["QTb"])
                                p1_ = nsm()
                                k.op("pe", lambda e, p1_=p1_, Qc=Qc, QTc=QTc: e.matmul(p1_[:], lhsT=QTc[:], rhs=Qc[:], start=True, stop=True), w=[p1_], r=[Qc, QTc])
                                k.op("act", lambda e, p1_=p1_, Qn_=Qn_: e.activation(out=Qn_[:], in_=p1_[:], func=AF.Copy), w=[Qn_], r=[p1_])
                                if s_ < 2:
                                    p2_ = nsm()
                                    k.op("pe", lambda e, p2_=p2_, Qc=Qc, QTc=QTc: e.matmul(p2_[:], lhsT=Qc[:], rhs=QTc[:], start=True, stop=True), w=[p2_], r=[Qc, QTc])
                                    k.op("act", lambda e, p2_=p2_, QTn_=QTn_: e.activation(out=QTn_[:], in_=p2_[:], func=AF.Copy), w=[QTn_], r=[p2_])
                                pX = nsm()
                                k.op("pe", lambda e, pX=pX, Yc=Yc, Qn_=Qn_: e.matmul(pX[:], lhsT=Yc[:], rhs=Qn_[:], start=True, stop=True), w=[pX], r=[Yc, Qn_])
                                pY = nsm()
                                k.op("pe", lambda e, pY=pY, Yc=Yc, Qn_=Qn_: e.matmul(pY[:], lhsT=Qn_[:], rhs=Yc[:], start=True, stop=True), w=[pY], r=[Yc, Qn_])
                                k.op("dve", lambda e, pX=pX, Xc=Xc, Xn=Xn: e.tensor_tensor(out=Xn[:], in0=pX[:], in1=Xc[:], op=ALU.add), w=[Xn], r=[pX, Xc])
                                k.op("dve", lambda e, pY=pY, Yc=Yc, Yn=Yn: e.tensor_tensor(out=Yn[:], in0=pY[:], in1=Yc[:], op=ALU.add), w=[Yn], r=[pY, Yc])
                                Xc, Xn = Xn, Xc; Yc, Yn = Yn, Yc
                                Qc, QTc = Qn_, QTn_
                            for b_ in (16, 32, 64):
                                pZ = nsm()
                                k.op("pe", lambda e, pZ=pZ, Yc=Yc, b_=b_: e.matmul(pZ[:], lhsT=Bs["No%d" % b_][:], rhs=Yc[:], start=True, stop=True), w=[pZ], r=[Bs["No%d" % b_], Yc])
                                k.op("act", lambda e, pZ=pZ: e.activation(out=Bs["Z"][:], in_=pZ[:], func=AF.Copy), w=[Bs["Z"]], r=[pZ])
                                if b_ < 64:
                                    pZp = nsm()
                                    k.op("pe", lambda e, pZp=pZp, Xc=Xc, b_=b_: e.matmul(pZp[:], lhsT=Bs["NoT%d" % b_][:], rhs=Xc[:], start=True, stop=True), w=[pZp], r=[Bs["NoT%d" % b_], Xc])
                                    k.op("act", lambda e, pZp=pZp: e.activation(out=Bs["Zp"][:], in_=pZp[:], func=AF.Copy), w=[Bs["Zp"]], r=[pZp])
                                pY = nsm()
                                k.op("pe", lambda e, pY=pY, Xc=Xc: e.matmul(pY[:], lhsT=Xc[:], rhs=Bs["Z"][:], start=True, stop=True), w=[pY], r=[Xc, Bs["Z"]])
                                if b_ < 64:
                                    pX = nsm()
                                    k.op("pe", lambda e, pX=pX, Yc=Yc: e.matmul(pX[:], lhsT=Yc[:], rhs=Bs["Zp"][:], start=True, stop=True), w=[pX], r=[Yc, Bs["Zp"]])
                                k.op("dve", lambda e, pY=pY, Yc=Yc, Yn=Yn: e.tensor_tensor(out=Yn[:], in0=pY[:], in1=Yc[:], op=ALU.add), w=[Yn], r=[pY, Yc])
                                if b_ < 64:
                                    k.op("dve", lambda e, pX=pX, Xc=Xc, Xn=Xn: e.tensor_tensor(out=Xn[:], in0=pX[:], in1=Xc[:], op=ALU.add), w=[Xn], r=[pX, Xc])
                                    Xc, Xn = Xn, Xc
                                Yc, Yn = Yn, Yc
                            PTc = Yc
                            AiT = PTc
                            pU = nsm()
                            k.op("pe", lambda e, pU=pU, AiT=AiT: e.matmul(pU[:], lhsT=AiT[:], rhs=Bs["vb"][:], start=True, stop=True), w=[pU], r=[AiT, Bs["vb"]])
                            k.op("act", lambda e, pU=pU: e.activation(out=Bs["u"][:], in_=pU[:], func=AF.Copy), w=[Bs["u"]], r=[pU])
                            pW = nsm()
                            k.op("pe", lambda e, pW=pW, AiT=AiT: e.matmul(pW[:], lhsT=Bs["kbg"][:], rhs=AiT[:], start=True, stop=True), w=[pW], r=[AiT, Bs["kbg"]])
                            k.op("act", lambda e, pW=pW: e.activation(out=Bs["wT"][:], in_=pW[:], func=AF.Copy), w=[Bs["wT"]], r=[pW])
                            Sc = S_[h][sblk[h] % 2]; Sn = S_[h][(sblk[h] + 1) % 2]; sblk[h] += 1
                            pws = nsm()
                            k.op("pe", lambda e, pws=pws, Sc=Sc: e.matmul(pws[:], lhsT=Bs["wT"][:], rhs=Sc[:], start=True, stop=True), w=[pws], r=[Bs["wT"], Sc])
                            k.op("dve", lambda e, pws=pws: e.tensor_tensor(out=Bs["vnew"][:], in0=Bs["u"][:], in1=pws[:], op=ALU.subtract), w=[Bs["vnew"]], r=[Bs["u"], pws])
                            po1 = nsm()
                            k.op("pe", lambda e, po1=po1, Sc=Sc, ts_=ts_: e.matmul(po1[:], lhsT=qn[:, ts_], rhs=Sc[:], start=True, stop=True), w=[po1], r=[qn, Sc])
                            k.op("dve", lambda e, po1=po1: e.tensor_scalar(out=Bs["o1"][:], in0=po1[:], scalar1=C["egc"][:, 0:1], scalar2=None, op0=ALU.mult), w=[Bs["o1"]], r=[po1, C["egc"]])
                            po2 = nsm()
                            k.op("pe", lambda e, po2=po2: e.matmul(po2[:], lhsT=Bs["qkT"][:], rhs=Bs["vnew"][:], start=True, stop=True), w=[po2], r=[Bs["qkT"], Bs["vnew"]])
                            k.op("dve", lambda e, po2=po2: e.tensor_tensor(out=Bs["o"][:], in0=Bs["o1"][:], in1=po2[:], op=ALU.add), w=[Bs["o"]], r=[Bs["o1"], po2])
                            pS = nsm()
                            k.op("pe", lambda e, pS=pS: e.matmul(pS[:], lhsT=Bs["ktail"][:], rhs=Bs["vnew"][:], start=True, stop=True), w=[pS], r=[Bs["ktail"], Bs["vnew"]])
                            k.op("dve", lambda e, pS=pS, Sc=Sc, Sn=Sn: e.scalar_tensor_tensor(out=Sn[:], in0=Sc[:], scalar=C["egl"][:, 0:1], in1=pS[:], op0=ALU.mult, op1=ALU.add),
                                 w=[Sn], r=[Sc, C["egl"], pS])
                            k.op("act", lambda e: e.activation(out=Bs["junk"][:], in_=Bs["o"][:], func=AF.Square), w=[Bs["junk"]], r=[Bs["o"]])
                            k.op("dve", lambda e: e.reduce_sum(out=C["ssq"][:, 0:1], in_=Bs["junk"][:], axis=AX.X), w=[C["ssq"]], r=[Bs["junk"]])
                            k.op("act", lambda e: e.activation(out=C["rso"][:, 0:1], in_=C["ssq"][:, 0:1], func=AF.Sqrt, bias=EPS, scale=1.0 / 128), w=[C["rso"]], r=[C["ssq"]])
                            k.op("dve", lambda e: e.reciprocal(out=C["rso"][:, 0:1], in_=C["rso"][:, 0:1]), w=[C["rso"]], r=[C["rso"]])
                            k.op("act", lambda e, h=h: e.activation(out=Bs["zs"][:], in_=tmsb[:, h * 128:(h + 1) * 128], func=AF.Silu), w=[Bs["zs"]], r=[tmsb])
                            k.op("pool", lambda e: e.tensor_tensor(out=Bs["zs"][:], in0=Bs["zs"][:], in1=dnw[:], op=ALU.mult), w=[Bs["zs"]], r=[Bs["zs"], dnw])
                            k.op("dve", lambda e: e.scalar_tensor_tensor(out=Bs["yout"][:], in0=Bs["o"][:], scalar=C["rso"][:, 0:1], in1=Bs["zs"][:], op0=ALU.mult, op1=ALU.mult),
                                 w=[Bs["yout"]], r=[Bs["o"], C["rso"], Bs["zs"]])
                            k.out_dma("sp", yb[b * BT + s * 128:b * BT + (s + 1) * 128, h, :], Bs["yout"][:], r=[Bs["yout"]])
            barrier()

        if do_c:
          kmax = k.sb([128, 2], name="kmax"); nkm = k.sb([128, 2], name="nkm")
          k.op("dve", lambda e: e.memset(kmax[:], 0.0), w=[kmax])
          with contextlib.ExitStack() as p1:
            ones = k.sb([128, 128], stack=p1, name="c_ones")
            k.op("dve", lambda e: e.memset(ones[:], 1.0), w=[ones])
            htq = [k.sb([128, 8, BT], BF16, stack=p1, name="chtq%d" % i) for i in range(4)]
            wst = [k.sb([128, NCH, 128], BF16, stack=p1, name="cwst%d" % i) for i in range(3)]
            big = [k.ps([128, BT], stack=p1, name="cbig%d" % i) for i in range(4)]
            ps_ssq = k.ps([128, BT], stack=p1, name="c_ssq"); ps_ssk = k.ps([128, BT], stack=p1, name="c_ssk"); ps_ssp = k.ps([128, BT], stack=p1, name="c_ssp")
            ctr = {"big": 0, "w": 0}
            def nbig():
                ctr["big"] += 1; return big[ctr["big"] % 4]
            wcv = w_c.rearrange("(c p) n -> p c n", p=128)
            def pc_chunk(j, width=128):
                wt = wst[ctr["w"] % 3]; ctr["w"] += 1
                k.dma("pool", wt[:, :, 0:width], wcv[:, :, j * 128:j * 128 + width], w=[wt])
                ps = nbig()
                for fc in range(NCH):
                    k.op("pe", lambda e, fc=fc, ps=ps, wt=wt: e.matmul(ps[0:width, :], lhsT=wt[:, fc, 0:width], rhs=htq[fc // 8][:, fc % 8, :],
                                                                       start=(fc == 0), stop=(fc == NCH - 1)), w=[ps], r=[wt, htq[fc // 8]])
                return ps
            qnw_t = k.sb([128, 8], stack=p1); kvnw_t = k.sb([128, 4], stack=p1)
            k.dma("sp", qnw_t[:], qnw[:, :], w=[qnw_t]); k.dma("sp", kvnw_t[:], kvnw[:, :], w=[kvnw_t])
            wq_t = k.sb([128, 8, 384], BF16, stack=p1); wkv_t = k.sb([128, 4, 512], BF16, stack=p1)
            k.dma("pool", wq_t[:], wq[:, :, :], w=[wq_t]); k.dma("pool", wkv_t[:], wkv[:, :, :], w=[wkv_t])
            rot_t = k.sb([64, 64], stack=p1); invf_t = k.sb([64, 1], stack=p1)
            k.dma("sp", rot_t[:], c_rot[:, :], w=[rot_t]); k.dma("sp", invf_t[:], c_invf[:, :], w=[invf_t])
            cq = k.sb([128, 8, BT], stack=p1); ckv = k.sb([128, 4, BT], stack=p1); kr = k.sb([64, BT], stack=p1)
            qn_ = k.sb([128, 8, BT], BF16, stack=p1); kvn = k.sb([128, 4, BT], BF16, stack=p1)
            scratch = [k.sb([128, BT], stack=p1, name="csq%d" % i) for i in range(2)]
            rs = k.sb([128, BT], stack=p1)
            posi = k.sb([64, BT], I32, stack=p1)
            R = {n: k.sb([64, BT], stack=p1, name="R_" + n) for n in ("ang", "n", "r", "sin", "cos", "t1", "t2", "qpe")}
            qnope = k.sb([128, BT], BF16, stack=p1); knope = k.sb([128, BT], BF16, stack=p1)
            qpa = k.sb([65, BT], BF16, stack=p1); kpa = k.sb([65, BT], BF16, stack=p1)
            vaug = k.sb([128, 4, 129], BF16, stack=p1)
            k.op("dve", lambda e: e.memset(vaug[:], 1.0), w=[vaug])
            k.op("dve", lambda e: e.memset(kpa[64:65, :], 1.0), w=[kpa])
            bmax = k.sb([128, 1], stack=p1)
            TWO_PI = 6.283185307179586

            def rope(src, dst_ap, dst_t):
                pr = nbig()
                k.op("pe", lambda e: e.matmul(pr[0:64, :], lhsT=rot_t[:], rhs=src[:], start=True, stop=True), w=[pr], r=[rot_t, src])
                k.op("pool", lambda e: e.tensor_tensor(out=R["t1"][:], in0=src[:], in1=R["cos"][:], op=ALU.mult), w=[R["t1"]], r=[src, R["cos"]])
                k.op("dve", lambda e: e.tensor_tensor(out=R["t2"][:], in0=pr[0:64, :], in1=R["sin"][:], op=ALU.mult), w=[R["t2"]], r=[pr, R["sin"]])
                k.op("pool", lambda e: e.tensor_tensor(out=dst_ap, in0=R["t1"][:], in1=R["t2"][:], op=ALU.add), w=[dst_t], r=[R["t1"], R["t2"]])

            for b in range(NB):
                load_h(htq, b)
                bs = slice(b * BT, (b + 1) * BT)
                k.dma("sp", posi[:], pos[0:1, bs].broadcast_to([64, BT]), w=[posi])
                k.op("dve", lambda e: e.tensor_copy(out=R["ang"][:], in_=posi[:]), w=[R["ang"]], r=[posi])
                k.op("dve", lambda e: e.tensor_scalar(out=R["ang"][:], in0=R["ang"][:], scalar1=invf_t[:, 0:1], scalar2=None, op0=ALU.mult), w=[R["ang"]], r=[R["ang"], invf_t])
                k.op("dve", lambda e: e.tensor_scalar(out=R["n"][:], in0=R["ang"][:], scalar1=1.0 / TWO_PI, scalar2=12582912.0, op0=ALU.mult, op1=ALU.add), w=[R["n"]], r=[R["ang"]])
                k.op("dve", lambda e: e.tensor_scalar(out=R["n"][:], in0=R["n"][:], scalar1=-12582912.0, scalar2=None, op0=ALU.add), w=[R["n"]], r=[R["n"]])
                k.op("dve", lambda e: e.scalar_tensor_tensor(out=R["r"][:], in0=R["n"][:], scalar=-6.28125, in1=R["ang"][:], op0=ALU.mult, op1=ALU.add), w=[R["r"]], r=[R["n"], R["ang"]])
                k.op("dve", lambda e: e.scalar_tensor_tensor(out=R["r"][:], in0=R["n"][:], scalar=-(TWO_PI - 6.28125), in1=R["r"][:], op0=ALU.mult, op1=ALU.add), w=[R["r"]], r=[R["n"], R["r"]])
                k.op("dve", lambda e: e.tensor_scalar(out=R["r"][:], in0=R["r"][:], scalar1=3.14159, scalar2=-3.14159, op0=ALU.min, op1=ALU.max), w=[R["r"]], r=[R["r"]])
                k.op("act", lambda e: e.activation(out=R["sin"][:], in_=R["r"][:], func=AF.Sin), w=[R["sin"]], r=[R["r"]])
                k.op("dve", lambda e: e.tensor_scalar(out=R["t1"][:], in0=R["r"][:], scalar1=-1.0, scalar2=None, op0=ALU.mult), w=[R["t1"]], r=[R["r"]])
                k.op("dve", lambda e: e.tensor_tensor(out=R["t1"][:], in0=R["t1"][:], in1=R["r"][:], op=ALU.max), w=[R["t1"]], r=[R["t1"], R["r"]])
                k.op("dve", lambda e: e.tensor_scalar(out=R["t1"][:], in0=R["t1"][:], scalar1=-1.0, scalar2=1.5707963, op0=ALU.mult, op1=ALU.add), w=[R["t1"]], r=[R["t1"]])
                k.op("act", lambda e: e.activation(out=R["cos"][:], in_=R["t1"][:], func=AF.Sin), w=[R["cos"]], r=[R["t1"]])
                for j in range(8):
                    ps = pc_chunk(j)
                    k.op("act", lambda e, ps=ps, j=j: e.activation(out=cq[:, j, :], in_=ps[:], func=AF.Copy), w=[cq], r=[ps])
                for j in range(4):
                    ps = pc_chunk(8 + j)
                    k.op("act", lambda e, ps=ps, j=j: e.activation(out=ckv[:, j, :], in_=ps[:], func=AF.Copy), w=[ckv], r=[ps])
                ps = pc_chunk(12, 64)
                k.op("act", lambda e, ps=ps: e.activation(out=kr[:], in_=ps[0:64, :], func=AF.Copy), w=[kr], r=[ps])
                emit_norm_mod(k, ones, cq, 8, BT, qnw_t, None, lambda c: (qn_[:, c, :], qn_), ps_ssq, scratch, rs, 1024.0)
                emit_norm_mod(k, ones, ckv, 4, BT, kvnw_t, None, lambda c: (kvn[:, c, :], kvn), ps_ssq, scratch, rs, 512.0)
                rope(kr, R["qpe"][:], R["qpe"])
                k.op("act", lambda e: e.activation(out=kpa[0:64, :], in_=R["qpe"][:], func=AF.Copy), w=[kpa], r=[R["qpe"]])
                k.op("act", lambda e: e.activation(out=scratch[0][0:64, :], in_=R["qpe"][:], func=AF.Square), w=[scratch[0]], r=[R["qpe"]])
                k.op("pe", lambda e: e.matmul(ps_ssp[:], lhsT=ones[0:64, :], rhs=scratch[0][0:64, :], start=True, stop=True), w=[ps_ssp], r=[ones, scratch[0]])
                k.op("act", lambda e: e.activation(out=rs[:], in_=ps_ssp[:], func=AF.Copy), w=[rs], r=[ps_ssp])
                k.out_dma("sp", s_kp[:, bs], kpa[:], r=[kpa])
                for hh in range(2):
                    pq = nbig()
                    for rc in range(8):
                        k.op("pe", lambda e, rc=rc, pq=pq: e.matmul(pq[:], lhsT=wq_t[:, rc, hh * 192:hh * 192 + 128], rhs=qn_[:, rc, :], start=(rc == 0), stop=(rc == 7)), w=[pq], r=[wq_t, qn_])
                    k.op("act", lambda e, pq=pq: e.activation(out=qnope[:], in_=pq[:], func=AF.Copy), w=[qnope], r=[pq])
                    k.op("act", lambda e, pq=pq: e.activation(out=scratch[0][:], in_=pq[:], func=AF.Square), w=[scratch[0]], r=[pq])
                    k.op("pe", lambda e: e.matmul(ps_ssk[:], lhsT=ones[:], rhs=scratch[0][:], start=True, stop=False), w=[ps_ssk], r=[ones, scratch[0]])
                    pq2 = nbig()
                    for rc in range(8):
                        k.op("pe", lambda e, rc=rc, pq2=pq2: e.matmul(pq2[0:64, :], lhsT=wq_t[:, rc, hh * 192 + 128:hh * 192 + 192], rhs=qn_[:, rc, :], start=(rc == 0), stop=(rc == 7)), w=[pq2], r=[wq_t, qn_])
                    k.op("act", lambda e, pq2=pq2: e.activation(out=R["qpe"][:], in_=pq2[0:64, :], func=AF.Copy), w=[R["qpe"]], r=[pq2])
                    k.op("act", lambda e: e.activation(out=scratch[1][0:64, :], in_=R["qpe"][:], func=AF.Square), w=[scratch[1]], r=[R["qpe"]])
                    k.op("pe", lambda e: e.matmul(ps_ssk[:], lhsT=ones[0:64, :], rhs=scratch[1][0:64, :], start=False, stop=True), w=[ps_ssk], r=[ones, scratch[1]])
                    k.op("act", lambda e: e.activation(out=qpa[64:65, :], in_=ps_ssk[64:65, :], func=AF.Sqrt), w=[qpa], r=[ps_ssk])
                    rope(R["qpe"], qpa[0:64, :], qpa)
                    k.out_dma("sp", s_qn[hh][:, bs], qnope[:], r=[qnope])
                    k.out_dma("sp", s_qp[hh][:, bs], qpa[:], r=[qpa])
                    pk = nbig()
                    for rc in range(4):
                        k.op("pe", lambda e, rc=rc, pk=pk: e.matmul(pk[:], lhsT=wkv_t[:, rc, hh * 256:hh * 256 + 128], rhs=kvn[:, rc, :], start=(rc == 0), stop=(rc == 3)), w=[pk], r=[wkv_t, kvn])
                    k.op("act", lambda e, pk=pk: e.activation(out=knope[:], in_=pk[:], func=AF.Copy), w=[knope], r=[pk])
                    k.op("act", lambda e, pk=pk: e.activation(out=scratch[0][:], in_=pk[:], func=AF.Square), w=[scratch[0]], r=[pk])
                    k.op("pe", lambda e: e.matmul(ps_ssk[:], lhsT=ones[:], rhs=scratch[0][:], start=True, stop=True), w=[ps_ssk], r=[ones, scratch[0]])
                    k.op("dve", lambda e: e.tensor_tensor(out=scratch[1][:], in0=ps_ssk[:], in1=rs[:], op=ALU.add), w=[scratch[1]], r=[ps_ssk, rs])
                    k.op("dve", lambda e: e.reduce_max(out=bmax[:], in_=scratch[1][:], axis=AX.X), w=[bmax], r=[scratch[1]])
                    k.op("dve", lambda e, hh=hh: e.tensor_tensor(out=kmax[:, hh:hh + 1], in0=kmax[:, hh:hh + 1], in1=bmax[:], op=ALU.max), w=[kmax], r=[kmax, bmax])
                    k.out_dma("sp", s_kn[hh][:, bs], knope[:], r=[knope])
                    for sub in range(4):
                        pv = nbig()
                        for rc in range(4):
                            k.op("pe", lambda e, rc=rc, pv=pv, sub=sub: e.matmul(pv[:, 0:128], lhsT=kvn[:, rc, sub * 128:(sub + 1) * 128], rhs=wkv_t[:, rc, hh * 256 + 128:hh * 256 + 256],
                                                                        start=(rc == 0), stop=(rc == 3)), w=[pv], r=[wkv_t, kvn])
                        k.op("act", lambda e, pv=pv, sub=sub: e.activation(out=vaug[:, sub, 0:128], in_=pv[:, 0:128], func=AF.Copy), w=[vaug], r=[pv])
                    k.out_dma("sp", s_v[hh][:, 4 * b:4 * b + 4, :], vaug[:], r=[vaug])
            k.op("act", lambda e: e.activation(out=nkm[:], in_=kmax[:], func=AF.Sqrt), w=[nkm], r=[kmax])
            k.op("dve", lambda e: e.tensor_scalar(out=nkm[:], in0=nkm[:], scalar1=-1.0, scalar2=None, op0=ALU.mult), w=[nkm], r=[nkm])
            barrier()
          with contextlib.ExitStack() as p2:
            am = k.sb([128, 4, 512], BF16, stack=p2)
            k.dma("pool", am[:], c_amask[:, :, :], w=[am])
            kp_res = k.sb([65, T], BF16, stack=p2); kn_res = k.sb([128, T], BF16, stack=p2); v_res = k.sb([128, T // 128, 129], BF16, stack=p2)
            qn_g = [k.sb([128, 512], BF16, stack=p2, name="qn_g%d" % i) for i in range(2)]
            qp_g = [k.sb([65, 512], BF16, stack=p2, name="qp_g%d" % i) for i in range(2)]
            pt_ = [k.sb([128, 512], BF16, stack=p2, name="pt%d" % i) for i in range(3)]
            pss = [k.ps([128, 512], stack=p2, name="pss%d" % i) for i in range(2)]
            po = [k.ps([128, 512], stack=p2, name="po%d" % i) for i in range(4)]
            rden = k.sb([128, 4], stack=p2); osb = [k.sb([128, 128], stack=p2, name="osb%d" % i) for i in range(2)]
            k.dma("sp", kp_res[:], s_kp[:, :], w=[kp_res])
            it = 0
            for hh in range(2):
                k.dma("sp", kn_res[:], s_kn[hh][:, :], w=[kn_res])
                k.dma("sp", v_res[:], s_v[hh][:, :, :], w=[v_res])
                for g in range(T // 512):
                    qn = qn_g[g % 2]; qp = qp_g[g % 2]
                    k.dma("sp", qn[:], s_qn[hh][:, g * 512:(g + 1) * 512], w=[qn])
                    k.dma("sp", qp[:], s_qp[hh][:, g * 512:(g + 1) * 512], w=[qp])
                    k.op("dve", lambda e, qp=qp, hh=hh: e.tensor_scalar(out=qp[64:65, :], in0=qp[64:65, :], scalar1=nkm[64:65, hh:hh + 1], scalar2=None, op0=ALU.mult), w=[qp], r=[qp, nkm])
                    for kt in range(4 * g + 4):
                        ps = pss[it % 2]; pt = pt_[it % 3]; it += 1
                        ks = slice(kt * 128, (kt + 1) * 128)
                        k.op("pe", lambda e, ps=ps, ks=ks, qn=qn: e.matmul(ps[:], lhsT=kn_res[:, ks], rhs=qn[:], start=True, stop=False), w=[ps], r=[kn_res, qn])
                        k.op("pe", lambda e, ps=ps, ks=ks, qp=qp: e.matmul(ps[:], lhsT=kp_res[0:65, ks], rhs=qp[0:65, :], start=False, stop=True), w=[ps], r=[kp_res, qp])
                        k.op("act", lambda e, ps=ps, pt=pt: e.activation(out=pt[:], in_=ps[:], func=AF.Exp, scale=ATT_SCALE), w=[pt], r=[ps])
                        m = kt - 4 * g
                        if m >= 0:
                            k.op("pool", lambda e, pt=pt, m=m: e.tensor_tensor(out=pt[:], in0=pt[:], in1=am[:, m, :], op=ALU.mult), w=[pt], r=[pt, am])
                        for qs in range(4):
                            if m >= 0 and qs < m: continue
                            k.op("pe", lambda e, qs=qs, pt=pt, kt=kt: e.matmul(po[qs][:, 0:129], lhsT=pt[:, qs * 128:(qs + 1) * 128], rhs=v_res[:, kt, :],
                                                                         start=(kt == 0), stop=(kt == 4 * g + qs)), w=[po[qs]], r=[pt, v_res])
                    for qs in range(4):
                        ob = osb[qs % 2]
                        k.op("dve", lambda e, qs=qs: e.reciprocal(out=rden[:, qs:qs + 1], in_=po[qs][:, 128:129]), w=[rden], r=[po[qs]])
                        k.op("dve", lambda e, qs=qs, ob=ob: e.tensor_scalar(out=ob[:], in0=po[qs][:, 0:128], scalar1=rden[:, qs:qs + 1], scalar2=None, op0=ALU.mult), w=[ob], r=[po[qs], rden])
                        k.out_dma("sp", yc[g * 512 + qs * 128:g * 512 + (qs + 1) * 128, hh, :], ob[:], r=[ob])
            barrier()
        k.finish()
    return nc


OFF = {}
def _mk_off():
    names = ["a_rec", "a_gate", "b_q", "b_k", "b_v", "b_z", "b_beta", "b_alpha", "c_q", "c_kv", "c_kr"]
    widths = [1024, 1024, 1536, 1536, 1536, 1536, 12, 12, 1024, 512, 64]
    s = 0
    for n, w in zip(names, widths):
        OFF[n] = s; s += w
_mk_off()


def core_heads(c):
    return c, (c, 8 + c % 4), (c, 8 + c % 4)


def mix_inputs(c, l, P, hT_bf16, consts):
    ha, hb, hc = core_heads(c)
    w_in = P["w_in"][l]
    def cols(name, h, w=128):
        o = OFF[name] + h * w
        return w_in[:, o:o + w]
    w_ab = np.concatenate([cols("a_rec", ha), cols("a_gate", ha)] + [cols(n, h) for h in hb for n in ("b_q", "b_k", "b_v")], axis=1)
    w_tm = np.concatenate([cols("b_z", hb[0]), cols("b_z", hb[1]), cols("b_beta", hb[0], 1), cols("b_beta", hb[1], 1),
                           cols("b_alpha", hb[0], 1), cols("b_alpha", hb[1], 1)], axis=1)
    w_c = w_in[:, OFF["c_q"]:OFF["c_q"] + 1024 + 512 + 64]
    sl = slice(ha * 128, (ha + 1) * 128)
    lru_p = np.stack([P["lru_conv_w"][l][j, sl] for j in range(4)] + [P["lru_conv_b"][l][sl], P["lru_ba"][l][sl], P["lru_bx"][l][sl], P["lru_lambda"][l][sl]], axis=1)
    lru_w = np.concatenate([P["lru_wa"][l][ha], P["lru_wx"][l][ha]], axis=1)
    dn_cw = np.stack([P["dn_conv_w"][l][j, i * 1536 + h * 128:i * 1536 + (h + 1) * 128] for h in hb for i in range(3) for j in range(4)], axis=1)
    dn_hp = np.broadcast_to(np.array([P["dn_a_log"][l][hb[0]], P["dn_a_log"][l][hb[1]], P["dn_dt_bias"][l][hb[0]], P["dn_dt_bias"][l][hb[1]]], np.float32)[None, :], (128, 4))
    dn_nw = np.broadcast_to(P["dn_norm_w"][l][None, :], (128, 128))
    wq = P["mla_w_q_up"][l][:, list(hc), :].reshape(8, 128, 2 * 192).transpose(1, 0, 2)
    wkv = P["mla_w_kv_up"][l][:, list(hc), :].reshape(4, 128, 2 * 256).transpose(1, 0, 2)
    d = {"hT": hT_bf16, "w_ab": w_ab, "w_c": w_c, "w_tm": w_tm, "lru_p": lru_p, "lru_w": lru_w, "dn_cw": dn_cw, "dn_hp": dn_hp, "dn_nw": dn_nw,
         "qnw": colvec(P["mla_q_norm_w"][l]), "kvnw": colvec(P["mla_kv_norm_w"][l]), "wq": wq, "wkv": wkv, "pos": P["positions"].reshape(1, -1).astype(np.int32)}
    d.update(consts)
    return {kk: np.ascontiguousarray(v) for kk, v in d.items()}


NEGINF = -3.0e38


def build_ffn(tc):
    nc = _new_nc()
    TT_ = 256
    def din(name, shape, dt=F32):
        return nc.dram_tensor(name, list(shape), dt, kind="ExternalInput").ap()
    xT = din("xT", [D, tc]); yT = din("yT", [D, tc])
    vec = din("vec", [128, 9, NCH])
    w_out = din("w_out", [D, D]); w_qry = din("w_qry", [D, 2048]); keysT = din("keysT", [128, 16, 128])
    UT = din("UT", [D, 16384]); V = din("V", [16384, D]); c_ident = din("ident", [128, 128])
    oT = nc.dram_tensor("oT", [D, tc], F32, kind="ExternalOutput").ap()
    s_x1 = nc.dram_tensor("s_x1", [D, tc], F32, kind="Internal").ap()
    xv = xT.rearrange("(c p) t -> p c t", p=128); yv = yT.rearrange("(c p) t -> p c t", p=128)
    ov = oT.rearrange("(c p) t -> p c t", p=128); x1v = s_x1.rearrange("(c p) t -> p c t", p=128)
    wov = w_out.rearrange("(c p) n -> p c n", p=128); wqv = w_qry.rearrange("(c p) n -> p c n", p=128)
    utv = UT.rearrange("(c p) e -> p c e", p=128)
    with contextlib.ExitStack() as st:
        k = KB(nc, st)
        def barrier():
            for e in k.eng:
                for key in k.sem:
                    if k.cnt[key] > 0:
                        k._wait(e, (key, k.cnt[key]))
        ones = k.sb([128, 128]); k.op("dve", lambda e: e.memset(ones[:], 1.0), w=[ones])
        ident = k.sb([128, 128]); k.dma("sp", ident[:], c_ident[:, :], w=[ident])
        vt = k.sb([128, 9, NCH]); k.dma("sp", vt[:], vec[:, :, :], w=[vt])
        kT = k.sb([128, 16, 128]); k.dma("sp", kT[:], keysT[:, :, :], w=[kT])
        wmod = k.sb([128, NCH])
        k.op("dve", lambda e: e.scalar_tensor_tensor(out=wmod[:], in0=vt[:, 4, :], scalar=1.0, in1=vt[:, 3, :], op0=ALU.add, op1=ALU.mult), w=[wmod], r=[vt])
        bna = TV(vt, vt.h[:, 0, :], "bna"); bnc = TV(vt, vt.h[:, 1, :], "bnc"); shf = TV(vt, vt.h[:, 5, :], "shf")
        h2b = k.sb([128, NCH, TT_], BF16)
        qT = k.sb([128, 16, TT_])
        for it in range(tc // TT_):
            tsl = slice(it * TT_, (it + 1) * TT_)
            with contextlib.ExitStack() as pa:
                yt = k.sb([128, NCH, TT_], stack=pa); xt = k.sb([128, NCH, TT_], stack=pa); ynb = k.sb([128, NCH, TT_], BF16, stack=pa)
                wst = [k.sb([128, NCH, 128], BF16, stack=pa, name="fw%d" % i) for i in range(3)]
                wqs = [k.sb([128, NCH, 128], stack=pa, name="fq%d" % i) for i in range(2)]
                scratch = [k.sb([128, TT_], stack=pa, name="fsq%d" % i) for i in range(2)]
                rs = k.sb([128, TT_], stack=pa); ps_ssq = k.ps([128, TT_], stack=pa)
                big = [k.ps([128, TT_], stack=pa, name="fbig%d" % i) for i in range(3)]
                for half in range(2):
                    cs = slice(half * 16, half * 16 + 16)
                    k.dma("sp", yt[:, cs, :], yv[:, cs, tsl], w=[yt]); k.dma("sp", xt[:, cs, :], xv[:, cs, tsl], w=[xt])
                emit_norm_mod(k, ones, yt, 8, TT_, bna, None, lambda c: (ynb[:, c, :], ynb), ps_ssq, scratch, rs, 1024.0, c0=0)
                emit_norm_mod(k, ones, yt, 12, TT_, bnc, None, lambda c: (ynb[:, 20 + c, :], ynb), ps_ssq, scratch, rs, 1536.0, c0=20)
                k.op("pool", lambda e: e.tensor_copy(out=ynb[:, 8:20, :], in_=yt[:, 8:20, :]), w=[ynb], r=[yt])
                for n in range(NCH):
                    wt = wst[n % 3]
                    k.dma("pool", wt[:], wov[:, :, n * 128:(n + 1) * 128], w=[wt])
                    ps = big[n % 3]
                    for fc in range(NCH):
                        k.op("pe", lambda e, fc=fc, ps=ps, wt=wt: e.matmul(ps[:], lhsT=wt[:, fc, :], rhs=ynb[:, fc, :], start=(fc == 0), stop=(fc == NCH - 1)), w=[ps], r=[wt, ynb])
                    k.op("dve", lambda e, n=n, ps=ps: e.scalar_tensor_tensor(out=xt[:, n, :], in0=ps[:], scalar=vt[:, 2, n:n + 1], in1=xt[:, n, :], op0=ALU.mult, op1=ALU.add),
                         w=[xt], r=[ps, vt, xt])
                k.dma("sp", x1v[:, :, tsl], xt[:], r=[xt])
                emit_norm_mod(k, ones, xt, NCH, TT_, wmod, shf, lambda c: (yt[:, c, :], yt), ps_ssq, scratch, rs, float(D))
                k.op("pool", lambda e: e.tensor_copy(out=h2b[:], in_=yt[:]), w=[h2b], r=[yt])
                for j in range(16):
                    wq_ = wqs[j % 2]
                    k.dma("sp", wq_[:], wqv[:, :, j * 128:(j + 1) * 128], w=[wq_])
                    ps = big[j % 3]
                    for fc in range(NCH):
                        k.op("pe", lambda e, fc=fc, ps=ps, wq_=wq_: e.matmul(ps[:], lhsT=wq_[:, fc, :], rhs=yt[:, fc, :], start=(fc == 0), stop=(fc == NCH - 1)), w=[ps], r=[wq_, yt])
                    k.op("act", lambda e, j=j, ps=ps: e.activation(out=qT[:, j, :], in_=ps[:], func=AF.Copy), w=[qT], r=[ps])
                barrier()
            with contextlib.ExitStack() as pb:
                NS = TT_ // 128
                sc = [k.sb([128, 16, 128], stack=pb, name="sc%d" % i) for i in range(NS)]
                tops = [k.sb([128, 16, 16], stack=pb, name="tops%d" % i) for i in range(NS)]
                tmp128 = k.sb([128, 128], stack=pb); cand = k.sb([128, 16, 16], stack=pb); cand2 = k.sb([128, 256], stack=pb)
                best = k.sb([128, 16], stack=pb); e16 = k.sb([128, 16], stack=pb)
                thr = [k.sb([128, 8], stack=pb, name="thr%d" % i) for i in range(NS)]
                negm = [k.sb([128, 8], stack=pb, name="negm%d" % i) for i in range(NS)]
                rZ = [k.sb([128, 8], stack=pb, name="rZ%d" % i) for i in range(NS)]
                gacc = [k.sb([128, 16, 128], stack=pb, name="gacc%d" % i) for i in range(NS)]
                S_ = k.sb([128, 16, 128], stack=pb); E_ = k.sb([128, 16, 128], stack=pb); M_ = k.sb([128, 16, 128], stack=pb)
                ust = [k.sb([128, NCH, 128], BF16, stack=pb, name="ust%d" % i) for i in range(2)]
                vg = [k.sb([128, D], BF16, stack=pb, name="vg%d" % i) for i in range(4)]
                actT = k.sb([128, TT_], stack=pb); coefT = [k.sb([128, TT_], BF16, stack=pb, name="coefT%d" % i) for i in range(4)]
                acc_out = k.sb([128, NCH, TT_], stack=pb)
                psm = [k.ps([128, 512], stack=pb, name="fpsm%d" % i) for i in range(2)]
                pu = [k.ps([128, TT_], stack=pb, name="fpu%d" % i) for i in range(2)]
                pg = k.ps([128, TT_], stack=pb, name="fpg"); pv = [k.ps([128, TT_], stack=pb, name="fpv%d" % i) for i in range(2)]
                for sb_ in range(NS):
                    ssl = slice(sb_ * 128, (sb_ + 1) * 128)
                    for hp in range(16):
                        ps = psm[hp % 2]
                        k.op("pe", lambda e, hp=hp, ps=ps: e.matmul(ps[:, 0:128], lhsT=qT[:, hp, ssl], rhs=kT[:, hp, :], start=True, stop=True), w=[ps], r=[qT, kT])
                        k.op("act", lambda e, hp=hp, ps=ps: e.activation(out=sc[sb_][:, hp, :], in_=ps[:, 0:128], func=AF.Copy), w=[sc[sb_]], r=[ps])
                        k.op("dve", lambda e, hp=hp: e.max(out=tops[sb_][:, hp, 0:8], in_=sc[sb_][:, hp, :]), w=[tops[sb_]], r=[sc[sb_]])
                        k.op("dve", lambda e, hp=hp: e.match_replace(out=tmp128[:], in_to_replace=tops[sb_][:, hp, 0:8], in_values=sc[sb_][:, hp, :], imm_value=NEGINF),
                             w=[tmp128], r=[tops[sb_], sc[sb_]])
                        k.op("dve", lambda e, hp=hp: e.max(out=tops[sb_][:, hp, 8:16], in_=tmp128[:]), w=[tops[sb_]], r=[tmp128])
                    for h in range(8):
                        for a in range(16):
                            k.op("dve", lambda e, a=a, h=h: e.tensor_scalar(out=cand[:, a, :], in0=tops[sb_][:, 2 * h + 1, :], scalar1=tops[sb_][:, 2 * h, a:a + 1], scalar2=None, op0=ALU.add),
                                 w=[cand], r=[tops[sb_]])
                        cf = cand.h[:].rearrange("p a b -> p (a b)")
                        k.op("dve", lambda e: e.max(out=best[:, 0:8], in_=cf), w=[best], r=[cand])
                        k.op("dve", lambda e: e.match_replace(out=cand2[:], in_to_replace=best[:, 0:8], in_values=cf, imm_value=NEGINF), w=[cand2], r=[best, cand])
                        k.op("dve", lambda e: e.max(out=best[:, 8:16], in_=cand2[:]), w=[best], r=[cand2])
                        k.op("dve", lambda e, h=h: e.tensor_copy(out=thr[sb_][:, h:h + 1], in_=best[:, 15:16]), w=[thr[sb_]], r=[best])
                        k.op("dve", lambda e, h=h: e.tensor_scalar(out=negm[sb_][:, h:h + 1], in0=best[:, 0:1], scalar1=-1.0, scalar2=None, op0=ALU.mult), w=[negm[sb_]], r=[best])
                        k.op("act", lambda e, h=h: e.activation(out=e16[:], in_=best[:], func=AF.Exp, bias=negm[sb_][:, h:h + 1], scale=1.0), w=[e16], r=[best, negm[sb_]])
                        k.op("dve", lambda e, h=h: e.reduce_sum(out=rZ[sb_][:, h:h + 1], in_=e16[:], axis=AX.X), w=[rZ[sb_]], r=[e16])
                        k.op("dve", lambda e, h=h: e.reciprocal(out=rZ[sb_][:, h:h + 1], in_=rZ[sb_][:, h:h + 1]), w=[rZ[sb_]], r=[rZ[sb_]])
                gi = 0
                for blk in range(8):
                    for sb_ in range(NS):
                        for h in range(8):
                            for a in range(16):
                                i1 = blk * 16 + a
                                k.op("dve" if a % 2 == 0 else "pool", lambda e, a=a, i1=i1, h=h: e.tensor_scalar(out=S_[:, a, :], in0=sc[sb_][:, 2 * h + 1, :], scalar1=sc[sb_][:, 2 * h, i1:i1 + 1],
                                                                                                           scalar2=None, op0=ALU.add), w=[S_], r=[sc[sb_]])
                            k.op("act", lambda e, h=h: e.activation(out=E_[:], in_=S_[:], func=AF.Exp, bias=negm[sb_][:, h:h + 1], scale=1.0), w=[E_], r=[S_, negm[sb_]])
                            k.op("dve", lambda e, h=h: e.scalar_tensor_tensor(out=M_[:], in0=S_[:], scalar=thr[sb_][:, h:h + 1], in1=E_[:], op0=ALU.is_ge, op1=ALU.mult), w=[M_], r=[S_, thr[sb_], E_])
                            if h == 0:
                                k.op("dve", lambda e, h=h: e.tensor_scalar(out=gacc[sb_][:], in0=M_[:], scalar1=rZ[sb_][:, h:h + 1], scalar2=None, op0=ALU.mult), w=[gacc[sb_]], r=[M_, rZ[sb_]])
                            else:
                                k.op("dve", lambda e, h=h: e.scalar_tensor_tensor(out=gacc[sb_][:], in0=M_[:], scalar=rZ[sb_][:, h:h + 1], in1=gacc[sb_][:], op0=ALU.mult, op1=ALU.add),
                                     w=[gacc[sb_]], r=[M_, rZ[sb_], gacc[sb_]])
                    for grp in range(4):
                        for ei in range(4):
                            a = grp * 4 + ei; et = blk * 16 + a
                            us = ust[et % 2]
                            k.dma("pool", us[:], utv[:, :, et * 128:(et + 1) * 128], w=[us])
                            k.dma("pool", vg[ei][:], V[et * 128:(et + 1) * 128, :], w=[vg[ei]])
                            p_u = pu[et % 2]
                            for fc in range(NCH):
                                k.op("pe", lambda e, fc=fc, p_u=p_u, us=us: e.matmul(p_u[:], lhsT=us[:, fc, :], rhs=h2b[:, fc, :], start=(fc == 0), stop=(fc == NCH - 1)), w=[p_u], r=[us, h2b])
                            k.op("act", lambda e, p_u=p_u: e.activation(out=actT[:], in_=p_u[:], func=AF.Gelu_apprx_tanh), w=[actT], r=[p_u])
                            for sb_ in range(NS):
                                k.op("pe", lambda e, sb_=sb_, a=a: e.matmul(pg[:, sb_ * 128:(sb_ + 1) * 128], lhsT=gacc[sb_][:, a, :], rhs=ident[:], start=True, stop=True), w=[pg], r=[gacc[sb_], ident])
                            k.op("dve", lambda e, ei=ei: e.tensor_tensor(out=coefT[ei][:], in0=pg[:], in1=actT[:], op=ALU.mult), w=[coefT[ei]], r=[pg, actT])
                        for n in range(NCH):
                            p_v = pv[n % 2]
                            for ei in range(4):
                                k.op("pe", lambda e, ei=ei, n=n, p_v=p_v: e.matmul(p_v[:], lhsT=vg[ei][:, n * 128:(n + 1) * 128], rhs=coefT[ei][:], start=(ei == 0), stop=(ei == 3)), w=[p_v], r=[vg[ei], coefT[ei]])
                            if gi == 0:
                                k.op("act", lambda e, n=n, p_v=p_v: e.activation(out=acc_out[:, n, :], in_=p_v[:], func=AF.Copy), w=[acc_out], r=[p_v])
                            else:
                                k.op("dve", lambda e, n=n, p_v=p_v: e.tensor_tensor(out=acc_out[:, n, :], in0=p_v[:], in1=acc_out[:, n, :], op=ALU.add), w=[acc_out], r=[p_v, acc_out])
                        gi += 1
                x1t = S_
                x1tv = x1t.h[:].rearrange("p a b -> p (a b)")
                for q4 in range(4):
                    cs = slice(q4 * 8, q4 * 8 + 8)
                    k.dma("sp", x1tv.rearrange("p (c t) -> p c t", t=TT_), x1v[:, cs, tsl], w=[x1t])
                    for c in range(8):
                        n = q4 * 8 + c
                        k.op("dve", lambda e, n=n, c=c: e.scalar_tensor_tensor(out=acc_out[:, n, :], in0=acc_out[:, n, :], scalar=vt[:, 6, n:n + 1], in1=x1tv[:, c * TT_:(c + 1) * TT_],
                                                                         op0=ALU.mult, op1=ALU.add), w=[acc_out], r=[acc_out, vt, x1t])
                k.out_dma("sp", ov[:, :, tsl], acc_out[:], r=[acc_out])
                barrier()
        k.finish()
    return nc


SEQ = 16384
_PROG = {}


def _prog(key, fn):
    if key not in _PROG:
        _PROG[key] = fn()
    return _PROG[key]


def kernel(**inp):
    P = {k_: np.asarray(v) for k_, v in inp.items()}
    T = SEQ
    tc = T // NCORES
    x = P["x"][0]
    ncols = 6 * D // NCORES
    mlf = P["mod_layer"].reshape(2, -1)
    res = run(_prog("mod", lambda: build_mod(ncols)),
              [{"ccol": colvec(P["c"][0]), "wm": np.ascontiguousarray(P["mod_w"][:, j * ncols:(j + 1) * ncols]),
                "ml": np.ascontiguousarray(mlf[:, j * ncols:(j + 1) * ncols])} for j in range(NCORES)])
    m = np.concatenate([r["out"] for r in res], axis=1).reshape(2, 6, D)
    xT = np.ascontiguousarray(x.T)
    consts = mix_consts()
    ident = np.eye(128, dtype=np.float32)
    for l in range(2):
        sh_a, sc_a, g_a, sh_f, sc_f, g_f = [m[l, i] for i in range(6)]
        res = run(_prog("pre_b", lambda: build_pre(tc, BF16)),
                  [{"xT": np.ascontiguousarray(xT[:, c * tc:(c + 1) * tc]), "wn": colvec(P["norm_mix_w"][l]), "scc": colvec(sc_a), "shc": colvec(sh_a)}
                   for c in range(NCORES)])
        hT = np.concatenate([r["hT"] for r in res], axis=1)
        res = run(_prog("mix", lambda: build_mix(T)), [mix_inputs(c, l, P, hT, consts) for c in range(NCORES)])
        del hT
        yT = np.empty((D, T), np.float32)
        for c in range(NCORES):
            ha, hb, hc = core_heads(c)
            yT[ha * 128:(ha + 1) * 128] = res[c]["yaT"]
            for i, h in enumerate(hb):
                yT[1024 + h * 128:1024 + (h + 1) * 128] = res[c]["yb"][:, i, :].T
            for i, h in enumerate(hc):
                yT[2560 + h * 128:2560 + (h + 1) * 128] = res[c]["yc"][:, i, :].T
        del res
        vec = np.zeros((128, 9, NCH), np.float32)
        vec[:, 0, :8] = colvec(P["branch_norm_a"][l]); vec[:, 1, :12] = colvec(P["branch_norm_c"][l])
        for i, v in ((2, g_a), (3, P["norm_ffn_w"][l]), (4, sc_f), (5, sh_f), (6, g_f)):
            vec[:, i, :] = colvec(v)
        keysT = np.ascontiguousarray(P["peer_sub_keys"][l].reshape(16, 128, 128).transpose(2, 0, 1))
        UT = np.ascontiguousarray(P["peer_u"][l].T)
        shared = {"vec": vec, "w_out": P["w_out"][l], "w_qry": P["peer_w_query"][l].reshape(D, 2048), "keysT": keysT, "UT": UT,
                  "V": P["peer_v"][l], "ident": ident}
        res = run(_prog("ffn", lambda: build_ffn(tc)),
                  [dict(shared, xT=np.ascontiguousarray(xT[:, c * tc:(c + 1) * tc]), yT=np.ascontiguousarray(yT[:, c * tc:(c + 1) * tc])) for c in range(NCORES)])
        del UT, shared, yT
        xT = np.concatenate([r["oT"] for r in res], axis=1)
        del res
    zeros = np.zeros(D, np.float32)
    res = run(_prog("pre_f", lambda: build_pre(tc, F32)),
              [{"xT": np.ascontiguousarray(xT[:, c * tc:(c + 1) * tc]), "wn": colvec(P["final_norm_w"]), "scc": colvec(zeros), "shc": colvec(zeros)}
               for c in range(NCORES)])
    oT = np.concatenate([r["hT"] for r in res], axis=1)
    return np.ascontiguousarray(oT.T)[None].astype(np.float32)
```

```python
import contextlib
import numpy as np
import concourse.bass as bass
import concourse.mybir as mybir
from concourse.bass_utils import run_bass_kernel_spmd

F32 = mybir.dt.float32
BF16 = mybir.dt.bfloat16
I32 = mybir.dt.int32
AF = mybir.ActivationFunctionType
ALU = mybir.AluOpType
AX = mybir.AxisListType

NCORES = 8
D = 4096
NCH = D // 128
EPS = 1e-6


class TT:
    def __init__(self, h, name):
        self.h = h; self.name = name; self.w = None; self.r = []
    def __getitem__(self, idx):
        return self.h[idx]


class TV:
    def __init__(self, parent, ap, name):
        self.p = parent; self.h = ap; self.name = name; self.excl = getattr(parent, "excl", False)
    def __getitem__(self, idx):
        return self.h[idx]
    @property
    def w(self): return self.p.w
    @w.setter
    def w(self, v): self.p.w = v
    @property
    def r(self): return self.p.r
    @r.setter
    def r(self, v): self.p.r = v


class KB:
    NDMA = 6
    def __init__(self, nc, stack):
        self.nc = nc; self.stack = stack
        self.eng = {"pe": nc.tensor, "act": nc.scalar, "dve": nc.vector, "pool": nc.gpsimd, "sp": nc.sync}
        self.sem = {}; self.cnt = {}; self.seen = {e: {} for e in self.eng}
        for e in self.eng:
            self.sem[e] = stack.enter_context(nc.semaphore("s_" + e)); self.cnt[e] = 0
        self.drr = {}
        for q in ("sp", "pool", "act"):
            for i in range(self.NDMA):
                k = "d_%s%d" % (q, i)
                self.sem[k] = stack.enter_context(nc.semaphore(k)); self.cnt[k] = 0
            self.drr[q] = 0
        self.ntile = 0
        self.last_out = []
    def sb(self, shape, dt=F32, name=None, stack=None):
        self.ntile += 1
        name = "%s_%d" % (name or "t", self.ntile)
        return TT((stack or self.stack).enter_context(self.nc.sbuf_tensor(name, list(shape), dt)), name)
    def ps(self, shape, dt=F32, name=None, stack=None):
        self.ntile += 1
        name = "%s_%d" % (name or "p", self.ntile)
        t = TT((stack or self.stack).enter_context(self.nc.psum_tensor(name, list(shape), dt)), name)
        t.excl = True
        return t
    def _wait(self, e, tok):
        if tok is None: return
        k, v = tok
        if self.seen[e].get(k, 0) >= v: return
        self.eng[e].wait_ge(self.sem[k], v)
        self.seen[e][k] = v
    def _deps(self, e, w, r):
        for t in r:
            self._wait(e, t.w)
            if getattr(t, "excl", False):
                for tok in t.r: self._wait(e, tok)
        for t in w:
            self._wait(e, t.w)
            for tok in t.r: self._wait(e, tok)
    def _mark(self, tok, w, r):
        for t in r:
            if t not in w: t.r.append(tok)
        for t in w:
            t.w = tok; t.r = []
    def op(self, e, fn, w=(), r=()):
        self._deps(e, w, r)
        ins = fn(self.eng[e])
        self.cnt[e] += 1
        ins.then_inc(self.sem[e], 1)
        tok = (e, self.cnt[e])
        self._mark(tok, w, r)
        return tok
    def dma(self, q, out, in_, w=(), r=(), **kw):
        i = self.drr[q]; self.drr[q] = (i + 1) % self.NDMA
        k = "d_%s%d" % (q, i)
        if self.cnt[k] > 0: self._wait(q, (k, self.cnt[k]))
        self._deps(q, w, r)
        ins = self.eng[q].dma_start(out=out, in_=in_, **kw)
        self.cnt[k] += 16
        ins.then_inc(self.sem[k], 16)
        tok = (k, self.cnt[k])
        self._mark(tok, w, r)
        return tok
    def out_dma(self, q, out, in_, r=(), **kw):
        tok = self.dma(q, out, in_, r=r, **kw)
        self.last_out.append(tok)
        return tok
    def finish(self, e="sp"):
        for t in self.last_out: self._wait(e, t)


def _new_nc():
    return bass.Bass("TRN2", target_bir_lowering=False)


def colvec(v):
    v = np.asarray(v, np.float32).reshape(-1, 128)
    return np.ascontiguousarray(v.T)


def build_mod(ncols):
    nc = _new_nc()
    ccol = nc.dram_tensor("ccol", [128, NCH], F32, kind="ExternalInput").ap()
    wm = nc.dram_tensor("wm", [D, ncols], F32, kind="ExternalInput").ap()
    ml = nc.dram_tensor("ml", [2, ncols], F32, kind="ExternalInput").ap()
    out = nc.dram_tensor("out", [2, ncols], F32, kind="ExternalOutput").ap()
    ng = ncols // 512
    with contextlib.ExitStack() as st:
        k = KB(nc, st)
        c_t = k.sb([128, NCH]); s_t = k.sb([128, NCH])
        k.dma("sp", c_t[:], ccol[:, :], w=[c_t])
        k.op("act", lambda e: e.activation(out=s_t[:], in_=c_t[:], func=AF.Silu), w=[s_t], r=[c_t])
        wb = [k.sb([128, ncols], name="wmb%d" % i) for i in range(3)]
        acc = [k.ps([1, 512], name="macc%d" % g) for g in range(ng)]
        for kc in range(NCH):
            b = wb[kc % 3]
            k.dma("sp" if kc % 2 == 0 else "pool", b[:], wm[kc * 128:(kc + 1) * 128, :], w=[b])
            for g in range(ng):
                k.op("pe", lambda e, g=g, b=b, kc=kc: e.matmul(acc[g][:], lhsT=s_t[:, kc:kc + 1], rhs=b[:, g * 512:(g + 1) * 512],
                                                      start=(kc == 0), stop=(kc == NCH - 1)), w=[acc[g]], r=[s_t, b])
        base = k.sb([1, ncols]); mlt = k.sb([1, 2, ncols]); res = k.sb([1, 2, ncols])
        k.dma("sp", mlt[:], ml.rearrange("(o l) n -> o l n", o=1), w=[mlt])
        for g in range(ng):
            k.op("act", lambda e, g=g: e.activation(out=base[:, g * 512:(g + 1) * 512], in_=acc[g][:], func=AF.Copy), w=[base], r=[acc[g]])
        for l in range(2):
            k.op("dve", lambda e, l=l: e.tensor_tensor(out=res[:, l, :], in0=mlt[:, l, :], in1=base[:], op=ALU.add), w=[res], r=[mlt, base])
        k.out_dma("sp", out.rearrange("(o l) n -> o l n", o=1), res[:], r=[res])
        k.finish()
    return nc


def emit_norm_mod(k, ones, src_tile, nchunks, tt, wmod, sh, dst_fn, ps_ssq, scratch, rs, dim, c0=0):
    for c in range(nchunks):
        sq = scratch[c % 2]
        k.op("act", lambda e, c=c, sq=sq: e.activation(out=sq[:], in_=src_tile[:, c0 + c, :], func=AF.Square), w=[sq], r=[src_tile])
        k.op("pe", lambda e, c=c, sq=sq: e.matmul(ps_ssq[:], lhsT=ones[:], rhs=sq[:], start=(c == 0), stop=(c == nchunks - 1)),
             w=[ps_ssq], r=[ones, sq])
    k.op("act", lambda e: e.activation(out=rs[:], in_=ps_ssq[:], func=AF.Sqrt, scale=1.0 / dim, bias=EPS), w=[rs], r=[ps_ssq])
    k.op("dve", lambda e: e.reciprocal(out=rs[:], in_=rs[:]), w=[rs], r=[rs])
    for c in range(nchunks):
        tmp = scratch[c % 2]
        k.op("dve", lambda e, c=c, tmp=tmp: e.scalar_tensor_tensor(out=tmp[:], in0=src_tile[:, c0 + c, :], scalar=wmod[:, c:c + 1], in1=rs[:],
                                                             op0=ALU.mult, op1=ALU.mult), w=[tmp], r=[src_tile, wmod, rs])
        ap, dt_ = dst_fn(c)
        if sh is None:
            k.op("act", lambda e, tmp=tmp, ap=ap: e.activation(out=ap, in_=tmp[:], func=AF.Copy), w=[dt_], r=[tmp])
        else:
            k.op("act", lambda e, c=c, tmp=tmp, ap=ap: e.activation(out=ap, in_=tmp[:], func=AF.Identity, bias=sh[:, c:c + 1], scale=1.0),
                 w=[dt_], r=[tmp, sh])


def build_pre(tc, out_dt):
    nc = _new_nc()
    xT = nc.dram_tensor("xT", [D, tc], F32, kind="ExternalInput").ap()
    wn = nc.dram_tensor("wn", [128, NCH], F32, kind="ExternalInput").ap()
    scc = nc.dram_tensor("scc", [128, NCH], F32, kind="ExternalInput").ap()
    shc = nc.dram_tensor("shc", [128, NCH], F32, kind="ExternalInput").ap()
    hT = nc.dram_tensor("hT", [D, tc], out_dt, kind="ExternalOutput").ap()
    TT_ = 512
    with contextlib.ExitStack() as st:
        k = KB(nc, st)
        ones = k.sb([128, 128]); k.op("dve", lambda e: e.memset(ones[:], 1.0), w=[ones])
        w_t = k.sb([128, NCH]); sc_t = k.sb([128, NCH]); sh_t = k.sb([128, NCH]); wmod = k.sb([128, NCH])
        k.dma("sp", w_t[:], wn[:, :], w=[w_t]); k.dma("sp", sc_t[:], scc[:, :], w=[sc_t]); k.dma("sp", sh_t[:], shc[:, :], w=[sh_t])
        k.op("dve", lambda e: e.scalar_tensor_tensor(out=wmod[:], in0=sc_t[:], scalar=1.0, in1=w_t[:], op0=ALU.add, op1=ALU.mult),
             w=[wmod], r=[sc_t, w_t])
        xt = [k.sb([128, NCH, TT_], name="xt%d" % i) for i in range(2)]
        ht = [k.sb([128, NCH, TT_], out_dt, name="ht%d" % i) for i in range(1)]
        scratch = [k.sb([128, TT_], name="sq%d" % i) for i in range(2)]
        rs = k.sb([128, TT_]); ps_ssq = k.ps([128, TT_])
        xv = xT.rearrange("(c p) t -> p c t", p=128); hv = hT.rearrange("(c p) t -> p c t", p=128)
        for it in range(tc // TT_):
            x_ = xt[it % 2]; h_ = ht[0]
            for half in range(2):
                cs = slice(half * 16, half * 16 + 16)
                k.dma("sp" if half == 0 else "pool", x_[:, cs, :], xv[:, cs, it * TT_:(it + 1) * TT_], w=[x_])
            emit_norm_mod(k, ones, x_, NCH, TT_, wmod, sh_t, lambda c, h_=h_: (h_[:, c, :], h_), ps_ssq, scratch, rs, float(D))
            for half in range(2):
                cs = slice(half * 16, half * 16 + 16)
                k.out_dma("sp" if half == 0 else "pool", hv[:, cs, it * TT_:(it + 1) * TT_], h_[:, cs, :], r=[h_])
        k.finish()
    return nc


def run(nc, in_maps):
    res = run_bass_kernel_spmd(nc, in_maps, core_ids=list(range(len(in_maps))))
    return res.results


ROPE = 64
ATT_SCALE = 192.0 ** -0.5
NEGBIG = -30000.0


def mix_consts():
    i = np.arange(128)
    c = {}
    c["ident"] = np.eye(128, dtype=np.float32)
    c["tri"] = (i[:, None] <= i[None, :]).astype(np.float32)
    c["negl"] = np.where(i[:, None] > i[None, :], 0.0, NEGBIG).astype(np.float32)
    c["negu"] = np.where(i[:, None] <= i[None, :], 0.0, NEGBIG).astype(np.float32)
    c["bd16"] = ((i[:, None] // 16) == (i[None, :] // 16)).astype(np.float32)
    for b in (16, 32, 64):
        off = (((i[:, None] // (2 * b)) == (i[None, :] // (2 * b))) & ((i[:, None] % (2 * b)) >= b) & ((i[None, :] % (2 * b)) < b)).astype(np.float32)
        c["off%d" % b] = off
        if b < 64:
            c["offT%d" % b] = np.ascontiguousarray(off.T)
    rm = np.zeros((64, 64), np.float32)
    for m in range(32):
        rm[m + 32, m] = -1.0
        rm[m, m + 32] = 1.0
    c["rot"] = rm
    inv = (10000.0 ** (-np.arange(0, 64, 2, dtype=np.float32) / 64.0)).astype(np.float32)
    c["invf"] = np.concatenate([inv, inv]).reshape(64, 1).astype(np.float32)
    p = np.arange(128)[:, None, None]; m = np.arange(4)[None, :, None]; q = np.arange(512)[None, None, :]
    c["amask"] = ((128 * m + p) <= q).astype(np.float32)
    return c


def build_mix(T, do_a=True, do_b=True, do_c=True):
    nc = _new_nc()
    BT = 512
    NB = T // BT
    def din(name, shape, dt=F32):
        return nc.dram_tensor(name, list(shape), dt, kind="ExternalInput").ap()
    hT = din("hT", [D, T], BF16)
    w_ab = din("w_ab", [D, 8 * 128]); w_c = din("w_c", [D, 12 * 128 + 64]); w_tm = din("w_tm", [D, 260])
    lru_p = din("lru_p", [128, 8]); lru_w = din("lru_w", [128, 256])
    dn_cw = din("dn_cw", [128, 24]); dn_hp = din("dn_hp", [128, 4]); dn_nw = din("dn_nw", [128, 128])
    qnw = din("qnw", [128, 8]); kvnw = din("kvnw", [128, 4])
    wq = din("wq", [128, 8, 384]); wkv = din("wkv", [128, 4, 512])
    pos = din("pos", [1, T], I32)
    c_ident = din("ident", [128, 128]); c_tri = din("tri", [128, 128]); c_negl = din("negl", [128, 128]); c_negu = din("negu", [128, 128])
    c_blk = {n: din(n, [128, 128]) for n in ("bd16", "off16", "off32", "off64", "offT16", "offT32")}
    c_rot = din("rot", [64, 64]); c_invf = din("invf", [64, 1]); c_amask = din("amask", [128, 4, 512])
    yaT = nc.dram_tensor("yaT", [128, T], F32, kind="ExternalOutput").ap()
    yb = nc.dram_tensor("yb", [T, 2, 128], F32, kind="ExternalOutput").ap()
    yc = nc.dram_tensor("yc", [T, 2, 128], F32, kind="ExternalOutput").ap()
    s_qn = [nc.dram_tensor("s_qn%d" % h, [128, T], BF16, kind="Internal").ap() for h in range(2)]
    s_qp = [nc.dram_tensor("s_qp%d" % h, [65, T], BF16, kind="Internal").ap() for h in range(2)]
    s_kn = [nc.dram_tensor("s_kn%d" % h, [128, T], BF16, kind="Internal").ap() for h in range(2)]
    s_kp = nc.dram_tensor("s_kp", [65, T], BF16, kind="Internal").ap()
    s_v = [nc.dram_tensor("s_v%d" % h, [128, T // 128, 129], BF16, kind="Internal").ap() for h in range(2)]
    s_km = nc.dram_tensor("s_km", [128, 2], F32, kind="Internal").ap()
    hv = hT.rearrange("(c p) t -> p c t", p=128)
    s_wab = nc.dram_tensor("s_wab", [8, 128, D], BF16, kind="Internal").ap()
    s_wc = nc.dram_tensor("s_wc", [13, 128, D], BF16, kind="Internal").ap()

    with contextlib.ExitStack() as st:
        k = KB(nc, st)
        with contextlib.ExitStack() as pc_:
            cv = [k.sb([128, D], BF16, stack=pc_, name="mcv%d" % i) for i in range(4)]
            wabv0 = w_ab.rearrange("(c p) n -> p c n", p=128); wcv0 = w_c.rearrange("(c p) n -> p c n", p=128)
            ci = 0
            for j in range(8):
                t_ = cv[ci % 4]; ci += 1
                k.dma("pool", t_.h[:].rearrange("p (c n) -> p c n", n=128), wabv0[:, :, j * 128:(j + 1) * 128], w=[t_])
                k.dma("sp", s_wab[j], t_[:], r=[t_])
            for j in range(13):
                wd = 128 if j < 12 else 64
                t_ = cv[ci % 4]; ci += 1
                if wd < 128:
                    k.op("dve", lambda e, t_=t_: e.memset(t_[:], 0.0), w=[t_])
                k.dma("pool", t_.h[:].rearrange("p (c n) -> p c n", n=128)[:, :, 0:wd], wcv0[:, :, j * 128:j * 128 + wd], w=[t_])
                k.dma("sp", s_wc[j], t_[:], r=[t_])
            for e_ in k.eng:
                for key in k.sem:
                    if k.cnt[key] > 0:
                        k._wait(e_, (key, k.cnt[key]))

        def barrier():
            for e in k.eng:
                for key in k.sem:
                    if k.cnt[key] > 0:
                        k._wait(e, (key, k.cnt[key]))

        def load_h(htq, b):
            for qd in range(4):
                k.dma("sp", htq[qd][:], hv[:, qd * 8:(qd + 1) * 8, b * BT:(b + 1) * BT], w=[htq[qd]])

        if do_a or do_b:
          with contextlib.ExitStack() as p1:
            ones = k.sb([128, 128], stack=p1); negones = k.sb([128, 128], stack=p1)
            k.op("dve", lambda e: e.memset(ones[:], 1.0), w=[ones]); k.op("dve", lambda e: e.memset(negones[:], -1.0), w=[negones])
            ident = k.sb([128, 128], stack=p1); tri = k.sb([128, 128], stack=p1); negl = k.sb([128, 128], stack=p1); negu = k.sb([128, 128], stack=p1)
            for t_, s_ in ((ident, c_ident), (tri, c_tri), (negl, c_negl), (negu, c_negu)):
                k.dma("sp", t_[:], s_[:, :], w=[t_])
            cm = {}
            for n_, ap_ in c_blk.items():
                cm[n_] = k.sb([128, 128], stack=p1, name="cm_" + n_)
                k.dma("sp", cm[n_][:], ap_[:, :], w=[cm[n_]])
            htq = [k.sb([128, 8, BT], BF16, stack=p1, name="htq%d" % i) for i in range(4)]
            wst = [k.sb([128, NCH, 128], BF16, stack=p1, name="wst%d" % i) for i in range(3)]
            wtm = k.sb([128, NCH, 260], BF16, stack=p1)
            for half in range(2):
                k.dma("pool", wtm[:, half * 16:(half + 1) * 16, :], w_tm.rearrange("(c p) n -> p c n", p=128)[:, half * 16:(half + 1) * 16, :], w=[wtm])
            big = [k.ps([128, BT], stack=p1, name="big%d" % i) for i in range(3)]
            smb = [k.ps([128, 512], stack=p1, name="smb%d" % i) for i in range(3)]
            small = [TV(smb[i % 3], smb[i % 3].h[:, (i // 3) * 128:(i // 3 + 1) * 128], "sm%d" % i) for i in range(12)]
            ptm = k.ps([128, 512], stack=p1, name="ptm")
            ctr = {"big": 0, "small": 0, "w": 0}
            def nbig():
                ctr["big"] += 1; return big[ctr["big"] % 3]
            def nsm():
                ctr["small"] += 1; return small[ctr["small"] % 12]
            wabv = w_ab.rearrange("(c p) n -> p c n", p=128)
            def p1_chunk(j):
                wt = wst[ctr["w"] % 3]; ctr["w"] += 1
                k.dma("sp", wt.h[:].rearrange("p c n -> p (c n)"), s_wab[j], w=[wt])
                ps = nbig()
                for fc in range(NCH):
                    k.op("pe", lambda e, fc=fc, ps=ps, wt=wt: e.matmul(ps[:], lhsT=wt[:, fc, :], rhs=htq[fc // 8][:, fc % 8, :],
                                                                       start=(fc == 0), stop=(fc == NCH - 1)), w=[ps], r=[wt, htq[fc // 8]])
                return ps
            def conv(dst, xbuf, cw, col0, bias_ap, eng="pool"):
                if bias_ap is None:
                    k.op(eng, lambda e: e.tensor_scalar(out=dst[:], in0=xbuf[:, 0:BT], scalar1=cw[:, col0:col0 + 1], scalar2=None, op0=ALU.mult), w=[dst], r=[xbuf, cw])
                else:
                    k.op(eng, lambda e: e.tensor_scalar(out=dst[:], in0=xbuf[:, 0:BT], scalar1=cw[:, col0:col0 + 1], scalar2=bias_ap, op0=ALU.mult, op1=ALU.add), w=[dst], r=[xbuf, cw])
                for j in range(1, 4):
                    k.op("dve", lambda e, j=j: e.scalar_tensor_tensor(out=dst[:], in0=xbuf[:, j:j + BT], scalar=cw[:, col0 + j:col0 + j + 1], in1=dst[:],
                                                                     op0=ALU.mult, op1=ALU.add), w=[dst], r=[xbuf, cw, dst])
                k.op("pool", lambda e: e.tensor_copy(out=xbuf[:, 0:3], in_=xbuf[:, BT:BT + 3]), w=[xbuf], r=[xbuf])

            lp = k.sb([128, 8], stack=p1); lw = k.sb([128, 256], stack=p1)
            k.dma("sp", lp[:], lru_p[:, :], w=[lp]); k.dma("sp", lw[:], lru_w[:, :], w=[lw])
            c1 = k.sb([128, 2], stack=p1)
            k.op("act", lambda e: e.activation(out=c1[:, 0:1], in_=lp[:, 7:8], func=AF.Exp, scale=-1.0), w=[c1], r=[lp])
            k.op("act", lambda e: e.activation(out=c1[:, 0:1], in_=c1[:, 0:1], func=AF.Ln, bias=1.0, scale=1.0), w=[c1], r=[c1])
            k.op("dve", lambda e: e.tensor_scalar(out=c1[:, 1:2], in0=c1[:, 0:1], scalar1=-16.0, scalar2=None, op0=ALU.mult), w=[c1], r=[c1])
            k.op("dve", lambda e: e.tensor_scalar(out=c1[:, 0:1], in0=c1[:, 0:1], scalar1=-8.0, scalar2=None, op0=ALU.mult), w=[c1], r=[c1])
            xa = k.sb([128, BT + 3], stack=p1); k.op("dve", lambda e: e.memset(xa[:], 0.0), w=[xa])
            hst = [k.sb([128, BT], stack=p1, name="hst%d" % i) for i in range(2)]
            k.op("dve", lambda e: e.memset(hst[1][:], 0.0), w=[hst[1]])
            A_t = {n: k.sb([128, BT], stack=p1, name="A_" + n) for n in ("u", "r", "i", "a", "b", "g")}
            dcw = k.sb([128, 24], stack=p1); dhp = k.sb([128, 4], stack=p1); dnw = k.sb([128, 128], stack=p1)
            k.dma("sp", dcw[:], dn_cw[:, :], w=[dcw]); k.dma("sp", dhp[:], dn_hp[:, :], w=[dhp]); k.dma("sp", dnw[:], dn_nw[:, :], w=[dnw])
            nega = k.sb([128, 2], stack=p1)
            k.op("act", lambda e: e.activation(out=nega[:], in_=dhp[:, 0:2], func=AF.Exp), w=[nega], r=[dhp])
            k.op("dve", lambda e: e.tensor_scalar(out=nega[:], in0=nega[:], scalar1=-1.0, scalar2=None, op0=ALU.mult), w=[nega], r=[nega])
            xb_ = [[k.sb([128, BT + 3], stack=p1, name="xb%d%d" % (h, i)) for i in range(3)] for h in range(2)]
            for h in range(2):
                for i in range(3):
                    k.op("pool", lambda e, h=h, i=i: e.memset(xb_[h][i][:], 0.0), w=[xb_[h][i]])
            S_ = [[k.sb([128, 128], stack=p1, name="S%d%d" % (h, i)) for i in range(2)] for h in range(2)]
            for h in range(2):
                k.op("pool", lambda e, h=h: e.memset(S_[h][0][:], 0.0), w=[S_[h][0]])
            B_t = {n: k.sb([128, BT], stack=p1, name="B_" + n) for n in ("qc", "kc", "vc", "sq", "rsq", "rsk", "qn", "kn")}
            sm_names = ["gt", "dl", "el", "du", "eu", "N", "NT", "qkT", "kbg", "ktail", "vb", "PTa", "PTb", "Qa", "QTa", "Qb", "QTb",
                        "u", "wT", "vnew", "o1", "o", "zs", "junk", "yout",
                        "N16", "No16", "No32", "No64", "NT16", "NoT16", "NoT32", "Xa", "Xb", "Z", "Zp"]
            Bs = {n: k.sb([128, 128], stack=p1, name="Bs_" + n) for n in sm_names}
            cols = {n: k.sb([128, 2], stack=p1, name="Bc_" + n) for n in ("beta", "nbeta", "g", "gc", "gl", "egc", "egl", "etail", "sp", "ssq", "rso")}
            tmsb = k.sb([128, 260], stack=p1)
            k.op("dve", lambda e: e.memset(cols["g"][:], 0.0), w=[cols["g"]])
            sblk = [0, 0]

            for b in range(NB):
                load_h(htq, b)
                if do_a:
                    ps = p1_chunk(0)
                    k.op("act", lambda e, ps=ps: e.activation(out=xa[:, 3:3 + BT], in_=ps[:], func=AF.Copy), w=[xa], r=[ps])
                    u = A_t["u"]
                    conv(u, xa, lp, 0, lp[:, 4:5])
                    for nm, woff, bcol in (("r", 0, 5), ("i", 128, 6)):
                        pg = nbig()
                        k.op("pe", lambda e, pg=pg, woff=woff: e.matmul(pg[:], lhsT=lw[:, woff:woff + 128], rhs=u[:], start=True, stop=True), w=[pg], r=[lw, u])
                        k.op("act", lambda e, pg=pg, nm=nm, bcol=bcol: e.activation(out=A_t[nm][:], in_=pg[:], func=AF.Sigmoid, bias=lp[:, bcol:bcol + 1], scale=1.0),
                             w=[A_t[nm]], r=[pg, lp])
                    k.op("act", lambda e: e.activation(out=A_t["a"][:], in_=A_t["r"][:], func=AF.Exp, scale=c1[:, 0:1]), w=[A_t["a"]], r=[A_t["r"], c1])
                    k.op("act", lambda e: e.activation(out=A_t["b"][:], in_=A_t["r"][:], func=AF.Exp, scale=c1[:, 1:2]), w=[A_t["b"]], r=[A_t["r"], c1])
                    k.op("dve", lambda e: e.tensor_scalar(out=A_t["b"][:], in0=A_t["b"][:], scalar1=-1.0, scalar2=1.0, op0=ALU.mult, op1=ALU.add), w=[A_t["b"]], r=[A_t["b"]])
                    k.op("dve", lambda e: e.tensor_scalar(out=A_t["b"][:], in0=A_t["b"][:], scalar1=1e-30, scalar2=None, op0=ALU.max), w=[A_t["b"]], r=[A_t["b"]])
                    k.op("act", lambda e: e.activation(out=A_t["b"][:], in_=A_t["b"][:], func=AF.Sqrt), w=[A_t["b"]], r=[A_t["b"]])
                    k.op("pool", lambda e: e.tensor_tensor(out=A_t["i"][:], in0=A_t["i"][:], in1=u[:], op=ALU.mult), w=[A_t["i"]], r=[A_t["i"], u])
                    k.op("pool", lambda e: e.tensor_tensor(out=A_t["b"][:], in0=A_t["b"][:], in1=A_t["i"][:], op=ALU.mult), w=[A_t["b"]], r=[A_t["b"], A_t["i"]])
                    hp_, hc_ = hst[(b + 1) % 2], hst[b % 2]
                    k.op("dve", lambda e, hp_=hp_, hc_=hc_: e.tensor_tensor_scan(out=hc_[:], data0=A_t["a"][:], data1=A_t["b"][:], initial=hp_[:, BT - 1:BT],
                                                                               op0=ALU.mult, op1=ALU.add), w=[hc_], r=[A_t["a"], A_t["b"], hp_])
                    ps = p1_chunk(1)
                    k.op("act", lambda e, ps=ps: e.activation(out=A_t["g"][:], in_=ps[:], func=AF.Gelu_apprx_tanh), w=[A_t["g"]], r=[ps])
                    k.op("pool", lambda e, hc_=hc_: e.tensor_tensor(out=A_t["g"][:], in0=A_t["g"][:], in1=hc_[:], op=ALU.mult), w=[A_t["g"]], r=[A_t["g"], hc_])
                    k.out_dma("sp", yaT[:, b * BT:(b + 1) * BT], A_t["g"][:], r=[A_t["g"]])
                if do_b:
                    for h in range(2):
                        dst3 = (B_t["qc"], B_t["kc"], B_t["vc"])
                        for i in range(3):
                            ps = p1_chunk(2 + 3 * h + i)
                            xbuf = xb_[h][i]
                            k.op("act", lambda e, ps=ps, xbuf=xbuf: e.activation(out=xbuf[:, 3:3 + BT], in_=ps[:], func=AF.Copy), w=[xbuf], r=[ps])
                            conv(dst3[i], xbuf, dcw, (h * 3 + i) * 4, None)
                            k.op("act", lambda e, d_=dst3[i]: e.activation(out=d_[:], in_=d_[:], func=AF.Silu), w=[dst3[i]], r=[dst3[i]])
                        for src, rs_, dstn, mul in ((B_t["qc"], B_t["rsq"], B_t["qn"], 128.0 ** -0.5), (B_t["kc"], B_t["rsk"], B_t["kn"], 1.0)):
                            k.op("act", lambda e, src=src: e.activation(out=B_t["sq"][:], in_=src[:], func=AF.Square), w=[B_t["sq"]], r=[src])
                            pg = nbig()
                            k.op("pe", lambda e, pg=pg: e.matmul(pg[:], lhsT=ones[:], rhs=B_t["sq"][:], start=True, stop=True), w=[pg], r=[ones, B_t["sq"]])
                            k.op("act", lambda e, pg=pg, rs_=rs_: e.activation(out=rs_[:], in_=pg[:], func=AF.Sqrt, bias=EPS, scale=1.0), w=[rs_], r=[pg])
                            k.op("dve", lambda e, rs_=rs_: e.reciprocal(out=rs_[:], in_=rs_[:]), w=[rs_], r=[rs_])
                            k.op("dve", lambda e, src=src, rs_=rs_, dstn=dstn, mul=mul: e.scalar_tensor_tensor(out=dstn[:], in0=src[:], scalar=mul, in1=rs_[:],
                                                                                                             op0=ALU.mult, op1=ALU.mult), w=[dstn], r=[src, rs_])
                        qn, kn, vc = B_t["qn"], B_t["kn"], B_t["vc"]
                        for s in range(4):
                            ts_ = slice(s * 128, (s + 1) * 128)
                            if h == 0:
                                pass
                            for fc in range(NCH):
                                k.op("pe", lambda e, fc=fc, ts_=ts_: e.matmul(ptm[:, 0:260], lhsT=htq[fc // 8][:, fc % 8, ts_], rhs=wtm[:, fc, :],
                                                                          start=(fc == 0), stop=(fc == NCH - 1)), w=[ptm], r=[wtm, htq[fc // 8]])
                            k.op("act", lambda e: e.activation(out=tmsb[:], in_=ptm[:, 0:260], func=AF.Copy), w=[tmsb], r=[ptm])
                            C = cols
                            k.op("act", lambda e, h=h: e.activation(out=C["beta"][:, 0:1], in_=tmsb[:, 256 + h:257 + h], func=AF.Sigmoid), w=[C["beta"]], r=[tmsb])
                            k.op("dve", lambda e: e.tensor_scalar(out=C["nbeta"][:, 0:1], in0=C["beta"][:, 0:1], scalar1=-1.0, scalar2=None, op0=ALU.mult), w=[C["nbeta"]], r=[C["beta"]])
                            k.op("act", lambda e, h=h: e.activation(out=C["sp"][:, 0:1], in_=tmsb[:, 258 + h:259 + h], func=AF.Exp, bias=dhp[:, 2 + h:3 + h], scale=1.0), w=[C["sp"]], r=[tmsb, dhp])
                            k.op("act", lambda e: e.activation(out=C["sp"][:, 0:1], in_=C["sp"][:, 0:1], func=AF.Ln, bias=1.0, scale=1.0), w=[C["sp"]], r=[C["sp"]])
                            k.op("dve", lambda e, h=h: e.tensor_scalar(out=C["g"][:, 0:1], in0=C["sp"][:, 0:1], scalar1=nega[:, h:h + 1], scalar2=None, op0=ALU.mult), w=[C["g"]], r=[C["sp"], nega])
                            g = C["g"]
                            k.op("dve", lambda e: e.tensor_scalar(out=Bs["gt"][:], in0=tri[:], scalar1=g[:, 0:1], scalar2=None, op0=ALU.mult), w=[Bs["gt"]], r=[tri, g])
                            pD = nsm()
                            k.op("pe", lambda e, pD=pD: e.matmul(pD[:], lhsT=Bs["gt"][:], rhs=ones[:], start=True, stop=False), w=[pD], r=[Bs["gt"], ones])
                            k.op("pe", lambda e, pD=pD: e.matmul(pD[:], lhsT=negones[:], rhs=Bs["gt"][:], start=False, stop=True), w=[pD], r=[Bs["gt"], negones])
                            pgc = nsm()
                            k.op("pe", lambda e, pgc=pgc: e.matmul(pgc[:, 0:2], lhsT=tri[:], rhs=g[:, 0:2], start=True, stop=True), w=[pgc], r=[tri, g])
                            k.op("pe", lambda e, pgc=pgc: e.matmul(pgc[:, 2:4], lhsT=ones[:], rhs=g[:, 0:2], start=True, stop=True), w=[pgc], r=[ones, g])
                            k.op("act", lambda e, pgc=pgc: e.activation(out=C["egc"][:, 0:1], in_=pgc[:, 0:1], func=AF.Exp), w=[C["egc"]], r=[pgc])
                            k.op("act", lambda e, pgc=pgc: e.activation(out=C["egl"][:, 0:1], in_=pgc[:, 2:3], func=AF.Exp), w=[C["egl"]], r=[pgc])
                            k.op("act", lambda e, pgc=pgc: e.activation(out=C["gl"][:, 0:1], in_=pgc[:, 2:3], func=AF.Copy), w=[C["gl"]], r=[pgc])
                            k.op("act", lambda e, pgc=pgc: e.activation(out=C["etail"][:, 0:1], in_=pgc[:, 0:1], func=AF.Exp, bias=C["gl"][:, 0:1], scale=-1.0), w=[C["etail"]], r=[pgc, C["gl"]])
                            k.op("dve", lambda e, pD=pD: e.tensor_tensor(out=Bs["dl"][:], in0=pD[:], in1=negl[:], op=ALU.add), w=[Bs["dl"]], r=[pD, negl])
                            k.op("act", lambda e: e.activation(out=Bs["el"][:], in_=Bs["dl"][:], func=AF.Exp), w=[Bs["el"]], r=[Bs["dl"]])
                            k.op("dve", lambda e, pD=pD: e.scalar_tensor_tensor(out=Bs["du"][:], in0=pD[:], scalar=-1.0, in1=negu[:], op0=ALU.mult, op1=ALU.add), w=[Bs["du"]], r=[pD, negu])
                            k.op("act", lambda e: e.activation(out=Bs["eu"][:], in_=Bs["du"][:], func=AF.Exp), w=[Bs["eu"]], r=[Bs["du"]])
                            pG = nsm()
                            k.op("pe", lambda e, pG=pG, ts_=ts_: e.matmul(pG[:], lhsT=kn[:, ts_], rhs=kn[:, ts_], start=True, stop=True), w=[pG], r=[kn])
                            k.op("dve", lambda e, pG=pG: e.scalar_tensor_tensor(out=Bs["N"][:], in0=pG[:], scalar=C["nbeta"][:, 0:1], in1=Bs["el"][:], op0=ALU.mult, op1=ALU.mult),
                                 w=[Bs["N"]], r=[pG, C["nbeta"], Bs["el"]])
                            pQ = nsm()
                            k.op("pe", lambda e, pQ=pQ, ts_=ts_: e.matmul(pQ[:], lhsT=kn[:, ts_], rhs=qn[:, ts_], start=True, stop=True), w=[pQ], r=[kn, qn])
                            k.op("dve", lambda e, pQ=pQ: e.tensor_tensor(out=Bs["qkT"][:], in0=pQ[:], in1=Bs["eu"][:], op=ALU.mult), w=[Bs["qkT"]], r=[pQ, Bs["eu"]])
                            pK = nsm()
                            k.op("pe", lambda e, pK=pK, ts_=ts_: e.matmul(pK[:], lhsT=kn[:, ts_], rhs=ident[:], start=True, stop=True), w=[pK], r=[kn, ident])
                            k.op("dve", lambda e, pK=pK: e.tensor_scalar(out=Bs["kbg"][:], in0=pK[:], scalar1=C["beta"][:, 0:1], scalar2=C["egc"][:, 0:1], op0=ALU.mult, op1=ALU.mult),
                                 w=[Bs["kbg"]], r=[pK, C["beta"], C["egc"]])
                            k.op("dve", lambda e, pK=pK: e.tensor_scalar(out=Bs["ktail"][:], in0=pK[:], scalar1=C["etail"][:, 0:1], scalar2=None, op0=ALU.mult), w=[Bs["ktail"]], r=[pK, C["etail"]])
                            pV = nsm()
                            k.op("pe", lambda e, pV=pV, ts_=ts_: e.matmul(pV[:], lhsT=vc[:, ts_], rhs=ident[:], start=True, stop=True), w=[pV], r=[vc, ident])
                            k.op("dve", lambda e, pV=pV: e.tensor_scalar(out=Bs["vb"][:], in0=pV[:], scalar1=C["beta"][:, 0:1], scalar2=None, op0=ALU.mult), w=[Bs["vb"]], r=[pV, C["beta"]])
                            N_ = Bs["N"]
                            k.op("pool", lambda e: e.tensor_tensor(out=Bs["N16"][:], in0=N_[:], in1=cm["bd16"][:], op=ALU.mult), w=[Bs["N16"]], r=[N_, cm["bd16"]])
                            for b_ in (16, 32, 64):
                                k.op("pool", lambda e, b_=b_: e.tensor_tensor(out=Bs["No%d" % b_][:], in0=N_[:], in1=cm["off%d" % b_][:], op=ALU.mult), w=[Bs["No%d" % b_]], r=[N_, cm["off%d" % b_]])
                            pT = nsm()
                            k.op("pe", lambda e, pT=pT: e.matmul(pT[:], lhsT=N_[:], rhs=ident[:], start=True, stop=True), w=[pT], r=[N_, ident])
                            k.op("dve", lambda e, pT=pT: e.tensor_tensor(out=Bs["NT16"][:], in0=pT[:], in1=cm["bd16"][:], op=ALU.mult), w=[Bs["NT16"]], r=[pT, cm["bd16"]])
                            for b_ in (16, 32):
                                k.op("dve", lambda e, pT=pT, b_=b_: e.tensor_tensor(out=Bs["NoT%d" % b_][:], in0=pT[:], in1=cm["offT%d" % b_][:], op=ALU.mult), w=[Bs["NoT%d" % b_]], r=[pT, cm["offT%d" % b_]])
                            Xc, Xn = Bs["Xa"], Bs["Xb"]; Yc, Yn = Bs["PTa"], Bs["PTb"]
                            k.op("pool", lambda e, Xc=Xc: e.tensor_tensor(out=Xc[:], in0=Bs["N16"][:], in1=ident[:], op=ALU.add), w=[Xc], r=[Bs["N16"], ident])
                            k.op("pool", lambda e, Yc=Yc: e.tensor_tensor(out=Yc[:], in0=Bs["NT16"][:], in1=ident[:], op=ALU.add), w=[Yc], r=[Bs["NT16"], ident])
                            Qc, QTc = Bs["N16"], Bs["NT16"]
                            for s_ in range(3):
                                Qn_, QTn_ = (Bs["Qa"], Bs["QTa"]) if s_ % 2 == 0 else (Bs["Qb"], Bs["QTb"])
                                p1_ = nsm()
                                k.op("pe", lambda e, p1_=p1_, Qc=Qc, QTc=QTc: e.matmul(p1_[:], lhsT=QTc[:], rhs=Qc[:], start=True, stop=True), w=[p1_], r=[Qc, QTc])
                                k.op("act", lambda e, p1_=p1_, Qn_=Qn_: e.activation(out=Qn_[:], in_=p1_[:], func=AF.Copy), w=[Qn_], r=[p1_])
                                if s_ < 2:
                                    p2_ = nsm()
                                    k.op("pe", lambda e, p2_=p2_, Qc=Qc, QTc=QTc: e.matmul(p2_[:], lhsT=Qc[:], rhs=QTc[:], start=True, stop=True), w=[p2_], r=[Qc, QTc])
                                    k.op("act", lambda e, p2_=p2_, QTn_=QTn_: e.activation(out=QTn_[:], in_=p2_[:], func=AF.Copy), w=[QTn_], r=[p2_])
                                pX = nsm()
                                k.op("pe", lambda e, pX=pX, Yc=Yc, Qn_=Qn_: e.matmul(pX[:], lhsT=Yc[:], rhs=Qn_[:], start=True, stop=True), w=[pX], r=[Yc, Qn_])
                                pY = nsm()
                                k.op("pe", lambda e, pY=pY, Yc=Yc, Qn_=Qn_: e.matmul(pY[:], lhsT=Qn_[:], rhs=Yc[:], start=True, stop=True), w=[pY], r=[Yc, Qn_])
                                k.op("dve", lambda e, pX=pX, Xc=Xc, Xn=Xn: e.tensor_tensor(out=Xn[:], in0=pX[:], in1=Xc[:], op=ALU.add), w=[Xn], r=[pX, Xc])
                                k.op("dve", lambda e, pY=pY, Yc=Yc, Yn=Yn: e.tensor_tensor(out=Yn[:], in0=pY[:], in1=Yc[:], op=ALU.add), w=[Yn], r=[pY, Yc])
                                Xc, Xn = Xn, Xc; Yc, Yn = Yn, Yc
                                Qc, QTc = Qn_, QTn_
                            for b_ in (16, 32, 64):
                                pZ = nsm()
                                k.op("pe", lambda e, pZ=pZ, Yc=Yc, b_=b_: e.matmul(pZ[:], lhsT=Bs["No%d" % b_][:], rhs=Yc[:], start=True, stop=True), w=[pZ], r=[Bs["No%d" % b_], Yc])
                                k.op("act", lambda e, pZ=pZ: e.activation(out=Bs["Z"][:], in_=pZ[:], func=AF.Copy), w=[Bs["Z"]], r=[pZ])
                                if b_ < 64:
                                    pZp = nsm()
                                    k.op("pe", lambda e, pZp=pZp, Xc=Xc, b_=b_: e.matmul(pZp[:], lhsT=Bs["NoT%d" % b_][:], rhs=Xc[:], start=True, stop=True), w=[pZp], r=[Bs["NoT%d" % b_], Xc])
                                    k.op("act", lambda e, pZp=pZp: e.activation(out=Bs["Zp"][:], in_=pZp[:], func=AF.Copy), w=[Bs["Zp"]], r=[pZp])
                                pY = nsm()
                                k.op("pe", lambda e, pY=pY, Xc=Xc: e.matmul(pY[:], lhsT=Xc[:], rhs=Bs["Z"][:], start=True, stop=True), w=[pY], r=[Xc, Bs["Z"]])
                                if b_ < 64:
                                    pX = nsm()
                                    k.op("pe", lambda e, pX=pX, Yc=Yc: e.matmul(pX[:], lhsT=Yc[:], rhs=Bs["Zp"][:], start=True, stop=True), w=[pX], r=[Yc, Bs["Zp"]])
                                k.op("dve", lambda e, pY=pY, Yc=Yc, Yn=Yn: e.tensor_tensor(out=Yn[:], in0=pY[:], in1=Yc[:], op=ALU.add), w=[Yn], r=[pY, Yc])
                                if b_ < 64:
                                    k.op("dve", lambda e, pX=pX, Xc=Xc, Xn=Xn: e.tensor_tensor(out=Xn[:], in0=pX[:], in1=Xc[:], op=ALU.add), w=[Xn], r=[pX, Xc])
                                    Xc, Xn = Xn, Xc
                                Yc, Yn = Yn, Yc
                            PTc = Yc
                            AiT = PTc
                            pU = nsm()
                            k.op("pe", lambda e, pU=pU, AiT=AiT: e.matmul(pU[:], lhsT=AiT[:], rhs=Bs["vb"][:], start=True, stop=True), w=[pU], r=[AiT, Bs["vb"]])
                            k.op("act", lambda e, pU=pU: e.activation(out=Bs["u"][:], in_=pU[:], func=AF.Copy), w=[Bs["u"]], r=[pU])
                            pW = nsm()
                            k.op("pe", lambda e, pW=pW, AiT=AiT: e.matmul(pW[:], lhsT=Bs["kbg"][:], rhs=AiT[:], start=True, stop=True), w=[pW], r=[AiT, Bs["kbg"]])
                            k.op("act", lambda e, pW=pW: e.activation(out=Bs["wT"][:], in_=pW[:], func=AF.Copy), w=[Bs["wT"]], r=[pW])
                            Sc = S_[h][sblk[h] % 2]; Sn = S_[h][(sblk[h] + 1) % 2]; sblk[h] += 1
                            pws = nsm()
                            k.op("pe", lambda e, pws=pws, Sc=Sc: e.matmul(pws[:], lhsT=Bs["wT"][:], rhs=Sc[:], start=True, stop=True), w=[pws], r=[Bs["wT"], Sc])
                            k.op("dve", lambda e, pws=pws: e.tensor_tensor(out=Bs["vnew"][:], in0=Bs["u"][:], in1=pws[:], op=ALU.subtract), w=[Bs["vnew"]], r=[Bs["u"], pws])
                            po1 = nsm()
                            k.op("pe", lambda e, po1=po1, Sc=Sc, ts_=ts_: e.matmul(po1[:], lhsT=qn[:, ts_], rhs=Sc[:], start=True, stop=True), w=[po1], r=[qn, Sc])
                            k.op("dve", lambda e, po1=po1: e.tensor_scalar(out=Bs["o1"][:], in0=po1[:], scalar1=C["egc"][:, 0:1], scalar2=None, op0=ALU.mult), w=[Bs["o1"]], r=[po1, C["egc"]])
                            po2 = nsm()
                            k.op("pe", lambda e, po2=po2: e.matmul(po2[:], lhsT=Bs["qkT"][:], rhs=Bs["vnew"][:], start=True, stop=True), w=[po2], r=[Bs["qkT"], Bs["vnew"]])
                            k.op("dve", lambda e, po2=po2: e.tensor_tensor(out=Bs["o"][:], in0=Bs["o1"][:], in1=po2[:], op=ALU.add), w=[Bs["o"]], r=[Bs["o1"], po2])
                            pS = nsm()
                            k.op("pe", lambda e, pS=pS: e.matmul(pS[:], lhsT=Bs["ktail"][:], rhs=Bs["vnew"][:], start=True, stop=True), w=[pS], r=[Bs["ktail"], Bs["vnew"]])
                            k.op("dve", lambda e, pS=pS, Sc=Sc, Sn=Sn: e.scalar_tensor_tensor(out=Sn[:], in0=Sc[:], scalar=C["egl"][:, 0:1], in1=pS[:], op0=ALU.mult, op1=ALU.add),
                                 w=[Sn], r=[Sc, C["egl"], pS])
                            k.op("act", lambda e: e.activation(out=Bs["junk"][:], in_=Bs["o"][:], func=AF.Square), w=[Bs["junk"]], r=[Bs["o"]])
                            k.op("dve", lambda e: e.reduce_sum(out=C["ssq"][:, 0:1], in_=Bs["junk"][:], axis=AX.X), w=[C["ssq"]], r=[Bs["junk"]])
                            k.op("act", lambda e: e.activation(out=C["rso"][:, 0:1], in_=C["ssq"][:, 0:1], func=AF.Sqrt, bias=EPS, scale=1.0 / 128), w=[C["rso"]], r=[C["ssq"]])
                            k.op("dve", lambda e: e.reciprocal(out=C["rso"][:, 0:1], in_=C["rso"][:, 0:1]), w=[C["rso"]], r=[C["rso"]])
                            k.op("act", lambda e, h=h: e.activation(out=Bs["zs"][:], in_=tmsb[:, h * 128:(h + 1) * 128], func=AF.Silu), w=[Bs["zs"]], r=[tmsb])
                            k.op("pool", lambda e: e.tensor_tensor(out=Bs["zs"][:], in0=Bs["zs"][:], in1=dnw[:], op=ALU.mult), w=[Bs["zs"]], r=[Bs["zs"], dnw])
                            k.op("dve", lambda e: e.scalar_tensor_tensor(out=Bs["yout"][:], in0=Bs["o"][:], scalar=C["rso"][:, 0:1], in1=Bs["zs"][:], op0=ALU.mult, op1=ALU.mult),
                                 w=[Bs["yout"]], r=[Bs["o"], C["rso"], Bs["zs"]])
                            k.out_dma("sp", yb[b * BT + s * 128:b * BT + (s + 1) * 128, h, :], Bs["yout"][:], r=[Bs["yout"]])
            barrier()

        if do_c:
          kmax = k.sb([128, 2], name="kmax"); nkm = k.sb([128, 2], name="nkm")
          k.op("dve", lambda e: e.memset(kmax[:], 0.0), w=[kmax])
          with contextlib.ExitStack() as p1:
            ones = k.sb([128, 128], stack=p1, name="c_ones")
            k.op("dve", lambda e: e.memset(ones[:], 1.0), w=[ones])
            htq = [k.sb([128, 8, BT], BF16, stack=p1, name="chtq%d" % i) for i in range(4)]
            wst = [k.sb([128, NCH, 128], BF16, stack=p1, name="cwst%d" % i) for i in range(3)]
            big = [k.ps([128, BT], stack=p1, name="cbig%d" % i) for i in range(4)]
            ps_ssq = k.ps([128, BT], stack=p1, name="c_ssq"); ps_ssk = k.ps([128, BT], stack=p1, name="c_ssk"); ps_ssp = k.ps([128, BT], stack=p1, name="c_ssp")
            ctr = {"big": 0, "w": 0}
            def nbig():
                ctr["big"] += 1; return big[ctr["big"] % 4]
            wcv = w_c.rearrange("(c p) n -> p c n", p=128)
            def pc_chunk(j, width=128):
                wt = wst[ctr["w"] % 3]; ctr["w"] += 1
                k.dma("sp", wt.h[:].rearrange("p c n -> p (c n)"), s_wc[j], w=[wt])
                ps = nbig()
                for fc in range(NCH):
                    k.op("pe", lambda e, fc=fc, ps=ps, wt=wt: e.matmul(ps[0:width, :], lhsT=wt[:, fc, 0:width], rhs=htq[fc // 8][:, fc % 8, :],
                                                                       start=(fc == 0), stop=(fc == NCH - 1)), w=[ps], r=[wt, htq[fc // 8]])
                return ps
            qnw_t = k.sb([128, 8], stack=p1); kvnw_t = k.sb([128, 4], stack=p1)
            k.dma("sp", qnw_t[:], qnw[:, :], w=[qnw_t]); k.dma("sp", kvnw_t[:], kvnw[:, :], w=[kvnw_t])
            wq_t = k.sb([128, 8, 384], BF16, stack=p1); wkv_t = k.sb([128, 4, 512], BF16, stack=p1)
            k.dma("pool", wq_t[:], wq[:, :, :], w=[wq_t]); k.dma("pool", wkv_t[:], wkv[:, :, :], w=[wkv_t])
            rot_t = k.sb([64, 64], stack=p1); invf_t = k.sb([64, 1], stack=p1)
            k.dma("sp", rot_t[:], c_rot[:, :], w=[rot_t]); k.dma("sp", invf_t[:], c_invf[:, :], w=[invf_t])
            cq = k.sb([128, 8, BT], stack=p1); ckv = k.sb([128, 4, BT], stack=p1); kr = k.sb([64, BT], stack=p1)
            qn_ = k.sb([128, 8, BT], BF16, stack=p1); kvn = k.sb([128, 4, BT], BF16, stack=p1)
            scratch = [k.sb([128, BT], stack=p1, name="csq%d" % i) for i in range(2)]
            rs = k.sb([128, BT], stack=p1)
            posi = k.sb([64, BT], I32, stack=p1)
            R = {n: k.sb([64, BT], stack=p1, name="R_" + n) for n in ("ang", "n", "r", "sin", "cos", "t1", "t2", "qpe")}
            qnope = k.sb([128, BT], BF16, stack=p1); knope = k.sb([128, BT], BF16, stack=p1)
            qpa = k.sb([65, BT], BF16, stack=p1); kpa = k.sb([65, BT], BF16, stack=p1)
            vaug = k.sb([128, 4, 129], BF16, stack=p1)
            k.op("dve", lambda e: e.memset(vaug[:], 1.0), w=[vaug])
            k.op("dve", lambda e: e.memset(kpa[64:65, :], 1.0), w=[kpa])
            bmax = k.sb([128, 1], stack=p1)
            TWO_PI = 6.283185307179586

            def rope(src, dst_ap, dst_t):
                pr = nbig()
                k.op("pe", lambda e: e.matmul(pr[0:64, :], lhsT=rot_t[:], rhs=src[:], start=True, stop=True), w=[pr], r=[rot_t, src])
                k.op("pool", lambda e: e.tensor_tensor(out=R["t1"][:], in0=src[:], in1=R["cos"][:], op=ALU.mult), w=[R["t1"]], r=[src, R["cos"]])
                k.op("dve", lambda e: e.tensor_tensor(out=R["t2"][:], in0=pr[0:64, :], in1=R["sin"][:], op=ALU.mult), w=[R["t2"]], r=[pr, R["sin"]])
                k.op("pool", lambda e: e.tensor_tensor(out=dst_ap, in0=R["t1"][:], in1=R["t2"][:], op=ALU.add), w=[dst_t], r=[R["t1"], R["t2"]])

            for b in range(NB):
                load_h(htq, b)
                bs = slice(b * BT, (b + 1) * BT)
                k.dma("sp", posi[:], pos[0:1, bs].broadcast_to([64, BT]), w=[posi])
                k.op("dve", lambda e: e.tensor_copy(out=R["ang"][:], in_=posi[:]), w=[R["ang"]], r=[posi])
                k.op("dve", lambda e: e.tensor_scalar(out=R["ang"][:], in0=R["ang"][:], scalar1=invf_t[:, 0:1], scalar2=None, op0=ALU.mult), w=[R["ang"]], r=[R["ang"], invf_t])
                k.op("dve", lambda e: e.tensor_scalar(out=R["n"][:], in0=R["ang"][:], scalar1=1.0 / TWO_PI, scalar2=12582912.0, op0=ALU.mult, op1=ALU.add), w=[R["n"]], r=[R["ang"]])
                k.op("dve", lambda e: e.tensor_scalar(out=R["n"][:], in0=R["n"][:], scalar1=-12582912.0, scalar2=None, op0=ALU.add), w=[R["n"]], r=[R["n"]])
                k.op("dve", lambda e: e.scalar_tensor_tensor(out=R["r"][:], in0=R["n"][:], scalar=-6.28125, in1=R["ang"][:], op0=ALU.mult, op1=ALU.add), w=[R["r"]], r=[R["n"], R["ang"]])
                k.op("dve", lambda e: e.scalar_tensor_tensor(out=R["r"][:], in0=R["n"][:], scalar=-(TWO_PI - 6.28125), in1=R["r"][:], op0=ALU.mult, op1=ALU.add), w=[R["r"]], r=[R["n"], R["r"]])
                k.op("dve", lambda e: e.tensor_scalar(out=R["r"][:], in0=R["r"][:], scalar1=3.14159, scalar2=-3.14159, op0=ALU.min, op1=ALU.max), w=[R["r"]], r=[R["r"]])
                k.op("act", lambda e: e.activation(out=R["sin"][:], in_=R["r"][:], func=AF.Sin), w=[R["sin"]], r=[R["r"]])
                k.op("dve", lambda e: e.tensor_scalar(out=R["t1"][:], in0=R["r"][:], scalar1=-1.0, scalar2=None, op0=ALU.mult), w=[R["t1"]], r=[R["r"]])
                k.op("dve", lambda e: e.tensor_tensor(out=R["t1"][:], in0=R["t1"][:], in1=R["r"][:], op=ALU.max), w=[R["t1"]], r=[R["t1"], R["r"]])
                k.op("dve", lambda e: e.tensor_scalar(out=R["t1"][:], in0=R["t1"][:], scalar1=-1.0, scalar2=1.5707963, op0=ALU.mult, op1=ALU.add), w=[R["t1"]], r=[R["t1"]])
                k.op("act", lambda e: e.activation(out=R["cos"][:], in_=R["t1"][:], func=AF.Sin), w=[R["cos"]], r=[R["t1"]])
                for j in range(8):
                    ps = pc_chunk(j)
                    k.op("act", lambda e, ps=ps, j=j: e.activation(out=cq[:, j, :], in_=ps[:], func=AF.Copy), w=[cq], r=[ps])
                for j in range(4):
                    ps = pc_chunk(8 + j)
                    k.op("act", lambda e, ps=ps, j=j: e.activation(out=ckv[:, j, :], in_=ps[:], func=AF.Copy), w=[ckv], r=[ps])
                ps = pc_chunk(12, 64)
                k.op("act", lambda e, ps=ps: e.activation(out=kr[:], in_=ps[0:64, :], func=AF.Copy), w=[kr], r=[ps])
                emit_norm_mod(k, ones, cq, 8, BT, qnw_t, None, lambda c: (qn_[:, c, :], qn_), ps_ssq, scratch, rs, 1024.0)
                emit_norm_mod(k, ones, ckv, 4, BT, kvnw_t, None, lambda c: (kvn[:, c, :], kvn), ps_ssq, scratch, rs, 512.0)
                rope(kr, R["qpe"][:], R["qpe"])
                k.op("act", lambda e: e.activation(out=kpa[0:64, :], in_=R["qpe"][:], func=AF.Copy), w=[kpa], r=[R["qpe"]])
                k.op("act", lambda e: e.activation(out=scratch[0][0:64, :], in_=R["qpe"][:], func=AF.Square), w=[scratch[0]], r=[R["qpe"]])
                k.op("pe", lambda e: e.matmul(ps_ssp[:], lhsT=ones[0:64, :], rhs=scratch[0][0:64, :], start=True, stop=True), w=[ps_ssp], r=[ones, scratch[0]])
                k.op("act", lambda e: e.activation(out=rs[:], in_=ps_ssp[:], func=AF.Copy), w=[rs], r=[ps_ssp])
                k.out_dma("sp", s_kp[:, bs], kpa[:], r=[kpa])
                for hh in range(2):
                    pq = nbig()
                    for rc in range(8):
                        k.op("pe", lambda e, rc=rc, pq=pq: e.matmul(pq[:], lhsT=wq_t[:, rc, hh * 192:hh * 192 + 128], rhs=qn_[:, rc, :], start=(rc == 0), stop=(rc == 7)), w=[pq], r=[wq_t, qn_])
                    k.op("act", lambda e, pq=pq: e.activation(out=qnope[:], in_=pq[:], func=AF.Copy), w=[qnope], r=[pq])
                    k.op("act", lambda e, pq=pq: e.activation(out=scratch[0][:], in_=pq[:], func=AF.Square), w=[scratch[0]], r=[pq])
                    k.op("pe", lambda e: e.matmul(ps_ssk[:], lhsT=ones[:], rhs=scratch[0][:], start=True, stop=False), w=[ps_ssk], r=[ones, scratch[0]])
                    pq2 = nbig()
                    for rc in range(8):
                        k.op("pe", lambda e, rc=rc, pq2=pq2: e.matmul(pq2[0:64, :], lhsT=wq_t[:, rc, hh * 192 + 128:hh * 192 + 192], rhs=qn_[:, rc, :], start=(rc == 0), stop=(rc == 7)), w=[pq2], r=[wq_t, qn_])
                    k.op("act", lambda e, pq2=pq2: e.activation(out=R["qpe"][:], in_=pq2[0:64, :], func=AF.Copy), w=[R["qpe"]], r=[pq2])
                    k.op("act", lambda e: e.activation(out=scratch[1][0:64, :], in_=R["qpe"][:], func=AF.Square), w=[scratch[1]], r=[R["qpe"]])
                    k.op("pe", lambda e: e.matmul(ps_ssk[:], lhsT=ones[0:64, :], rhs=scratch[1][0:64, :], start=False, stop=True), w=[ps_ssk], r=[ones, scratch[1]])
                    k.op("act", lambda e: e.activation(out=qpa[64:65, :], in_=ps_ssk[64:65, :], func=AF.Sqrt), w=[qpa], r=[ps_ssk])
                    rope(R["qpe"], qpa[0:64, :], qpa)
                    k.out_dma("sp", s_qn[hh][:, bs], qnope[:], r=[qnope])
                    k.out_dma("sp", s_qp[hh][:, bs], qpa[:], r=[qpa])
                    pk = nbig()
                    for rc in range(4):
                        k.op("pe", lambda e, rc=rc, pk=pk: e.matmul(pk[:], lhsT=wkv_t[:, rc, hh * 256:hh * 256 + 128], rhs=kvn[:, rc, :], start=(rc == 0), stop=(rc == 3)), w=[pk], r=[wkv_t, kvn])
                    k.op("act", lambda e, pk=pk: e.activation(out=knope[:], in_=pk[:], func=AF.Copy), w=[knope], r=[pk])
                    k.op("act", lambda e, pk=pk: e.activation(out=scratch[0][:], in_=pk[:], func=AF.Square), w=[scratch[0]], r=[pk])
                    k.op("pe", lambda e: e.matmul(ps_ssk[:], lhsT=ones[:], rhs=scratch[0][:], start=True, stop=True), w=[ps_ssk], r=[ones, scratch[0]])
                    k.op("dve", lambda e: e.tensor_tensor(out=scratch[1][:], in0=ps_ssk[:], in1=rs[:], op=ALU.add), w=[scratch[1]], r=[ps_ssk, rs])
                    k.op("dve", lambda e: e.reduce_max(out=bmax[:], in_=scratch[1][:], axis=AX.X), w=[bmax], r=[scratch[1]])
                    k.op("dve", lambda e, hh=hh: e.tensor_tensor(out=kmax[:, hh:hh + 1], in0=kmax[:, hh:hh + 1], in1=bmax[:], op=ALU.max), w=[kmax], r=[kmax, bmax])
                    k.out_dma("sp", s_kn[hh][:, bs], knope[:], r=[knope])
                    for sub in range(4):
                        pv = nbig()
                        for rc in range(4):
                            k.op("pe", lambda e, rc=rc, pv=pv, sub=sub: e.matmul(pv[:, 0:128], lhsT=kvn[:, rc, sub * 128:(sub + 1) * 128], rhs=wkv_t[:, rc, hh * 256 + 128:hh * 256 + 256],
                                                                        start=(rc == 0), stop=(rc == 3)), w=[pv], r=[wkv_t, kvn])
                        k.op("act", lambda e, pv=pv, sub=sub: e.activation(out=vaug[:, sub, 0:128], in_=pv[:, 0:128], func=AF.Copy), w=[vaug], r=[pv])
                    k.out_dma("sp", s_v[hh][:, 4 * b:4 * b + 4, :], vaug[:], r=[vaug])
            k.op("act", lambda e: e.activation(out=nkm[:], in_=kmax[:], func=AF.Sqrt), w=[nkm], r=[kmax])
            k.op("dve", lambda e: e.tensor_scalar(out=nkm[:], in0=nkm[:], scalar1=-1.0, scalar2=None, op0=ALU.mult), w=[nkm], r=[nkm])
            barrier()
          with contextlib.ExitStack() as p2:
            am = k.sb([128, 4, 512], BF16, stack=p2)
            k.dma("pool", am[:], c_amask[:, :, :], w=[am])
            kp_res = k.sb([65, T], BF16, stack=p2); kn_res = k.sb([128, T], BF16, stack=p2); v_res = k.sb([128, T // 128, 129], BF16, stack=p2)
            qn_g = [k.sb([128, 512], BF16, stack=p2, name="qn_g%d" % i) for i in range(2)]
            qp_g = [k.sb([65, 512], BF16, stack=p2, name="qp_g%d" % i) for i in range(2)]
            pt_ = [k.sb([128, 512], BF16, stack=p2, name="pt%d" % i) for i in range(3)]
            pss = [k.ps([128, 512], stack=p2, name="pss%d" % i) for i in range(2)]
            po = [k.ps([128, 512], stack=p2, name="po%d" % i) for i in range(4)]
            rden = k.sb([128, 4], stack=p2); osb = [k.sb([128, 128], stack=p2, name="osb%d" % i) for i in range(2)]
            k.dma("sp", kp_res[:], s_kp[:, :], w=[kp_res])
            it = 0
            for hh in range(2):
                k.dma("sp", kn_res[:], s_kn[hh][:, :], w=[kn_res])
                k.dma("sp", v_res[:], s_v[hh][:, :, :], w=[v_res])
                for g in range(T // 512):
                    qn = qn_g[g % 2]; qp = qp_g[g % 2]
                    k.dma("sp", qn[:], s_qn[hh][:, g * 512:(g + 1) * 512], w=[qn])
                    k.dma("sp", qp[:], s_qp[hh][:, g * 512:(g + 1) * 512], w=[qp])
                    k.op("dve", lambda e, qp=qp, hh=hh: e.tensor_scalar(out=qp[64:65, :], in0=qp[64:65, :], scalar1=nkm[64:65, hh:hh + 1], scalar2=None, op0=ALU.mult), w=[qp], r=[qp, nkm])
                    for kt in range(4 * g + 4):
                        ps = pss[it % 2]; pt = pt_[it % 3]; it += 1
                        ks = slice(kt * 128, (kt + 1) * 128)
                        k.op("pe", lambda e, ps=ps, ks=ks, qn=qn: e.matmul(ps[:], lhsT=kn_res[:, ks], rhs=qn[:], start=True, stop=False), w=[ps], r=[kn_res, qn])
                        k.op("pe", lambda e, ps=ps, ks=ks, qp=qp: e.matmul(ps[:], lhsT=kp_res[0:65, ks], rhs=qp[0:65, :], start=False, stop=True), w=[ps], r=[kp_res, qp])
                        k.op("act", lambda e, ps=ps, pt=pt: e.activation(out=pt[:], in_=ps[:], func=AF.Exp, scale=ATT_SCALE), w=[pt], r=[ps])
                        m = kt - 4 * g
                        if m >= 0:
                            k.op("pool", lambda e, pt=pt, m=m: e.tensor_tensor(out=pt[:], in0=pt[:], in1=am[:, m, :], op=ALU.mult), w=[pt], r=[pt, am])
                        for qs in range(4):
                            if m >= 0 and qs < m: continue
                            k.op("pe", lambda e, qs=qs, pt=pt, kt=kt: e.matmul(po[qs][:, 0:129], lhsT=pt[:, qs * 128:(qs + 1) * 128], rhs=v_res[:, kt, :],
                                                                         start=(kt == 0), stop=(kt == 4 * g + qs)), w=[po[qs]], r=[pt, v_res])
                    for qs in range(4):
                        ob = osb[qs % 2]
                        k.op("dve", lambda e, qs=qs: e.reciprocal(out=rden[:, qs:qs + 1], in_=po[qs][:, 128:129]), w=[rden], r=[po[qs]])
                        k.op("dve", lambda e, qs=qs, ob=ob: e.tensor_scalar(out=ob[:], in0=po[qs][:, 0:128], scalar1=rden[:, qs:qs + 1], scalar2=None, op0=ALU.mult), w=[ob], r=[po[qs], rden])
                        k.out_dma("sp", yc[g * 512 + qs * 128:g * 512 + (qs + 1) * 128, hh, :], ob[:], r=[ob])
            barrier()
        k.finish()
    return nc


OFF = {}
def _mk_off():
    names = ["a_rec", "a_gate", "b_q", "b_k", "b_v", "b_z", "b_beta", "b_alpha", "c_q", "c_kv", "c_kr"]
    widths = [1024, 1024, 1536, 1536, 1536, 1536, 12, 12, 1024, 512, 64]
    s = 0
    for n, w in zip(names, widths):
        OFF[n] = s; s += w
_mk_off()


def core_heads(c):
    return c, (c, 8 + c % 4), (c, 8 + c % 4)


def mix_inputs(c, l, P, hT_bf16, consts):
    ha, hb, hc = core_heads(c)
    w_in = P["w_in"][l]
    def cols(name, h, w=128):
        o = OFF[name] + h * w
        return w_in[:, o:o + w]
    w_ab = np.concatenate([cols("a_rec", ha), cols("a_gate", ha)] + [cols(n, h) for h in hb for n in ("b_q", "b_k", "b_v")], axis=1)
    w_tm = np.concatenate([cols("b_z", hb[0]), cols("b_z", hb[1]), cols("b_beta", hb[0], 1), cols("b_beta", hb[1], 1),
                           cols("b_alpha", hb[0], 1), cols("b_alpha", hb[1], 1)], axis=1)
    w_c = w_in[:, OFF["c_q"]:OFF["c_q"] + 1024 + 512 + 64]
    sl = slice(ha * 128, (ha + 1) * 128)
    lru_p = np.stack([P["lru_conv_w"][l][j, sl] for j in range(4)] + [P["lru_conv_b"][l][sl], P["lru_ba"][l][sl], P["lru_bx"][l][sl], P["lru_lambda"][l][sl]], axis=1)
    lru_w = np.concatenate([P["lru_wa"][l][ha], P["lru_wx"][l][ha]], axis=1)
    dn_cw = np.stack([P["dn_conv_w"][l][j, i * 1536 + h * 128:i * 1536 + (h + 1) * 128] for h in hb for i in range(3) for j in range(4)], axis=1)
    dn_hp = np.broadcast_to(np.array([P["dn_a_log"][l][hb[0]], P["dn_a_log"][l][hb[1]], P["dn_dt_bias"][l][hb[0]], P["dn_dt_bias"][l][hb[1]]], np.float32)[None, :], (128, 4))
    dn_nw = np.broadcast_to(P["dn_norm_w"][l][None, :], (128, 128))
    wq = P["mla_w_q_up"][l][:, list(hc), :].reshape(8, 128, 2 * 192).transpose(1, 0, 2)
    wkv = P["mla_w_kv_up"][l][:, list(hc), :].reshape(4, 128, 2 * 256).transpose(1, 0, 2)
    d = {"hT": hT_bf16, "w_ab": w_ab, "w_c": w_c, "w_tm": w_tm, "lru_p": lru_p, "lru_w": lru_w, "dn_cw": dn_cw, "dn_hp": dn_hp, "dn_nw": dn_nw,
         "qnw": colvec(P["mla_q_norm_w"][l]), "kvnw": colvec(P["mla_kv_norm_w"][l]), "wq": wq, "wkv": wkv, "pos": P["positions"].reshape(1, -1).astype(np.int32)}
    d.update(consts)
    return {kk: np.ascontiguousarray(v) for kk, v in d.items()}


NEGINF = -3.0e38


def build_ffn(tc):
    nc = _new_nc()
    TT_ = 256
    def din(name, shape, dt=F32):
        return nc.dram_tensor(name, list(shape), dt, kind="ExternalInput").ap()
    xT = din("xT", [D, tc]); yT = din("yT", [D, tc])
    vec = din("vec", [128, 9, NCH])
    w_out = din("w_out", [D, D]); w_qry = din("w_qry", [D, 2048]); keysT = din("keysT", [128, 16, 128])
    UT = din("UT", [D, 16384]); V = din("V", [16384, D]); c_ident = din("ident", [128, 128])
    oT = nc.dram_tensor("oT", [D, tc], F32, kind="ExternalOutput").ap()
    s_x1 = nc.dram_tensor("s_x1", [D, tc], F32, kind="Internal").ap()
    s_ut = nc.dram_tensor("s_ut", [128, 128, D], BF16, kind="Internal").ap()
    s_wo = nc.dram_tensor("s_wo", [NCH, 128, D], BF16, kind="Internal").ap()
    s_vb = nc.dram_tensor("s_vb", [16384, D], BF16, kind="Internal").ap()
    xv = xT.rearrange("(c p) t -> p c t", p=128); yv = yT.rearrange("(c p) t -> p c t", p=128)
    ov = oT.rearrange("(c p) t -> p c t", p=128); x1v = s_x1.rearrange("(c p) t -> p c t", p=128)
    wov = w_out.rearrange("(c p) n -> p c n", p=128); wqv = w_qry.rearrange("(c p) n -> p c n", p=128)
    utv = UT.rearrange("(c p) e -> p c e", p=128)
    with contextlib.ExitStack() as st:
        k = KB(nc, st)
        def barrier():
            for e in k.eng:
                for key in k.sem:
                    if k.cnt[key] > 0:
                        k._wait(e, (key, k.cnt[key]))
        ones = k.sb([128, 128]); k.op("dve", lambda e: e.memset(ones[:], 1.0), w=[ones])
        ident = k.sb([128, 128]); k.dma("sp", ident[:], c_ident[:, :], w=[ident])
        vt = k.sb([128, 9, NCH]); k.dma("sp", vt[:], vec[:, :, :], w=[vt])
        kT = k.sb([128, 16, 128]); k.dma("sp", kT[:], keysT[:, :, :], w=[kT])
        wmod = k.sb([128, NCH])
        k.op("dve", lambda e: e.scalar_tensor_tensor(out=wmod[:], in0=vt[:, 4, :], scalar=1.0, in1=vt[:, 3, :], op0=ALU.add, op1=ALU.mult), w=[wmod], r=[vt])
        bna = TV(vt, vt.h[:, 0, :], "bna"); bnc = TV(vt, vt.h[:, 1, :], "bnc"); shf = TV(vt, vt.h[:, 5, :], "shf")
        h2b = k.sb([128, NCH, TT_], BF16)
        qT = k.sb([128, 16, TT_])
        with contextlib.ExitStack() as pc_:
            cv = [k.sb([128, D], BF16, stack=pc_, name="cv%d" % i) for i in range(6)]
            ci = 0
            for n in range(NCH):
                t_ = cv[ci % 6]; ci += 1
                k.dma("pool", t_.h[:].rearrange("p (c n) -> p c n", n=128), wov[:, :, n * 128:(n + 1) * 128], w=[t_])
                k.dma("sp", s_wo[n], t_[:], r=[t_])
            for et in range(128):
                t_ = cv[ci % 6]; ci += 1
                k.dma("pool", t_.h[:].rearrange("p (c n) -> p c n", n=128), utv[:, :, et * 128:(et + 1) * 128], w=[t_])
                k.dma("sp", s_ut[et], t_[:], r=[t_])
                t_ = cv[ci % 6]; ci += 1
                k.dma("pool", t_[:], V[et * 128:(et + 1) * 128, :], w=[t_])
                k.dma("act", s_vb[et * 128:(et + 1) * 128, :], t_[:], r=[t_])
            barrier()
        for it in range(tc // TT_):
            tsl = slice(it * TT_, (it + 1) * TT_)
            with contextlib.ExitStack() as pa:
                yt = k.sb([128, NCH, TT_], stack=pa); xt = k.sb([128, NCH, TT_], stack=pa); ynb = k.sb([128, NCH, TT_], BF16, stack=pa)
                wst = [k.sb([128, NCH, 128], BF16, stack=pa, name="fw%d" % i) for i in range(3)]
                wqs = [k.sb([128, NCH, 128], stack=pa, name="fq%d" % i) for i in range(2)]
                scratch = [k.sb([128, TT_], stack=pa, name="fsq%d" % i) for i in range(2)]
                rs = k.sb([128, TT_], stack=pa); ps_ssq = k.ps([128, TT_], stack=pa)
                big = [k.ps([128, TT_], stack=pa, name="fbig%d" % i) for i in range(3)]
                for half in range(2):
                    cs = slice(half * 16, half * 16 + 16)
                    k.dma("sp", yt[:, cs, :], yv[:, cs, tsl], w=[yt]); k.dma("sp", xt[:, cs, :], xv[:, cs, tsl], w=[xt])
                emit_norm_mod(k, ones, yt, 8, TT_, bna, None, lambda c: (ynb[:, c, :], ynb), ps_ssq, scratch, rs, 1024.0, c0=0)
                emit_norm_mod(k, ones, yt, 12, TT_, bnc, None, lambda c: (ynb[:, 20 + c, :], ynb), ps_ssq, scratch, rs, 1536.0, c0=20)
                k.op("pool", lambda e: e.tensor_copy(out=ynb[:, 8:20, :], in_=yt[:, 8:20, :]), w=[ynb], r=[yt])
                for n in range(NCH):
                    wt = wst[n % 3]
                    k.dma("sp", wt.h[:].rearrange("p c n -> p (c n)"), s_wo[n], w=[wt])
                    ps = big[n % 3]
                    for fc in range(NCH):
                        k.op("pe", lambda e, fc=fc, ps=ps, wt=wt: e.matmul(ps[:], lhsT=wt[:, fc, :], rhs=ynb[:, fc, :], start=(fc == 0), stop=(fc == NCH - 1)), w=[ps], r=[wt, ynb])
                    k.op("dve", lambda e, n=n, ps=ps: e.scalar_tensor_tensor(out=xt[:, n, :], in0=ps[:], scalar=vt[:, 2, n:n + 1], in1=xt[:, n, :], op0=ALU.mult, op1=ALU.add),
                         w=[xt], r=[ps, vt, xt])
                k.dma("sp", x1v[:, :, tsl], xt[:], r=[xt])
                emit_norm_mod(k, ones, xt, NCH, TT_, wmod, shf, lambda c: (yt[:, c, :], yt), ps_ssq, scratch, rs, float(D))
                k.op("pool", lambda e: e.tensor_copy(out=h2b[:], in_=yt[:]), w=[h2b], r=[yt])
                for j in range(16):
                    wq_ = wqs[j % 2]
                    k.dma("sp", wq_[:], wqv[:, :, j * 128:(j + 1) * 128], w=[wq_])
                    ps = big[j % 3]
                    for fc in range(NCH):
                        k.op("pe", lambda e, fc=fc, ps=ps, wq_=wq_: e.matmul(ps[:], lhsT=wq_[:, fc, :], rhs=yt[:, fc, :], start=(fc == 0), stop=(fc == NCH - 1)), w=[ps], r=[wq_, yt])
                    k.op("act", lambda e, j=j, ps=ps: e.activation(out=qT[:, j, :], in_=ps[:], func=AF.Copy), w=[qT], r=[ps])
                barrier()
            with contextlib.ExitStack() as pb:
                NS = TT_ // 128
                sc = [k.sb([128, 16, 128], stack=pb, name="sc%d" % i) for i in range(NS)]
                tops = [k.sb([128, 16, 16], stack=pb, name="tops%d" % i) for i in range(NS)]
                tmp128 = k.sb([128, 128], stack=pb); cand = k.sb([128, 16, 16], stack=pb); cand2 = k.sb([128, 256], stack=pb)
                best = k.sb([128, 16], stack=pb); e16 = k.sb([128, 16], stack=pb)
                thr = [k.sb([128, 8], stack=pb, name="thr%d" % i) for i in range(NS)]
                negm = [k.sb([128, 8], stack=pb, name="negm%d" % i) for i in range(NS)]
                rZ = [k.sb([128, 8], stack=pb, name="rZ%d" % i) for i in range(NS)]
                gacc = [k.sb([128, 16, 128], stack=pb, name="gacc%d" % i) for i in range(NS)]
                S_ = k.sb([128, 16, 128], stack=pb); E_ = k.sb([128, 16, 128], stack=pb); M_ = k.sb([128, 16, 128], stack=pb)
                ust = [k.sb([128, NCH, 128], BF16, stack=pb, name="ust%d" % i) for i in range(2)]
                vg = [k.sb([128, D], BF16, stack=pb, name="vg%d" % i) for i in range(4)]
                actT = k.sb([128, TT_], stack=pb); coefT = [k.sb([128, TT_], BF16, stack=pb, name="coefT%d" % i) for i in range(4)]
                acc_out = k.sb([128, NCH, TT_], stack=pb)
                psm = [k.ps([128, 512], stack=pb, name="fpsm%d" % i) for i in range(2)]
                pu = [k.ps([128, TT_], stack=pb, name="fpu%d" % i) for i in range(2)]
                pg = k.ps([128, TT_], stack=pb, name="fpg"); pv = [k.ps([128, TT_], stack=pb, name="fpv%d" % i) for i in range(2)]
                for sb_ in range(NS):
                    ssl = slice(sb_ * 128, (sb_ + 1) * 128)
                    for hp in range(16):
                        ps = psm[hp % 2]
                        k.op("pe", lambda e, hp=hp, ps=ps: e.matmul(ps[:, 0:128], lhsT=qT[:, hp, ssl], rhs=kT[:, hp, :], start=True, stop=True), w=[ps], r=[qT, kT])
                        k.op("act", lambda e, hp=hp, ps=ps: e.activation(out=sc[sb_][:, hp, :], in_=ps[:, 0:128], func=AF.Copy), w=[sc[sb_]], r=[ps])
                        k.op("dve", lambda e, hp=hp: e.max(out=tops[sb_][:, hp, 0:8], in_=sc[sb_][:, hp, :]), w=[tops[sb_]], r=[sc[sb_]])
                        k.op("dve", lambda e, hp=hp: e.match_replace(out=tmp128[:], in_to_replace=tops[sb_][:, hp, 0:8], in_values=sc[sb_][:, hp, :], imm_value=NEGINF),
                             w=[tmp128], r=[tops[sb_], sc[sb_]])
                        k.op("dve", lambda e, hp=hp: e.max(out=tops[sb_][:, hp, 8:16], in_=tmp128[:]), w=[tops[sb_]], r=[tmp128])
                    for h in range(8):
                        for a in range(16):
                            k.op("dve", lambda e, a=a, h=h: e.tensor_scalar(out=cand[:, a, :], in0=tops[sb_][:, 2 * h + 1, :], scalar1=tops[sb_][:, 2 * h, a:a + 1], scalar2=None, op0=ALU.add),
                                 w=[cand], r=[tops[sb_]])
                        cf = cand.h[:].rearrange("p a b -> p (a b)")
                        k.op("dve", lambda e: e.max(out=best[:, 0:8], in_=cf), w=[best], r=[cand])
                        k.op("dve", lambda e: e.match_replace(out=cand2[:], in_to_replace=best[:, 0:8], in_values=cf, imm_value=NEGINF), w=[cand2], r=[best, cand])
                        k.op("dve", lambda e: e.max(out=best[:, 8:16], in_=cand2[:]), w=[best], r=[cand2])
                        k.op("dve", lambda e, h=h: e.tensor_copy(out=thr[sb_][:, h:h + 1], in_=best[:, 15:16]), w=[thr[sb_]], r=[best])
                        k.op("dve", lambda e, h=h: e.tensor_scalar(out=negm[sb_][:, h:h + 1], in0=best[:, 0:1], scalar1=-1.0, scalar2=None, op0=ALU.mult), w=[negm[sb_]], r=[best])
                        k.op("act", lambda e, h=h: e.activation(out=e16[:], in_=best[:], func=AF.Exp, bias=negm[sb_][:, h:h + 1], scale=1.0), w=[e16], r=[best, negm[sb_]])
                        k.op("dve", lambda e, h=h: e.reduce_sum(out=rZ[sb_][:, h:h + 1], in_=e16[:], axis=AX.X), w=[rZ[sb_]], r=[e16])
                        k.op("dve", lambda e, h=h: e.reciprocal(out=rZ[sb_][:, h:h + 1], in_=rZ[sb_][:, h:h + 1]), w=[rZ[sb_]], r=[rZ[sb_]])
                gi = 0
                for blk in range(8):
                    for sb_ in range(NS):
                        for h in range(8):
                            for a in range(16):
                                i1 = blk * 16 + a
                                if a % 2 == 0:
                                    k.op("dve", lambda e, a=a, i1=i1, h=h: e.tensor_scalar(out=S_[:, a, :], in0=sc[sb_][:, 2 * h + 1, :], scalar1=sc[sb_][:, 2 * h, i1:i1 + 1],
                                                                                      scalar2=None, op0=ALU.add), w=[S_], r=[sc[sb_]])
                                else:
                                    k.op("pool", lambda e, a=a, i1=i1, h=h: e.tensor_scalar(out=S_[:, a, :], in0=sc[sb_][:, 2 * h + 1, :], scalar1=sc[sb_][:, 2 * h, i1:i1 + 1],
                                                                                       scalar2=None, op0=ALU.add), w=[S_], r=[sc[sb_]])
                            k.op("act", lambda e, h=h: e.activation(out=E_[:], in_=S_[:], func=AF.Exp, bias=negm[sb_][:, h:h + 1], scale=1.0), w=[E_], r=[S_, negm[sb_]])
                            k.op("dve", lambda e, h=h: e.scalar_tensor_tensor(out=M_[:], in0=S_[:], scalar=thr[sb_][:, h:h + 1], in1=E_[:], op0=ALU.is_ge, op1=ALU.mult), w=[M_], r=[S_, thr[sb_], E_])
                            if h == 0:
                                k.op("dve", lambda e, h=h: e.tensor_scalar(out=gacc[sb_][:], in0=M_[:], scalar1=rZ[sb_][:, h:h + 1], scalar2=None, op0=ALU.mult), w=[gacc[sb_]], r=[M_, rZ[sb_]])
                            else:
                                k.op("dve", lambda e, h=h: e.scalar_tensor_tensor(out=gacc[sb_][:], in0=M_[:], scalar=rZ[sb_][:, h:h + 1], in1=gacc[sb_][:], op0=ALU.mult, op1=ALU.add),
                                     w=[gacc[sb_]], r=[M_, rZ[sb_], gacc[sb_]])
                    for grp in range(4):
                        for ei in range(4):
                            a = grp * 4 + ei; et = blk * 16 + a
                            us = ust[et % 2]
                            k.dma("sp", us.h[:].rearrange("p c n -> p (c n)"), s_ut[et], w=[us])
                            k.dma("sp", vg[ei][:], s_vb[et * 128:(et + 1) * 128, :], w=[vg[ei]])
                            p_u = pu[et % 2]
                            for fc in range(NCH):
                                k.op("pe", lambda e, fc=fc, p_u=p_u, us=us: e.matmul(p_u[:], lhsT=us[:, fc, :], rhs=h2b[:, fc, :], start=(fc == 0), stop=(fc == NCH - 1)), w=[p_u], r=[us, h2b])
                            k.op("act", lambda e, p_u=p_u: e.activation(out=actT[:], in_=p_u[:], func=AF.Gelu_apprx_tanh), w=[actT], r=[p_u])
                            for sb_ in range(NS):
                                k.op("pe", lambda e, sb_=sb_, a=a: e.matmul(pg[:, sb_ * 128:(sb_ + 1) * 128], lhsT=gacc[sb_][:, a, :], rhs=ident[:], start=True, stop=True), w=[pg], r=[gacc[sb_], ident])
                            k.op("dve", lambda e, ei=ei: e.tensor_tensor(out=coefT[ei][:], in0=pg[:], in1=actT[:], op=ALU.mult), w=[coefT[ei]], r=[pg, actT])
                        for n in range(NCH):
                            p_v = pv[n % 2]
                            for ei in range(4):
                                k.op("pe", lambda e, ei=ei, n=n, p_v=p_v: e.matmul(p_v[:], lhsT=vg[ei][:, n * 128:(n + 1) * 128], rhs=coefT[ei][:], start=(ei == 0), stop=(ei == 3)), w=[p_v], r=[vg[ei], coefT[ei]])
                            if gi == 0:
                                k.op("act", lambda e, n=n, p_v=p_v: e.activation(out=acc_out[:, n, :], in_=p_v[:], func=AF.Copy), w=[acc_out], r=[p_v])
                            else:
                                k.op("dve", lambda e, n=n, p_v=p_v: e.tensor_tensor(out=acc_out[:, n, :], in0=p_v[:], in1=acc_out[:, n, :], op=ALU.add), w=[acc_out], r=[p_v, acc_out])
                        gi += 1
                x1t = S_
                x1tv = x1t.h[:].rearrange("p a b -> p (a b)")
                for q4 in range(4):
                    cs = slice(q4 * 8, q4 * 8 + 8)
                    k.dma("sp", x1tv.rearrange("p (c t) -> p c t", t=TT_), x1v[:, cs, tsl], w=[x1t])
                    for c in range(8):
                        n = q4 * 8 + c
                        k.op("dve", lambda e, n=n, c=c: e.scalar_tensor_tensor(out=acc_out[:, n, :], in0=acc_out[:, n, :], scalar=vt[:, 6, n:n + 1], in1=x1tv[:, c * TT_:(c + 1) * TT_],
                                                                         op0=ALU.mult, op1=ALU.add), w=[acc_out], r=[acc_out, vt, x1t])
                k.out_dma("sp", ov[:, :, tsl], acc_out[:], r=[acc_out])
                barrier()
        k.finish()
    return nc


SEQ = 16384
_PROG = {}


def _prog(key, fn):
    if key not in _PROG:
        _PROG[key] = fn()
    return _PROG[key]


def kernel(**inp):
    P = {k_: np.asarray(v) for k_, v in inp.items()}
    T = SEQ
    tc = T // NCORES
    x = P["x"][0]
    ncols = 6 * D // NCORES
    mlf = P["mod_layer"].reshape(2, -1)
    res = run(_prog("mod", lambda: build_mod(ncols)),
              [{"ccol": colvec(P["c"][0]), "wm": np.ascontiguousarray(P["mod_w"][:, j * ncols:(j + 1) * ncols]),
                "ml": np.ascontiguousarray(mlf[:, j * ncols:(j + 1) * ncols])} for j in range(NCORES)])
    m = np.concatenate([r["out"] for r in res], axis=1).reshape(2, 6, D)
    xT = np.ascontiguousarray(x.T)
    consts = mix_consts()
    ident = np.eye(128, dtype=np.float32)
    for l in range(2):
        sh_a, sc_a, g_a, sh_f, sc_f, g_f = [m[l, i] for i in range(6)]
        res = run(_prog("pre_b", lambda: build_pre(tc, BF16)),
                  [{"xT": np.ascontiguousarray(xT[:, c * tc:(c + 1) * tc]), "wn": colvec(P["norm_mix_w"][l]), "scc": colvec(sc_a), "shc": colvec(sh_a)}
                   for c in range(NCORES)])
        hT = np.concatenate([r["hT"] for r in res], axis=1)
        res = run(_prog("mix", lambda: build_mix(T)), [mix_inputs(c, l, P, hT, consts) for c in range(NCORES)])
        del hT
        yT = np.empty((D, T), np.float32)
        for c in range(NCORES):
            ha, hb, hc = core_heads(c)
            yT[ha * 128:(ha + 1) * 128] = res[c]["yaT"]
            for i, h in enumerate(hb):
                yT[1024 + h * 128:1024 + (h + 1) * 128] = res[c]["yb"][:, i, :].T
            for i, h in enumerate(hc):
                yT[2560 + h * 128:2560 + (h + 1) * 128] = res[c]["yc"][:, i, :].T
        del res
        vec = np.zeros((128, 9, NCH), np.float32)
        vec[:, 0, :8] = colvec(P["branch_norm_a"][l]); vec[:, 1, :12] = colvec(P["branch_norm_c"][l])
        for i, v in ((2, g_a), (3, P["norm_ffn_w"][l]), (4, sc_f), (5, sh_f), (6, g_f)):
            vec[:, i, :] = colvec(v)
        keysT = np.ascontiguousarray(P["peer_sub_keys"][l].reshape(16, 128, 128).transpose(2, 0, 1))
        UT = np.ascontiguousarray(P["peer_u"][l].T)
        shared = {"vec": vec, "w_out": P["w_out"][l], "w_qry": P["peer_w_query"][l].reshape(D, 2048), "keysT": keysT, "UT": UT,
                  "V": P["peer_v"][l], "ident": ident}
        res = run(_prog("ffn", lambda: build_ffn(tc)),
                  [dict(shared, xT=np.ascontiguousarray(xT[:, c * tc:(c + 1) * tc]), yT=np.ascontiguousarray(yT[:, c * tc:(c + 1) * tc])) for c in range(NCORES)])
        del UT, shared, yT
        xT = np.concatenate([r["oT"] for r in res], axis=1)
        del res
    zeros = np.zeros(D, np.float32)
    res = run(_prog("pre_f", lambda: build_pre(tc, F32)),
              [{"xT": np.ascontiguousarray(xT[:, c * tc:(c + 1) * tc]), "wn": colvec(P["final_norm_w"]), "scc": colvec(zeros), "shc": colvec(zeros)}
               for c in range(NCORES)])
    oT = np.concatenate([r["hT"] for r in res], axis=1)
    return np.ascontiguousarray(oT.T)[None].astype(np.float32)
```

```python
import contextlib
import numpy as np
import concourse.bass as bass
import concourse.mybir as mybir
from concourse.bass_utils import run_bass_kernel_spmd

F32 = mybir.dt.float32
BF16 = mybir.dt.bfloat16
I32 = mybir.dt.int32
AF = mybir.ActivationFunctionType
ALU = mybir.AluOpType
AX = mybir.AxisListType

NCORES = 8
D = 4096
NCH = D // 128
EPS = 1e-6


class TT:
    def __init__(self, h, name):
        self.h = h; self.name = name; self.w = None; self.r = []
    def __getitem__(self, idx):
        return self.h[idx]


class TV:
    def __init__(self, parent, ap, name):
        self.p = parent; self.h = ap; self.name = name; self.excl = getattr(parent, "excl", False)
    def __getitem__(self, idx):
        return self.h[idx]
    @property
    def w(self): return self.p.w
    @w.setter
    def w(self, v): self.p.w = v
    @property
    def r(self): return self.p.r
    @r.setter
    def r(self, v): self.p.r = v


class KB:
    NDMA = 6
    def __init__(self, nc, stack):
        self.nc = nc; self.stack = stack
        self.eng = {"pe": nc.tensor, "act": nc.scalar, "dve": nc.vector, "pool": nc.gpsimd, "sp": nc.sync}
        self.sem = {}; self.cnt = {}; self.seen = {e: {} for e in self.eng}
        for e in self.eng:
            self.sem[e] = stack.enter_context(nc.semaphore("s_" + e)); self.cnt[e] = 0
        self.drr = {}
        for q in ("sp", "pool", "act"):
            for i in range(self.NDMA):
                k = "d_%s%d" % (q, i)
                self.sem[k] = stack.enter_context(nc.semaphore(k)); self.cnt[k] = 0
            self.drr[q] = 0
        self.ntile = 0
        self.last_out = []
    def sb(self, shape, dt=F32, name=None, stack=None):
        self.ntile += 1
        name = "%s_%d" % (name or "t", self.ntile)
        return TT((stack or self.stack).enter_context(self.nc.sbuf_tensor(name, list(shape), dt)), name)
    def ps(self, shape, dt=F32, name=None, stack=None):
        self.ntile += 1
        name = "%s_%d" % (name or "p", self.ntile)
        t = TT((stack or self.stack).enter_context(self.nc.psum_tensor(name, list(shape), dt)), name)
        t.excl = True
        return t
    def _wait(self, e, tok):
        if tok is None: return
        k, v = tok
        if self.seen[e].get(k, 0) >= v: return
        self.eng[e].wait_ge(self.sem[k], v)
        self.seen[e][k] = v
    def _deps(self, e, w, r):
        for t in r:
            self._wait(e, t.w)
            if getattr(t, "excl", False):
                for tok in t.r: self._wait(e, tok)
        for t in w:
            self._wait(e, t.w)
            for tok in t.r: self._wait(e, tok)
    def _mark(self, tok, w, r):
        for t in r:
            if t not in w: t.r.append(tok)
        for t in w:
            t.w = tok; t.r = []
    def op(self, e, fn, w=(), r=()):
        self._deps(e, w, r)
        ins = fn(self.eng[e])
        self.cnt[e] += 1
        ins.then_inc(self.sem[e], 1)
        tok = (e, self.cnt[e])
        self._mark(tok, w, r)
        return tok
    def dma(self, q, out, in_, w=(), r=(), **kw):
        i = self.drr[q]; self.drr[q] = (i + 1) % self.NDMA
        k = "d_%s%d" % (q, i)
        if self.cnt[k] > 0: self._wait(q, (k, self.cnt[k]))
        self._deps(q, w, r)
        ins = self.eng[q].dma_start(out=out, in_=in_, **kw)
        self.cnt[k] += 16
        ins.then_inc(self.sem[k], 16)
        tok = (k, self.cnt[k])
        self._mark(tok, w, r)
        return tok
    def out_dma(self, q, out, in_, r=(), **kw):
        tok = self.dma(q, out, in_, r=r, **kw)
        self.last_out.append(tok)
        return tok
    def finish(self, e="sp"):
        for t in self.last_out: self._wait(e, t)


def _new_nc():
    return bass.Bass("TRN2", target_bir_lowering=False)


def colvec(v):
    v = np.asarray(v, np.float32).reshape(-1, 128)
    return np.ascontiguousarray(v.T)


def build_mod(ncols):
    nc = _new_nc()
    ccol = nc.dram_tensor("ccol", [128, NCH], F32, kind="ExternalInput").ap()
    wm = nc.dram_tensor("wm", [D, ncols], F32, kind="ExternalInput").ap()
    ml = nc.dram_tensor("ml", [2, ncols], F32, kind="ExternalInput").ap()
    out = nc.dram_tensor("out", [2, ncols], F32, kind="ExternalOutput").ap()
    ng = ncols // 512
    with contextlib.ExitStack() as st:
        k = KB(nc, st)
        c_t = k.sb([128, NCH]); s_t = k.sb([128, NCH])
        k.dma("sp", c_t[:], ccol[:, :], w=[c_t])
        k.op("act", lambda e: e.activation(out=s_t[:], in_=c_t[:], func=AF.Silu), w=[s_t], r=[c_t])
        wb = [k.sb([128, ncols], name="wmb%d" % i) for i in range(3)]
        acc = [k.ps([1, 512], name="macc%d" % g) for g in range(ng)]
        for kc in range(NCH):
            b = wb[kc % 3]
            k.dma("sp" if kc % 2 == 0 else "pool", b[:], wm[kc * 128:(kc + 1) * 128, :], w=[b])
            for g in range(ng):
                k.op("pe", lambda e, g=g, b=b, kc=kc: e.matmul(acc[g][:], lhsT=s_t[:, kc:kc + 1], rhs=b[:, g * 512:(g + 1) * 512],
                                                      start=(kc == 0), stop=(kc == NCH - 1)), w=[acc[g]], r=[s_t, b])
        base = k.sb([1, ncols]); mlt = k.sb([1, 2, ncols]); res = k.sb([1, 2, ncols])
        k.dma("sp", mlt[:], ml.rearrange("(o l) n -> o l n", o=1), w=[mlt])
        for g in range(ng):
            k.op("act", lambda e, g=g: e.activation(out=base[:, g * 512:(g + 1) * 512], in_=acc[g][:], func=AF.Copy), w=[base], r=[acc[g]])
        for l in range(2):
            k.op("dve", lambda e, l=l: e.tensor_tensor(out=res[:, l, :], in0=mlt[:, l, :], in1=base[:], op=ALU.add), w=[res], r=[mlt, base])
        k.out_dma("sp", out.rearrange("(o l) n -> o l n", o=1), res[:], r=[res])
        k.finish()
    return nc


def emit_norm_mod(k, ones, src_tile, nchunks, tt, wmod, sh, dst_fn, ps_ssq, scratch, rs, dim, c0=0):
    for c in range(nchunks):
        sq = scratch[c % 2]
        k.op("act", lambda e, c=c, sq=sq: e.activation(out=sq[:], in_=src_tile[:, c0 + c, :], func=AF.Square), w=[sq], r=[src_tile])
        k.op("pe", lambda e, c=c, sq=sq: e.matmul(ps_ssq[:], lhsT=ones[:], rhs=sq[:], start=(c == 0), stop=(c == nchunks - 1)),
             w=[ps_ssq], r=[ones, sq])
    k.op("act", lambda e: e.activation(out=rs[:], in_=ps_ssq[:], func=AF.Sqrt, scale=1.0 / dim, bias=EPS), w=[rs], r=[ps_ssq])
    k.op("dve", lambda e: e.reciprocal(out=rs[:], in_=rs[:]), w=[rs], r=[rs])
    for c in range(nchunks):
        tmp = scratch[c % 2]
        k.op("dve", lambda e, c=c, tmp=tmp: e.scalar_tensor_tensor(out=tmp[:], in0=src_tile[:, c0 + c, :], scalar=wmod[:, c:c + 1], in1=rs[:],
                                                             op0=ALU.mult, op1=ALU.mult), w=[tmp], r=[src_tile, wmod, rs])
        ap, dt_ = dst_fn(c)
        if sh is None:
            k.op("act", lambda e, tmp=tmp, ap=ap: e.activation(out=ap, in_=tmp[:], func=AF.Copy), w=[dt_], r=[tmp])
        else:
            k.op("act", lambda e, c=c, tmp=tmp, ap=ap: e.activation(out=ap, in_=tmp[:], func=AF.Identity, bias=sh[:, c:c + 1], scale=1.0),
                 w=[dt_], r=[tmp, sh])


def build_pre(tc, out_dt):
    nc = _new_nc()
    xT = nc.dram_tensor("xT", [D, tc], F32, kind="ExternalInput").ap()
    wn = nc.dram_tensor("wn", [128, NCH], F32, kind="ExternalInput").ap()
    scc = nc.dram_tensor("scc", [128, NCH], F32, kind="ExternalInput").ap()
    shc = nc.dram_tensor("shc", [128, NCH], F32, kind="ExternalInput").ap()
    hT = nc.dram_tensor("hT", [D, tc], out_dt, kind="ExternalOutput").ap()
    TT_ = 512
    with contextlib.ExitStack() as st:
        k = KB(nc, st)
        ones = k.sb([128, 128]); k.op("dve", lambda e: e.memset(ones[:], 1.0), w=[ones])
        w_t = k.sb([128, NCH]); sc_t = k.sb([128, NCH]); sh_t = k.sb([128, NCH]); wmod = k.sb([128, NCH])
        k.dma("sp", w_t[:], wn[:, :], w=[w_t]); k.dma("sp", sc_t[:], scc[:, :], w=[sc_t]); k.dma("sp", sh_t[:], shc[:, :], w=[sh_t])
        k.op("dve", lambda e: e.scalar_tensor_tensor(out=wmod[:], in0=sc_t[:], scalar=1.0, in1=w_t[:], op0=ALU.add, op1=ALU.mult),
             w=[wmod], r=[sc_t, w_t])
        xt = [k.sb([128, NCH, TT_], name="xt%d" % i) for i in range(2)]
        ht = [k.sb([128, NCH, TT_], out_dt, name="ht%d" % i) for i in range(1)]
        scratch = [k.sb([128, TT_], name="sq%d" % i) for i in range(2)]
        rs = k.sb([128, TT_]); ps_ssq = k.ps([128, TT_])
        xv = xT.rearrange("(c p) t -> p c t", p=128); hv = hT.rearrange("(c p) t -> p c t", p=128)
        for it in range(tc // TT_):
            x_ = xt[it % 2]; h_ = ht[0]
            for half in range(2):
                cs = slice(half * 16, half * 16 + 16)
                k.dma("sp" if half == 0 else "pool", x_[:, cs, :], xv[:, cs, it * TT_:(it + 1) * TT_], w=[x_])
            emit_norm_mod(k, ones, x_, NCH, TT_, wmod, sh_t, lambda c, h_=h_: (h_[:, c, :], h_), ps_ssq, scratch, rs, float(D))
            for half in range(2):
                cs = slice(half * 16, half * 16 + 16)
                k.out_dma("sp" if half == 0 else "pool", hv[:, cs, it * TT_:(it + 1) * TT_], h_[:, cs, :], r=[h_])
        k.finish()
    return nc


def run(nc, in_maps):
    res = run_bass_kernel_spmd(nc, in_maps, core_ids=list(range(len(in_maps))))
    return res.results


ROPE = 64
ATT_SCALE = 192.0 ** -0.5
NEGBIG = -30000.0


def mix_consts():
    i = np.arange(128)
    c = {}
    c["ident"] = np.eye(128, dtype=np.float32)
    c["tri"] = (i[:, None] <= i[None, :]).astype(np.float32)
    c["negl"] = np.where(i[:, None] > i[None, :], 0.0, NEGBIG).astype(np.float32)
    c["negu"] = np.where(i[:, None] <= i[None, :], 0.0, NEGBIG).astype(np.float32)
    c["bd16"] = ((i[:, None] // 16) == (i[None, :] // 16)).astype(np.float32)
    for b in (16, 32, 64):
        off = (((i[:, None] // (2 * b)) == (i[None, :] // (2 * b))) & ((i[:, None] % (2 * b)) >= b) & ((i[None, :] % (2 * b)) < b)).astype(np.float32)
        c["off%d" % b] = off
        if b < 64:
            c["offT%d" % b] = np.ascontiguousarray(off.T)
    rm = np.zeros((64, 64), np.float32)
    for m in range(32):
        rm[m + 32, m] = -1.0
        rm[m, m + 32] = 1.0
    c["rot"] = rm
    inv = (10000.0 ** (-np.arange(0, 64, 2, dtype=np.float32) / 64.0)).astype(np.float32)
    c["invf"] = np.concatenate([inv, inv]).reshape(64, 1).astype(np.float32)
    p = np.arange(128)[:, None, None]; m = np.arange(4)[None, :, None]; q = np.arange(512)[None, None, :]
    c["amask"] = ((128 * m + p) <= q).astype(np.float32)
    return c


def build_mix(T, do_a=True, do_b=True, do_c=True):
    nc = _new_nc()
    BT = 512
    NB = T // BT
    def din(name, shape, dt=F32):
        return nc.dram_tensor(name, list(shape), dt, kind="ExternalInput").ap()
    hT = din("hT", [D, T], BF16)
    w_ab = din("w_ab", [D, 8 * 128]); w_c = din("w_c", [D, 12 * 128 + 64]); w_tm = din("w_tm", [D, 260])
    lru_p = din("lru_p", [128, 8]); lru_w = din("lru_w", [128, 256])
    dn_cw = din("dn_cw", [128, 24]); dn_hp = din("dn_hp", [128, 4]); dn_nw = din("dn_nw", [128, 128])
    qnw = din("qnw", [128, 8]); kvnw = din("kvnw", [128, 4])
    wq = din("wq", [128, 8, 384]); wkv = din("wkv", [128, 4, 512])
    pos = din("pos", [1, T], I32)
    c_ident = din("ident", [128, 128]); c_tri = din("tri", [128, 128]); c_negl = din("negl", [128, 128]); c_negu = din("negu", [128, 128])
    c_blk = {n: din(n, [128, 128]) for n in ("bd16", "off16", "off32", "off64", "offT16", "offT32")}
    c_rot = din("rot", [64, 64]); c_invf = din("invf", [64, 1]); c_amask = din("amask", [128, 4, 512])
    yaT = nc.dram_tensor("yaT", [128, T], F32, kind="ExternalOutput").ap()
    yb = nc.dram_tensor("yb", [T, 2, 128], F32, kind="ExternalOutput").ap()
    yc = nc.dram_tensor("yc", [T, 2, 128], F32, kind="ExternalOutput").ap()
    s_qn = [nc.dram_tensor("s_qn%d" % h, [128, T], BF16, kind="Internal").ap() for h in range(2)]
    s_qp = [nc.dram_tensor("s_qp%d" % h, [65, T], BF16, kind="Internal").ap() for h in range(2)]
    s_kn = [nc.dram_tensor("s_kn%d" % h, [128, T], BF16, kind="Internal").ap() for h in range(2)]
    s_kp = nc.dram_tensor("s_kp", [65, T], BF16, kind="Internal").ap()
    s_v = [nc.dram_tensor("s_v%d" % h, [128, T // 128, 129], BF16, kind="Internal").ap() for h in range(2)]
    s_km = nc.dram_tensor("s_km", [128, 2], F32, kind="Internal").ap()
    hv = hT.rearrange("(c p) t -> p c t", p=128)
    s_wab = nc.dram_tensor("s_wab", [8, 128, D], BF16, kind="Internal").ap()
    s_wc = nc.dram_tensor("s_wc", [13, 128, D], BF16, kind="Internal").ap()

    with contextlib.ExitStack() as st:
        k = KB(nc, st)
        with contextlib.ExitStack() as pc_:
            cv = [k.sb([128, D], BF16, stack=pc_, name="mcv%d" % i) for i in range(4)]
            wabv0 = w_ab.rearrange("(c p) n -> p c n", p=128); wcv0 = w_c.rearrange("(c p) n -> p c n", p=128)
            ci = 0
            for j in range(8):
                t_ = cv[ci % 4]; ci += 1
                k.dma("pool", t_.h[:].rearrange("p (c n) -> p c n", n=128), wabv0[:, :, j * 128:(j + 1) * 128], w=[t_])
                k.dma("sp", s_wab[j], t_[:], r=[t_])
            for j in range(13):
                wd = 128 if j < 12 else 64
                t_ = cv[ci % 4]; ci += 1
                if wd < 128:
                    k.op("dve", lambda e, t_=t_: e.memset(t_[:], 0.0), w=[t_])
                k.dma("pool", t_.h[:].rearrange("p (c n) -> p c n", n=128)[:, :, 0:wd], wcv0[:, :, j * 128:j * 128 + wd], w=[t_])
                k.dma("sp", s_wc[j], t_[:], r=[t_])
            for e_ in k.eng:
                for key in k.sem:
                    if k.cnt[key] > 0:
                        k._wait(e_, (key, k.cnt[key]))

        def barrier():
            for e in k.eng:
                for key in k.sem:
                    if k.cnt[key] > 0:
                        k._wait(e, (key, k.cnt[key]))

        def load_h(htq, b):
            for qd in range(4):
                k.dma("sp", htq[qd][:], hv[:, qd * 8:(qd + 1) * 8, b * BT:(b + 1) * BT], w=[htq[qd]])

        if do_a or do_b:
          with contextlib.ExitStack() as p1:
            ones = k.sb([128, 128], stack=p1); negones = k.sb([128, 128], stack=p1)
            k.op("dve", lambda e: e.memset(ones[:], 1.0), w=[ones]); k.op("dve", lambda e: e.memset(negones[:], -1.0), w=[negones])
            ident = k.sb([128, 128], stack=p1); tri = k.sb([128, 128], stack=p1); negl = k.sb([128, 128], stack=p1); negu = k.sb([128, 128], stack=p1)
            for t_, s_ in ((ident, c_ident), (tri, c_tri), (negl, c_negl), (negu, c_negu)):
                k.dma("sp", t_[:], s_[:, :], w=[t_])
            cm = {}
            for n_, ap_ in c_blk.items():
                cm[n_] = k.sb([128, 128], stack=p1, name="cm_" + n_)
                k.dma("sp", cm[n_][:], ap_[:, :], w=[cm[n_]])
            htq = [k.sb([128, 8, BT], BF16, stack=p1, name="htq%d" % i) for i in range(4)]
            wst = [k.sb([128, NCH, 128], BF16, stack=p1, name="wst%d" % i) for i in range(3)]
            wtm = k.sb([128, NCH, 260], BF16, stack=p1)
            for half in range(2):
                k.dma("pool", wtm[:, half * 16:(half + 1) * 16, :], w_tm.rearrange("(c p) n -> p c n", p=128)[:, half * 16:(half + 1) * 16, :], w=[wtm])
            big = [k.ps([128, BT], stack=p1, name="big%d" % i) for i in range(3)]
            smb = [k.ps([128, 512], stack=p1, name="smb%d" % i) for i in range(3)]
            small = [TV(smb[i % 3], smb[i % 3].h[:, (i // 3) * 128:(i // 3 + 1) * 128], "sm%d" % i) for i in range(12)]
            ptm = k.ps([128, 512], stack=p1, name="ptm")
            ctr = {"big": 0, "small": 0, "w": 0}
            def nbig():
                ctr["big"] += 1; return big[ctr["big"] % 3]
            def nsm():
                ctr["small"] += 1; return small[ctr["small"] % 12]
            wabv = w_ab.rearrange("(c p) n -> p c n", p=128)
            def p1_chunk(j):
                wt = wst[ctr["w"] % 3]; ctr["w"] += 1
                k.dma("sp", wt.h[:].rearrange("p c n -> p (c n)"), s_wab[j], w=[wt])
                ps = nbig()
                for fc in range(NCH):
                    k.op("pe", lambda e, fc=fc, ps=ps, wt=wt: e.matmul(ps[:], lhsT=wt[:, fc, :], rhs=htq[fc // 8][:, fc % 8, :],
                                                                       start=(fc == 0), stop=(fc == NCH - 1)), w=[ps], r=[wt, htq[fc // 8]])
                return ps
            def conv(dst, xbuf, cw, col0, bias_ap, eng="pool"):
                if bias_ap is None:
                    k.op(eng, lambda e: e.tensor_scalar(out=dst[:], in0=xbuf[:, 0:BT], scalar1=cw[:, col0:col0 + 1], scalar2=None, op0=ALU.mult), w=[dst], r=[xbuf, cw])
                else:
                    k.op(eng, lambda e: e.tensor_scalar(out=dst[:], in0=xbuf[:, 0:BT], scalar1=cw[:, col0:col0 + 1], scalar2=bias_ap, op0=ALU.mult, op1=ALU.add), w=[dst], r=[xbuf, cw])
                for j in range(1, 4):
                    k.op("dve", lambda e, j=j: e.scalar_tensor_tensor(out=dst[:], in0=xbuf[:, j:j + BT], scalar=cw[:, col0 + j:col0 + j + 1], in1=dst[:],
                                                                     op0=ALU.mult, op1=ALU.add), w=[dst], r=[xbuf, cw, dst])
                k.op("pool", lambda e: e.tensor_copy(out=xbuf[:, 0:3], in_=xbuf[:, BT:BT + 3]), w=[xbuf], r=[xbuf])

            lp = k.sb([128, 8], stack=p1); lw = k.sb([128, 256], stack=p1)
            k.dma("sp", lp[:], lru_p[:, :], w=[lp]); k.dma("sp", lw[:], lru_w[:, :], w=[lw])
            c1 = k.sb([128, 2], stack=p1)
            k.op("act", lambda e: e.activation(out=c1[:, 0:1], in_=lp[:, 7:8], func=AF.Exp, scale=-1.0), w=[c1], r=[lp])
            k.op("act", lambda e: e.activation(out=c1[:, 0:1], in_=c1[:, 0:1], func=AF.Ln, bias=1.0, scale=1.0), w=[c1], r=[c1])
            k.op("dve", lambda e: e.tensor_scalar(out=c1[:, 1:2], in0=c1[:, 0:1], scalar1=-16.0, scalar2=None, op0=ALU.mult), w=[c1], r=[c1])
            k.op("dve", lambda e: e.tensor_scalar(out=c1[:, 0:1], in0=c1[:, 0:1], scalar1=-8.0, scalar2=None, op0=ALU.mult), w=[c1], r=[c1])
            xa = k.sb([128, BT + 3], stack=p1); k.op("dve", lambda e: e.memset(xa[:], 0.0), w=[xa])
            hst = [k.sb([128, BT], stack=p1, name="hst%d" % i) for i in range(2)]
            k.op("dve", lambda e: e.memset(hst[1][:], 0.0), w=[hst[1]])
            A_t = {n: k.sb([128, BT], stack=p1, name="A_" + n) for n in ("u", "r", "i", "a", "b", "g")}
            dcw = k.sb([128, 24], stack=p1); dhp = k.sb([128, 4], stack=p1); dnw = k.sb([128, 128], stack=p1)
            k.dma("sp", dcw[:], dn_cw[:, :], w=[dcw]); k.dma("sp", dhp[:], dn_hp[:, :], w=[dhp]); k.dma("sp", dnw[:], dn_nw[:, :], w=[dnw])
            nega = k.sb([128, 2], stack=p1)
            k.op("act", lambda e: e.activation(out=nega[:], in_=dhp[:, 0:2], func=AF.Exp), w=[nega], r=[dhp])
            k.op("dve", lambda e: e.tensor_scalar(out=nega[:], in0=nega[:], scalar1=-1.0, scalar2=None, op0=ALU.mult), w=[nega], r=[nega])
            xb_ = [[k.sb([128, BT + 3], stack=p1, name="xb%d%d" % (h, i)) for i in range(3)] for h in range(2)]
            for h in range(2):
                for i in range(3):
                    k.op("pool", lambda e, h=h, i=i: e.memset(xb_[h][i][:], 0.0), w=[xb_[h][i]])
            S_ = [[k.sb([128, 128], stack=p1, name="S%d%d" % (h, i)) for i in range(2)] for h in range(2)]
            for h in range(2):
                k.op("pool", lambda e, h=h: e.memset(S_[h][0][:], 0.0), w=[S_[h][0]])
            B_t = {n: k.sb([128, BT], stack=p1, name="B_" + n) for n in ("qc", "kc", "vc", "sq", "rsq", "rsk", "qn", "kn")}
            sm_names = ["gt", "dl", "el", "du", "eu", "N", "NT", "qkT", "kbg", "ktail", "vb", "PTa", "PTb", "Qa", "QTa", "Qb", "QTb",
                        "u", "wT", "vnew", "o1", "o", "zs", "junk", "yout",
                        "N16", "No16", "No32", "No64", "NT16", "NoT16", "NoT32", "Xa", "Xb", "Z", "Zp"]
            Bs = {n: k.sb([128, 128], stack=p1, name="Bs_" + n) for n in sm_names}
            cols = {n: k.sb([128, 2], stack=p1, name="Bc_" + n) for n in ("beta", "nbeta", "g", "gc", "gl", "egc", "egl", "etail", "sp", "ssq", "rso")}
            tmsb = k.sb([128, 260], stack=p1)
            k.op("dve", lambda e: e.memset(cols["g"][:], 0.0), w=[cols["g"]])
            sblk = [0, 0]

            for b in range(NB):
                load_h(htq, b)
                if do_a:
                    ps = p1_chunk(0)
                    k.op("act", lambda e, ps=ps: e.activation(out=xa[:, 3:3 + BT], in_=ps[:], func=AF.Copy), w=[xa], r=[ps])
                    u = A_t["u"]
                    conv(u, xa, lp, 0, lp[:, 4:5])
                    for nm, woff, bcol in (("r", 0, 5), ("i", 128, 6)):
                        pg = nbig()
                        k.op("pe", lambda e, pg=pg, woff=woff: e.matmul(pg[:], lhsT=lw[:, woff:woff + 128], rhs=u[:], start=True, stop=True), w=[pg], r=[lw, u])
                        k.op("act", lambda e, pg=pg, nm=nm, bcol=bcol: e.activation(out=A_t[nm][:], in_=pg[:], func=AF.Sigmoid, bias=lp[:, bcol:bcol + 1], scale=1.0),
                             w=[A_t[nm]], r=[pg, lp])
                    k.op("act", lambda e: e.activation(out=A_t["a"][:], in_=A_t["r"][:], func=AF.Exp, scale=c1[:, 0:1]), w=[A_t["a"]], r=[A_t["r"], c1])
                    k.op("act", lambda e: e.activation(out=A_t["b"][:], in_=A_t["r"][:], func=AF.Exp, scale=c1[:, 1:2]), w=[A_t["b"]], r=[A_t["r"], c1])
                    k.op("dve", lambda e: e.tensor_scalar(out=A_t["b"][:], in0=A_t["b"][:], scalar1=-1.0, scalar2=1.0, op0=ALU.mult, op1=ALU.add), w=[A_t["b"]], r=[A_t["b"]])
                    k.op("dve", lambda e: e.tensor_scalar(out=A_t["b"][:], in0=A_t["b"][:], scalar1=1e-30, scalar2=None, op0=ALU.max), w=[A_t["b"]], r=[A_t["b"]])
                    k.op("act", lambda e: e.activation(out=A_t["b"][:], in_=A_t["b"][:], func=AF.Sqrt), w=[A_t["b"]], r=[A_t["b"]])
                    k.op("pool", lambda e: e.tensor_tensor(out=A_t["i"][:], in0=A_t["i"][:], in1=u[:], op=ALU.mult), w=[A_t["i"]], r=[A_t["i"], u])
                    k.op("pool", lambda e: e.tensor_tensor(out=A_t["b"][:], in0=A_t["b"][:], in1=A_t["i"][:], op=ALU.mult), w=[A_t["b"]], r=[A_t["b"], A_t["i"]])
                    hp_, hc_ = hst[(b + 1) % 2], hst[b % 2]
                    k.op("dve", lambda e, hp_=hp_, hc_=hc_: e.tensor_tensor_scan(out=hc_[:], data0=A_t["a"][:], data1=A_t["b"][:], initial=hp_[:, BT - 1:BT],
                                                                               op0=ALU.mult, op1=ALU.add), w=[hc_], r=[A_t["a"], A_t["b"], hp_])
                    ps = p1_chunk(1)
                    k.op("act", lambda e, ps=ps: e.activation(out=A_t["g"][:], in_=ps[:], func=AF.Gelu_apprx_tanh), w=[A_t["g"]], r=[ps])
                    k.op("pool", lambda e, hc_=hc_: e.tensor_tensor(out=A_t["g"][:], in0=A_t["g"][:], in1=hc_[:], op=ALU.mult), w=[A_t["g"]], r=[A_t["g"], hc_])
                    k.out_dma("sp", yaT[:, b * BT:(b + 1) * BT], A_t["g"][:], r=[A_t["g"]])
                if do_b:
                    for h in range(2):
                        dst3 = (B_t["qc"], B_t["kc"], B_t["vc"])
                        for i in range(3):
                            ps = p1_chunk(2 + 3 * h + i)
                            xbuf = xb_[h][i]
                            k.op("act", lambda e, ps=ps, xbuf=xbuf: e.activation(out=xbuf[:, 3:3 + BT], in_=ps[:], func=AF.Copy), w=[xbuf], r=[ps])
                            conv(dst3[i], xbuf, dcw, (h * 3 + i) * 4, None)
                            k.op("act", lambda e, d_=dst3[i]: e.activation(out=d_[:], in_=d_[:], func=AF.Silu), w=[dst3[i]], r=[dst3[i]])
                        for src, rs_, dstn, mul in ((B_t["qc"], B_t["rsq"], B_t["qn"], 128.0 ** -0.5), (B_t["kc"], B_t["rsk"], B_t["kn"], 1.0)):
                            k.op("act", lambda e, src=src: e.activation(out=B_t["sq"][:], in_=src[:], func=AF.Square), w=[B_t["sq"]], r=[src])
                            pg = nbig()
                            k.op("pe", lambda e, pg=pg: e.matmul(pg[:], lhsT=ones[:], rhs=B_t["sq"][:], start=True, stop=True), w=[pg], r=[ones, B_t["sq"]])
                            k.op("act", lambda e, pg=pg, rs_=rs_: e.activation(out=rs_[:], in_=pg[:], func=AF.Sqrt, bias=EPS, scale=1.0), w=[rs_], r=[pg])
                            k.op("dve", lambda e, rs_=rs_: e.reciprocal(out=rs_[:], in_=rs_[:]), w=[rs_], r=[rs_])
                            k.op("dve", lambda e, src=src, rs_=rs_, dstn=dstn, mul=mul: e.scalar_tensor_tensor(out=dstn[:], in0=src[:], scalar=mul, in1=rs_[:],
                                                                                                             op0=ALU.mult, op1=ALU.mult), w=[dstn], r=[src, rs_])
                        qn, kn, vc = B_t["qn"], B_t["kn"], B_t["vc"]
                        for s in range(4):
                            ts_ = slice(s * 128, (s + 1) * 128)
                            if h == 0:
                                pass
                            for fc in range(NCH):
                                k.op("pe", lambda e, fc=fc, ts_=ts_: e.matmul(ptm[:, 0:260], lhsT=htq[fc // 8][:, fc % 8, ts_], rhs=wtm[:, fc, :],
                                                                          start=(fc == 0), stop=(fc == NCH - 1)), w=[ptm], r=[wtm, htq[fc // 8]])
                            k.op("act", lambda e: e.activation(out=tmsb[:], in_=ptm[:, 0:260], func=AF.Copy), w=[tmsb], r=[ptm])
                            C = cols
                            k.op("act", lambda e, h=h: e.activation(out=C["beta"][:, 0:1], in_=tmsb[:, 256 + h:257 + h], func=AF.Sigmoid), w=[C["beta"]], r=[tmsb])
                            k.op("dve", lambda e: e.tensor_scalar(out=C["nbeta"][:, 0:1], in0=C["beta"][:, 0:1], scalar1=-1.0, scalar2=None, op0=ALU.mult), w=[C["nbeta"]], r=[C["beta"]])
                            k.op("act", lambda e, h=h: e.activation(out=C["sp"][:, 0:1], in_=tmsb[:, 258 + h:259 + h], func=AF.Exp, bias=dhp[:, 2 + h:3 + h], scale=1.0), w=[C["sp"]], r=[tmsb, dhp])
                            k.op("act", lambda e: e.activation(out=C["sp"][:, 0:1], in_=C["sp"][:, 0:1], func=AF.Ln, bias=1.0, scale=1.0), w=[C["sp"]], r=[C["sp"]])
                            k.op("dve", lambda e, h=h: e.tensor_scalar(out=C["g"][:, 0:1], in0=C["sp"][:, 0:1], scalar1=nega[:, h:h + 1], scalar2=None, op0=ALU.mult), w=[C["g"]], r=[C["sp"], nega])
                            g = C["g"]
                            k.op("dve", lambda e: e.tensor_scalar(out=Bs["gt"][:], in0=tri[:], scalar1=g[:, 0:1], scalar2=None, op0=ALU.mult), w=[Bs["gt"]], r=[tri, g])
                            pD = nsm()
                            k.op("pe", lambda e, pD=pD: e.matmul(pD[:], lhsT=Bs["gt"][:], rhs=ones[:], start=True, stop=False), w=[pD], r=[Bs["gt"], ones])
                            k.op("pe", lambda e, pD=pD: e.matmul(pD[:], lhsT=negones[:], rhs=Bs["gt"][:], start=False, stop=True), w=[pD], r=[Bs["gt"], negones])
                            pgc = nsm()
                            k.op("pe", lambda e, pgc=pgc: e.matmul(pgc[:, 0:2], lhsT=tri[:], rhs=g[:, 0:2], start=True, stop=True), w=[pgc], r=[tri, g])
                            k.op("pe", lambda e, pgc=pgc: e.matmul(pgc[:, 2:4], lhsT=ones[:], rhs=g[:, 0:2], start=True, stop=True), w=[pgc], r=[ones, g])
                            k.op("act", lambda e, pgc=pgc: e.activation(out=C["egc"][:, 0:1], in_=pgc[:, 0:1], func=AF.Exp), w=[C["egc"]], r=[pgc])
                            k.op("act", lambda e, pgc=pgc: e.activation(out=C["egl"][:, 0:1], in_=pgc[:, 2:3], func=AF.Exp), w=[C["egl"]], r=[pgc])
                            k.op("act", lambda e, pgc=pgc: e.activation(out=C["gl"][:, 0:1], in_=pgc[:, 2:3], func=AF.Copy), w=[C["gl"]], r=[pgc])
                            k.op("act", lambda e, pgc=pgc: e.activation(out=C["etail"][:, 0:1], in_=pgc[:, 0:1], func=AF.Exp, bias=C["gl"][:, 0:1], scale=-1.0), w=[C["etail"]], r=[pgc, C["gl"]])
                            k.op("dve", lambda e, pD=pD: e.tensor_tensor(out=Bs["dl"][:], in0=pD[:], in1=negl[:], op=ALU.add), w=[Bs["dl"]], r=[pD, negl])
                            k.op("act", lambda e: e.activation(out=Bs["el"][:], in_=Bs["dl"][:], func=AF.Exp), w=[Bs["el"]], r=[Bs["dl"]])
                            k.op("dve", lambda e, pD=pD: e.scalar_tensor_tensor(out=Bs["du"][:], in0=pD[:], scalar=-1.0, in1=negu[:], op0=ALU.mult, op1=ALU.add), w=[Bs["du"]], r=[pD, negu])
                            k.op("act", lambda e: e.activation(out=Bs["eu"][:], in_=Bs["du"][:], func=AF.Exp), w=[Bs["eu"]], r=[Bs["du"]])
                            pG = nsm()
                            k.op("pe", lambda e, pG=pG, ts_=ts_: e.matmul(pG[:], lhsT=kn[:, ts_], rhs=kn[:, ts_], start=True, stop=True), w=[pG], r=[kn])
                            k.op("dve", lambda e, pG=pG: e.scalar_tensor_tensor(out=Bs["N"][:], in0=pG[:], scalar=C["nbeta"][:, 0:1], in1=Bs["el"][:], op0=ALU.mult, op1=ALU.mult),
                                 w=[Bs["N"]], r=[pG, C["nbeta"], Bs["el"]])
                            pQ = nsm()
                            k.op("pe", lambda e, pQ=pQ, ts_=ts_: e.matmul(pQ[:], lhsT=kn[:, ts_], rhs=qn[:, ts_], start=True, stop=True), w=[pQ], r=[kn, qn])
                            k.op("dve", lambda e, pQ=pQ: e.tensor_tensor(out=Bs["qkT"][:], in0=pQ[:], in1=Bs["eu"][:], op=ALU.mult), w=[Bs["qkT"]], r=[pQ, Bs["eu"]])
                            pK = nsm()
                            k.op("pe", lambda e, pK=pK, ts_=ts_: e.matmul(pK[:], lhsT=kn[:, ts_], rhs=ident[:], start=True, stop=True), w=[pK], r=[kn, ident])
                            k.op("dve", lambda e, pK=pK: e.tensor_scalar(out=Bs["kbg"][:], in0=pK[:], scalar1=C["beta"][:, 0:1], scalar2=C["egc"][:, 0:1], op0=ALU.mult, op1=ALU.mult),
                                 w=[Bs["kbg"]], r=[pK, C["beta"], C["egc"]])
                            k.op("dve", lambda e, pK=pK: e.tensor_scalar(out=Bs["ktail"][:], in0=pK[:], scalar1=C["etail"][:, 0:1], scalar2=None, op0=ALU.mult), w=[Bs["ktail"]], r=[pK, C["etail"]])
                            pV = nsm()
                            k.op("pe", lambda e, pV=pV, ts_=ts_: e.matmul(pV[:], lhsT=vc[:, ts_], rhs=ident[:], start=True, stop=True), w=[pV], r=[vc, ident])
                            k.op("dve", lambda e, pV=pV: e.tensor_scalar(out=Bs["vb"][:], in0=pV[:], scalar1=C["beta"][:, 0:1], scalar2=None, op0=ALU.mult), w=[Bs["vb"]], r=[pV, C["beta"]])
                            N_ = Bs["N"]
                            k.op("pool", lambda e: e.tensor_tensor(out=Bs["N16"][:], in0=N_[:], in1=cm["bd16"][:], op=ALU.mult), w=[Bs["N16"]], r=[N_, cm["bd16"]])
                            for b_ in (16, 32, 64):
                                k.op("pool", lambda e, b_=b_: e.tensor_tensor(out=Bs["No%d" % b_][:], in0=N_[:], in1=cm["off%d" % b_][:], op=ALU.mult), w=[Bs["No%d" % b_]], r=[N_, cm["off%d" % b_]])
                            pT = nsm()
                            k.op("pe", lambda e, pT=pT: e.matmul(pT[:], lhsT=N_[:], rhs=ident[:], start=True, stop=True), w=[pT], r=[N_, ident])
                            k.op("dve", lambda e, pT=pT: e.tensor_tensor(out=Bs["NT16"][:], in0=pT[:], in1=cm["bd16"][:], op=ALU.mult), w=[Bs["NT16"]], r=[pT, cm["bd16"]])
                            for b_ in (16, 32):
                                k.op("dve", lambda e, pT=pT, b_=b_: e.tensor_tensor(out=Bs["NoT%d" % b_][:], in0=pT[:], in1=cm["offT%d" % b_][:], op=ALU.mult), w=[Bs["NoT%d" % b_]], r=[pT, cm["offT%d" % b_]])
                            Xc, Xn = Bs["Xa"], Bs["Xb"]; Yc, Yn = Bs["PTa"], Bs["PTb"]
                            k.op("pool", lambda e, Xc=Xc: e.tensor_tensor(out=Xc[:], in0=Bs["N16"][:], in1=ident[:], op=ALU.add), w=[Xc], r=[Bs["N16"], ident])
                            k.op("pool", lambda e, Yc=Yc: e.tensor_tensor(out=Yc[:], in0=Bs["NT16"][:], in1=ident[:], op=ALU.add), w=[Yc], r=[Bs["NT16"], ident])
                            Qc, QTc = Bs["N16"], Bs["NT16"]
                            for s_ in range(3):
                                Qn_, QTn_ = (Bs["Qa"], Bs["QTa"]) if s_ % 2 == 0 else (Bs["Qb"], Bs["QTb"])
                                p1_ = nsm()
                                k.op("pe", lambda e, p1_=p1_, Qc=Qc, QTc=QTc: e.matmul(p1_[:], lhsT=QTc[:], rhs=Qc[:], start=True, stop=True), w=[p1_], r=[Qc, QTc])
                                k.op("act", lambda e, p1_=p1_, Qn_=Qn_: e.activation(out=Qn_[:], in_=p1_[:], func=AF.Copy), w=[Qn_], r=[p1_])
                                if s_ < 2:
                                    p2_ = nsm()
                                    k.op("pe", lambda e, p2_=p2_, Qc=Qc, QTc=QTc: e.matmul(p2_[:], lhsT=Qc[:], rhs=QTc[:], start=True, stop=True), w=[p2_], r=[Qc, QTc])
                                    k.op("act", lambda e, p2_=p2_, QTn_=QTn_: e.activation(out=QTn_[:], in_=p2_[:], func=AF.Copy), w=[QTn_], r=[p2_])
                                pX = nsm()
                                k.op("pe", lambda e, pX=pX, Yc=Yc, Qn_=Qn_: e.matmul(pX[:], lhsT=Yc[:], rhs=Qn_[:], start=True, stop=True), w=[pX], r=[Yc, Qn_])
                                pY = nsm()
                                k.op("pe", lambda e, pY=pY, Yc=Yc, Qn_=Qn_: e.matmul(pY[:], lhsT=Qn_[:], rhs=Yc[:], start=True, stop=True), w=[pY], r=[Yc, Qn_])
                                k.op("dve", lambda e, pX=pX, Xc=Xc, Xn=Xn: e.tensor_tensor(out=Xn[:], in0=pX[:], in1=Xc[:], op=ALU.add), w=[Xn], r=[pX, Xc])
                                k.op("dve", lambda e, pY=pY, Yc=Yc, Yn=Yn: e.tensor_tensor(out=Yn[:], in0=pY[:], in1=Yc[:], op=ALU.add), w=[Yn], r=[pY, Yc])
                                Xc, Xn = Xn, Xc; Yc, Yn = Yn, Yc
                                Qc, QTc = Qn_, QTn_
                            for b_ in (16, 32, 64):
                                pZ = nsm()
                                k.op("pe", lambda e, pZ=pZ, Yc=Yc, b_=b_: e.matmul(pZ[:], lhsT=Bs["No%d" % b_][:], rhs=Yc[:], start=True, stop=True), w=[pZ], r=[Bs["No%d" % b_], Yc])
                                k.op("act", lambda e, pZ=pZ: e.activation(out=Bs["Z"][:], in_=pZ[:], func=AF.Copy), w=[Bs["Z"]], r=[pZ])
                                if b_ < 64:
                                    pZp = nsm()
                                    k.op("pe", lambda e, pZp=pZp, Xc=Xc, b_=b_: e.matmul(pZp[:], lhsT=Bs["NoT%d" % b_][:], rhs=Xc[:], start=True, stop=True), w=[pZp], r=[Bs["NoT%d" % b_], Xc])
                                    k.op("act", lambda e, pZp=pZp: e.activation(out=Bs["Zp"][:], in_=pZp[:], func=AF.Copy), w=[Bs["Zp"]], r=[pZp])
                                pY = nsm()
                                k.op("pe", lambda e, pY=pY, Xc=Xc: e.matmul(pY[:], lhsT=Xc[:], rhs=Bs["Z"][:], start=True, stop=True), w=[pY], r=[Xc, Bs["Z"]])
                                if b_ < 64:
                                    pX = nsm()
                                    k.op("pe", lambda e, pX=pX, Yc=Yc: e.matmul(pX[:], lhsT=Yc[:], rhs=Bs["Zp"][:], start=True, stop=True), w=[pX], r=[Yc, Bs["Zp"]])
                                k.op("dve", lambda e, pY=pY, Yc=Yc, Yn=Yn: e.tensor_tensor(out=Yn[:], in0=pY[:], in1=Yc[:], op=ALU.add), w=[Yn], r=[pY, Yc])
                                if b_ < 64:
                                    k.op("dve", lambda e, pX=pX, Xc=Xc, Xn=Xn: e.tensor_tensor(out=Xn[:], in0=pX[:], in1=Xc[:], op=ALU.add), w=[Xn], r=[pX, Xc])
                                    Xc, Xn = Xn, Xc
                                Yc, Yn = Yn, Yc
                            PTc = Yc
                            AiT = PTc
                            pU = nsm()
                            k.op("pe", lambda e, pU=pU, AiT=AiT: e.matmul(pU[:], lhsT=AiT[:], rhs=Bs["vb"][:], start=True, stop=True), w=[pU], r=[AiT, Bs["vb"]])
                            k.op("act", lambda e, pU=pU: e.activation(out=Bs["u"][:], in_=pU[:], func=AF.Copy), w=[Bs["u"]], r=[pU])
                            pW = nsm()
                            k.op("pe", lambda e, pW=pW, AiT=AiT: e.matmul(pW[:], lhsT=Bs["kbg"][:], rhs=AiT[:], start=True, stop=True), w=[pW], r=[AiT, Bs["kbg"]])
                            k.op("act", lambda e, pW=pW: e.activation(out=Bs["wT"][:], in_=pW[:], func=AF.Copy), w=[Bs["wT"]], r=[pW])
                            Sc = S_[h][sblk[h] % 2]; Sn = S_[h][(sblk[h] + 1) % 2]; sblk[h] += 1
                            pws = nsm()
                            k.op("pe", lambda e, pws=pws, Sc=Sc: e.matmul(pws[:], lhsT=Bs["wT"][:], rhs=Sc[:], start=True, stop=True), w=[pws], r=[Bs["wT"], Sc])
                            k.op("dve", lambda e, pws=pws: e.tensor_tensor(out=Bs["vnew"][:], in0=Bs["u"][:], in1=pws[:], op=ALU.subtract), w=[Bs["vnew"]], r=[Bs["u"], pws])
                            po1 = nsm()
                            k.op("pe", lambda e, po1=po1, Sc=Sc, ts_=ts_: e.matmul(po1[:], lhsT=qn[:, ts_], rhs=Sc[:], start=True, stop=True), w=[po1], r=[qn, Sc])
                            k.op("dve", lambda e, po1=po1: e.tensor_scalar(out=Bs["o1"][:], in0=po1[:], scalar1=C["egc"][:, 0:1], scalar2=None, op0=ALU.mult), w=[Bs["o1"]], r=[po1, C["egc"]])
                            po2 = nsm()
                            k.op("pe", lambda e, po2=po2: e.matmul(po2[:], lhsT=Bs["qkT"][:], rhs=Bs["vnew"][:], start=True, stop=True), w=[po2], r=[Bs["qkT"], Bs["vnew"]])
                            k.op("dve", lambda e, po2=po2: e.tensor_tensor(out=Bs["o"][:], in0=Bs["o1"][:], in1=po2[:], op=ALU.add), w=[Bs["o"]], r=[Bs["o1"], po2])
                            pS = nsm()
                            k.op("pe", lambda e, pS=pS: e.matmul(pS[:], lhsT=Bs["ktail"][:], rhs=Bs["vnew"][:], start=True, stop=True), w=[pS], r=[Bs["ktail"], Bs["vnew"]])
                            k.op("dve", lambda e, pS=pS, Sc=Sc, Sn=Sn: e.scalar_tensor_tensor(out=Sn[:], in0=Sc[:], scalar=C["egl"][:, 0:1], in1=pS[:], op0=ALU.mult, op1=ALU.add),
                                 w=[Sn], r=[Sc, C["egl"], pS])
                            k.op("act", lambda e: e.activation(out=Bs["junk"][:], in_=Bs["o"][:], func=AF.Square), w=[Bs["junk"]], r=[Bs["o"]])
                            k.op("dve", lambda e: e.reduce_sum(out=C["ssq"][:, 0:1], in_=Bs["junk"][:], axis=AX.X), w=[C["ssq"]], r=[Bs["junk"]])
                            k.op("act", lambda e: e.activation(out=C["rso"][:, 0:1], in_=C["ssq"][:, 0:1], func=AF.Sqrt, bias=EPS, scale=1.0 / 128), w=[C["rso"]], r=[C["ssq"]])
                            k.op("dve", lambda e: e.reciprocal(out=C["rso"][:, 0:1], in_=C["rso"][:, 0:1]), w=[C["rso"]], r=[C["rso"]])
                            k.op("act", lambda e, h=h: e.activation(out=Bs["zs"][:], in_=tmsb[:, h * 128:(h + 1) * 128], func=AF.Silu), w=[Bs["zs"]], r=[tmsb])
                            k.op("pool", lambda e: e.tensor_tensor(out=Bs["zs"][:], in0=Bs["zs"][:], in1=dnw[:], op=ALU.mult), w=[Bs["zs"]], r=[Bs["zs"], dnw])
                            k.op("dve", lambda e: e.scalar_tensor_tensor(out=Bs["yout"][:], in0=Bs["o"][:], scalar=C["rso"][:, 0:1], in1=Bs["zs"][:], op0=ALU.mult, op1=ALU.mult),
                                 w=[Bs["yout"]], r=[Bs["o"], C["rso"], Bs["zs"]])
                            k.out_dma("sp", yb[b * BT + s * 128:b * BT + (s + 1) * 128, h, :], Bs["yout"][:], r=[Bs["yout"]])
            barrier()

        if do_c:
          kmax = k.sb([128, 2], name="kmax"); nkm = k.sb([128, 2], name="nkm")
          k.op("dve", lambda e: e.memset(kmax[:], 0.0), w=[kmax])
          with contextlib.ExitStack() as p1:
            ones = k.sb([128, 128], stack=p1, name="c_ones")
            k.op("dve", lambda e: e.memset(ones[:], 1.0), w=[ones])
            htq = [k.sb([128, 8, BT], BF16, stack=p1, name="chtq%d" % i) for i in range(4)]
            wst = [k.sb([128, NCH, 128], BF16, stack=p1, name="cwst%d" % i) for i in range(3)]
            big = [k.ps([128, BT], stack=p1, name="cbig%d" % i) for i in range(4)]
            ps_ssq = k.ps([128, BT], stack=p1, name="c_ssq"); ps_ssk = k.ps([128, BT], stack=p1, name="c_ssk"); ps_ssp = k.ps([128, BT], stack=p1, name="c_ssp")
            ctr = {"big": 0, "w": 0}
            def nbig():
                ctr["big"] += 1; return big[ctr["big"] % 4]
            wcv = w_c.rearrange("(c p) n -> p c n", p=128)
            def pc_chunk(j, width=128):
                wt = wst[ctr["w"] % 3]; ctr["w"] += 1
                k.dma("sp", wt.h[:].rearrange("p c n -> p (c n)"), s_wc[j], w=[wt])
                ps = nbig()
                for fc in range(NCH):
                    k.op("pe", lambda e, fc=fc, ps=ps, wt=wt: e.matmul(ps[0:width, :], lhsT=wt[:, fc, 0:width], rhs=htq[fc // 8][:, fc % 8, :],
                                                                       start=(fc == 0), stop=(fc == NCH - 1)), w=[ps], r=[wt, htq[fc // 8]])
                return ps
            qnw_t = k.sb([128, 8], stack=p1); kvnw_t = k.sb([128, 4], stack=p1)
            k.dma("sp", qnw_t[:], qnw[:, :], w=[qnw_t]); k.dma("sp", kvnw_t[:], kvnw[:, :], w=[kvnw_t])
            wq_t = k.sb([128, 8, 384], BF16, stack=p1); wkv_t = k.sb([128, 4, 512], BF16, stack=p1)
            k.dma("pool", wq_t[:], wq[:, :, :], w=[wq_t]); k.dma("pool", wkv_t[:], wkv[:, :, :], w=[wkv_t])
            rot_t = k.sb([64, 64], stack=p1); invf_t = k.sb([64, 1], stack=p1)
            k.dma("sp", rot_t[:], c_rot[:, :], w=[rot_t]); k.dma("sp", invf_t[:], c_invf[:, :], w=[invf_t])
            cq = k.sb([128, 8, BT], stack=p1); ckv = k.sb([128, 4, BT], stack=p1); kr = k.sb([64, BT], stack=p1)
            qn_ = k.sb([128, 8, BT], BF16, stack=p1); kvn = k.sb([128, 4, BT], BF16, stack=p1)
            scratch = [k.sb([128, BT], stack=p1, name="csq%d" % i) for i in range(2)]
            rs = k.sb([128, BT], stack=p1)
            posi = k.sb([64, BT], I32, stack=p1)
            R = {n: k.sb([64, BT], stack=p1, name="R_" + n) for n in ("ang", "n", "r", "sin", "cos", "t1", "t2", "qpe")}
            qnope = k.sb([128, BT], BF16, stack=p1); knope = k.sb([128, BT], BF16, stack=p1)
            qpa = k.sb([65, BT], BF16, stack=p1); kpa = k.sb([65, BT], BF16, stack=p1)
            vaug = k.sb([128, 4, 129], BF16, stack=p1)
            k.op("dve", lambda e: e.memset(vaug[:], 1.0), w=[vaug])
            k.op("dve", lambda e: e.memset(kpa[64:65, :], 1.0), w=[kpa])
            bmax = k.sb([128, 1], stack=p1)
            TWO_PI = 6.283185307179586

            def rope(src, dst_ap, dst_t):
                pr = nbig()
                k.op("pe", lambda e: e.matmul(pr[0:64, :], lhsT=rot_t[:], rhs=src[:], start=True, stop=True), w=[pr], r=[rot_t, src])
                k.op("pool", lambda e: e.tensor_tensor(out=R["t1"][:], in0=src[:], in1=R["cos"][:], op=ALU.mult), w=[R["t1"]], r=[src, R["cos"]])
                k.op("dve", lambda e: e.tensor_tensor(out=R["t2"][:], in0=pr[0:64, :], in1=R["sin"][:], op=ALU.mult), w=[R["t2"]], r=[pr, R["sin"]])
                k.op("pool", lambda e: e.tensor_tensor(out=dst_ap, in0=R["t1"][:], in1=R["t2"][:], op=ALU.add), w=[dst_t], r=[R["t1"], R["t2"]])

            for b in range(NB):
                load_h(htq, b)
                bs = slice(b * BT, (b + 1) * BT)
                k.dma("sp", posi[:], pos[0:1, bs].broadcast_to([64, BT]), w=[posi])
                k.op("dve", lambda e: e.tensor_copy(out=R["ang"][:], in_=posi[:]), w=[R["ang"]], r=[posi])
                k.op("dve", lambda e: e.tensor_scalar(out=R["ang"][:], in0=R["ang"][:], scalar1=invf_t[:, 0:1], scalar2=None, op0=ALU.mult), w=[R["ang"]], r=[R["ang"], invf_t])
                k.op("dve", lambda e: e.tensor_scalar(out=R["n"][:], in0=R["ang"][:], scalar1=1.0 / TWO_PI, scalar2=12582912.0, op0=ALU.mult, op1=ALU.add), w=[R["n"]], r=[R["ang"]])
                k.op("dve", lambda e: e.tensor_scalar(out=R["n"][:], in0=R["n"][:], scalar1=-12582912.0, scalar2=None, op0=ALU.add), w=[R["n"]], r=[R["n"]])
                k.op("dve", lambda e: e.scalar_tensor_tensor(out=R["r"][:], in0=R["n"][:], scalar=-6.28125, in1=R["ang"][:], op0=ALU.mult, op1=ALU.add), w=[R["r"]], r=[R["n"], R["ang"]])
                k.op("dve", lambda e: e.scalar_tensor_tensor(out=R["r"][:], in0=R["n"][:], scalar=-(TWO_PI - 6.28125), in1=R["r"][:], op0=ALU.mult, op1=ALU.add), w=[R["r"]], r=[R["n"], R["r"]])
                k.op("dve", lambda e: e.tensor_scalar(out=R["r"][:], in0=R["r"][:], scalar1=3.14159, scalar2=-3.14159, op0=ALU.min, op1=ALU.max), w=[R["r"]], r=[R["r"]])
                k.op("act", lambda e: e.activation(out=R["sin"][:], in_=R["r"][:], func=AF.Sin), w=[R["sin"]], r=[R["r"]])
                k.op("dve", lambda e: e.tensor_scalar(out=R["t1"][:], in0=R["r"][:], scalar1=-1.0, scalar2=None, op0=ALU.mult), w=[R["t1"]], r=[R["r"]])
                k.op("dve", lambda e: e.tensor_tensor(out=R["t1"][:], in0=R["t1"][:], in1=R["r"][:], op=ALU.max), w=[R["t1"]], r=[R["t1"], R["r"]])
                k.op("dve", lambda e: e.tensor_scalar(out=R["t1"][:], in0=R["t1"][:], scalar1=-1.0, scalar2=1.5707963, op0=ALU.mult, op1=ALU.add), w=[R["t1"]], r=[R["t1"]])
                k.op("act", lambda e: e.activation(out=R["cos"][:], in_=R["t1"][:], func=AF.Sin), w=[R["cos"]], r=[R["t1"]])
                for j in range(8):
                    ps = pc_chunk(j)
                    k.op("act", lambda e, ps=ps, j=j: e.activation(out=cq[:, j, :], in_=ps[:], func=AF.Copy), w=[cq], r=[ps])
                for j in range(4):
                    ps = pc_chunk(8 + j)
                    k.op("act", lambda e, ps=ps, j=j: e.activation(out=ckv[:, j, :], in_=ps[:], func=AF.Copy), w=[ckv], r=[ps])
                ps = pc_chunk(12, 64)
                k.op("act", lambda e, ps=ps: e.activation(out=kr[:], in_=ps[0:64, :], func=AF.Copy), w=[kr], r=[ps])
                emit_norm_mod(k, ones, cq, 8, BT, qnw_t, None, lambda c: (qn_[:, c, :], qn_), ps_ssq, scratch, rs, 1024.0)
                emit_norm_mod(k, ones, ckv, 4, BT, kvnw_t, None, lambda c: (kvn[:, c, :], kvn), ps_ssq, scratch, rs, 512.0)
                rope(kr, R["qpe"][:], R["qpe"])
                k.op("act", lambda e: e.activation(out=kpa[0:64, :], in_=R["qpe"][:], func=AF.Copy), w=[kpa], r=[R["qpe"]])
                k.op("act", lambda e: e.activation(out=scratch[0][0:64, :], in_=R["qpe"][:], func=AF.Square), w=[scratch[0]], r=[R["qpe"]])
                k.op("pe", lambda e: e.matmul(ps_ssp[:], lhsT=ones[0:64, :], rhs=scratch[0][0:64, :], start=True, stop=True), w=[ps_ssp], r=[ones, scratch[0]])
                k.op("act", lambda e: e.activation(out=rs[:], in_=ps_ssp[:], func=AF.Copy), w=[rs], r=[ps_ssp])
                k.out_dma("sp", s_kp[:, bs], kpa[:], r=[kpa])
                for hh in range(2):
                    pq = nbig()
                    for rc in range(8):
                        k.op("pe", lambda e, rc=rc, pq=pq: e.matmul(pq[:], lhsT=wq_t[:, rc, hh * 192:hh * 192 + 128], rhs=qn_[:, rc, :], start=(rc == 0), stop=(rc == 7)), w=[pq], r=[wq_t, qn_])
                    k.op("act", lambda e, pq=pq: e.activation(out=qnope[:], in_=pq[:], func=AF.Copy), w=[qnope], r=[pq])
                    k.op("act", lambda e, pq=pq: e.activation(out=scratch[0][:], in_=pq[:], func=AF.Square), w=[scratch[0]], r=[pq])
                    k.op("pe", lambda e: e.matmul(ps_ssk[:], lhsT=ones[:], rhs=scratch[0][:], start=True, stop=False), w=[ps_ssk], r=[ones, scratch[0]])
                    pq2 = nbig()
                    for rc in range(8):
                        k.op("pe", lambda e, rc=rc, pq2=pq2: e.matmul(pq2[0:64, :], lhsT=wq_t[:, rc, hh * 192 + 128:hh * 192 + 192], rhs=qn_[:, rc, :], start=(rc == 0), stop=(rc == 7)), w=[pq2], r=[wq_t, qn_])
                    k.op("act", lambda e, pq2=pq2: e.activation(out=R["qpe"][:], in_=pq2[0:64, :], func=AF.Copy), w=[R["qpe"]], r=[pq2])
                    k.op("act", lambda e: e.activation(out=scratch[1][0:64, :], in_=R["qpe"][:], func=AF.Square), w=[scratch[1]], r=[R["qpe"]])
                    k.op("pe", lambda e: e.matmul(ps_ssk[:], lhsT=ones[0:64, :], rhs=scratch[1][0:64, :], start=False, stop=True), w=[ps_ssk], r=[ones, scratch[1]])
                    k.op("act", lambda e: e.activation(out=qpa[64:65, :], in_=ps_ssk[64:65, :], func=AF.Sqrt), w=[qpa], r=[ps_ssk])
                    rope(R["qpe"], qpa[0:64, :], qpa)
                    k.out_dma("sp", s_qn[hh][:, bs], qnope[:], r=[qnope])
                    k.out_dma("sp", s_qp[hh][:, bs], qpa[:], r=[qpa])
                    pk = nbig()
                    for rc in range(4):
                        k.op("pe", lambda e, rc=rc, pk=pk: e.matmul(pk[:], lhsT=wkv_t[:, rc, hh * 256:hh * 256 + 128], rhs=kvn[:, rc, :], start=(rc == 0), stop=(rc == 3)), w=[pk], r=[wkv_t, kvn])
                    k.op("act", lambda e, pk=pk: e.activation(out=knope[:], in_=pk[:], func=AF.Copy), w=[knope], r=[pk])
                    k.op("act", lambda e, pk=pk: e.activation(out=scratch[0][:], in_=pk[:], func=AF.Square), w=[scratch[0]], r=[pk])
                    k.op("pe", lambda e: e.matmul(ps_ssk[:], lhsT=ones[:], rhs=scratch[0][:], start=True, stop=True), w=[ps_ssk], r=[ones, scratch[0]])
                    k.op("dve", lambda e: e.tensor_tensor(out=scratch[1][:], in0=ps_ssk[:], in1=rs[:], op=ALU.add), w=[scratch[1]], r=[ps_ssk, rs])
                    k.op("dve", lambda e: e.reduce_max(out=bmax[:], in_=scratch[1][:], axis=AX.X), w=[bmax], r=[scratch[1]])
                    k.op("dve", lambda e, hh=hh: e.tensor_tensor(out=kmax[:, hh:hh + 1], in0=kmax[:, hh:hh + 1], in1=bmax[:], op=ALU.max), w=[kmax], r=[kmax, bmax])
                    k.out_dma("sp", s_kn[hh][:, bs], knope[:], r=[knope])
                    for sub in range(4):
                        pv = nbig()
                        for rc in range(4):
                            k.op("pe", lambda e, rc=rc, pv=pv, sub=sub: e.matmul(pv[:, 0:128], lhsT=kvn[:, rc, sub * 128:(sub + 1) * 128], rhs=wkv_t[:, rc, hh * 256 + 128:hh * 256 + 256],
                                                                        start=(rc == 0), stop=(rc == 3)), w=[pv], r=[wkv_t, kvn])
                        k.op("act", lambda e, pv=pv, sub=sub: e.activation(out=vaug[:, sub, 0:128], in_=pv[:, 0:128], func=AF.Copy), w=[vaug], r=[pv])
                    k.out_dma("sp", s_v[hh][:, 4 * b:4 * b + 4, :], vaug[:], r=[vaug])
            k.op("act", lambda e: e.activation(out=nkm[:], in_=kmax[:], func=AF.Sqrt), w=[nkm], r=[kmax])
            k.op("dve", lambda e: e.tensor_scalar(out=nkm[:], in0=nkm[:], scalar1=-1.0, scalar2=None, op0=ALU.mult), w=[nkm], r=[nkm])
            barrier()
          with contextlib.ExitStack() as p2:
            am = k.sb([128, 4, 512], BF16, stack=p2)
            k.dma("pool", am[:], c_amask[:, :, :], w=[am])
            kp_res = k.sb([65, T], BF16, stack=p2); kn_res = k.sb([128, T], BF16, stack=p2); v_res = k.sb([128, T // 128, 129], BF16, stack=p2)
            qn_g = [k.sb([128, 512], BF16, stack=p2, name="qn_g%d" % i) for i in range(2)]
            qp_g = [k.sb([65, 512], BF16, stack=p2, name="qp_g%d" % i) for i in range(2)]
            pt_ = [k.sb([128, 512], BF16, stack=p2, name="pt%d" % i) for i in range(3)]
            pss = [k.ps([128, 512], stack=p2, name="pss%d" % i) for i in range(2)]
            po = [k.ps([128, 512], stack=p2, name="po%d" % i) for i in range(4)]
            rden = k.sb([128, 4], stack=p2); osb = [k.sb([128, 128], stack=p2, name="osb%d" % i) for i in range(2)]
            k.dma("sp", kp_res[:], s_kp[:, :], w=[kp_res])
            it = 0
            for hh in range(2):
                k.dma("sp", kn_res[:], s_kn[hh][:, :], w=[kn_res])
                k.dma("sp", v_res[:], s_v[hh][:, :, :], w=[v_res])
                for g in range(T // 512):
                    qn = qn_g[g % 2]; qp = qp_g[g % 2]
                    k.dma("sp", qn[:], s_qn[hh][:, g * 512:(g + 1) * 512], w=[qn])
                    k.dma("sp", qp[:], s_qp[hh][:, g * 512:(g + 1) * 512], w=[qp])
                    k.op("dve", lambda e, qp=qp, hh=hh: e.tensor_scalar(out=qp[64:65, :], in0=qp[64:65, :], scalar1=nkm[64:65, hh:hh + 1], scalar2=None, op0=ALU.mult), w=[qp], r=[qp, nkm])
                    def emit_pv(kt, pt, m):
                        for qs in range(4):
                            if m >= 0 and qs < m: continue
                            k.op("pe", lambda e, qs=qs, pt=pt, kt=kt: e.matmul(po[qs][:, 0:129], lhsT=pt[:, qs * 128:(qs + 1) * 128], rhs=v_res[:, kt, :],
                                                                         start=(kt == 0), stop=(kt == 4 * g + qs)), w=[po[qs]], r=[pt, v_res])
                    pend = None
                    for kt in range(4 * g + 4):
                        ps = pss[it % 2]; pt = pt_[it % 3]; it += 1
                        ks = slice(kt * 128, (kt + 1) * 128)
                        k.op("pe", lambda e, ps=ps, ks=ks, qn=qn: e.matmul(ps[:], lhsT=kn_res[:, ks], rhs=qn[:], start=True, stop=False), w=[ps], r=[kn_res, qn])
                        k.op("pe", lambda e, ps=ps, ks=ks, qp=qp: e.matmul(ps[:], lhsT=kp_res[0:65, ks], rhs=qp[0:65, :], start=False, stop=True), w=[ps], r=[kp_res, qp])
                        k.op("act", lambda e, ps=ps, pt=pt: e.activation(out=pt[:], in_=ps[:], func=AF.Exp, scale=ATT_SCALE), w=[pt], r=[ps])
                        m = kt - 4 * g
                        if m >= 0:
                            k.op("pool", lambda e, pt=pt, m=m: e.tensor_tensor(out=pt[:], in0=pt[:], in1=am[:, m, :], op=ALU.mult), w=[pt], r=[pt, am])
                        if pend is not None:
                            emit_pv(*pend)
                        pend = (kt, pt, m)
                    emit_pv(*pend)
                    for qs in range(4):
                        ob = osb[qs % 2]
                        k.op("dve", lambda e, qs=qs: e.reciprocal(out=rden[:, qs:qs + 1], in_=po[qs][:, 128:129]), w=[rden], r=[po[qs]])
                        k.op("dve", lambda e, qs=qs, ob=ob: e.tensor_scalar(out=ob[:], in0=po[qs][:, 0:128], scalar1=rden[:, qs:qs + 1], scalar2=None, op0=ALU.mult), w=[ob], r=[po[qs], rden])
                        k.out_dma("sp", yc[g * 512 + qs * 128:g * 512 + (qs + 1) * 128, hh, :], ob[:], r=[ob])
            barrier()
        k.finish()
    return nc


OFF = {}
def _mk_off():
    names = ["a_rec", "a_gate", "b_q", "b_k", "b_v", "b_z", "b_beta", "b_alpha", "c_q", "c_kv", "c_kr"]
    widths = [1024, 1024, 1536, 1536, 1536, 1536, 12, 12, 1024, 512, 64]
    s = 0
    for n, w in zip(names, widths):
        OFF[n] = s; s += w
_mk_off()


def core_heads(c):
    return c, (c, 8 + c % 4), (c, 8 + c % 4)


def mix_inputs(c, l, P, hT_bf16, consts):
    ha, hb, hc = core_heads(c)
    w_in = P["w_in"][l]
    def cols(name, h, w=128):
        o = OFF[name] + h * w
        return w_in[:, o:o + w]
    w_ab = np.concatenate([cols("a_rec", ha), cols("a_gate", ha)] + [cols(n, h) for h in hb for n in ("b_q", "b_k", "b_v")], axis=1)
    w_tm = np.concatenate([cols("b_z", hb[0]), cols("b_z", hb[1]), cols("b_beta", hb[0], 1), cols("b_beta", hb[1], 1),
                           cols("b_alpha", hb[0], 1), cols("b_alpha", hb[1], 1)], axis=1)
    w_c = w_in[:, OFF["c_q"]:OFF["c_q"] + 1024 + 512 + 64]
    sl = slice(ha * 128, (ha + 1) * 128)
    lru_p = np.stack([P["lru_conv_w"][l][j, sl] for j in range(4)] + [P["lru_conv_b"][l][sl], P["lru_ba"][l][sl], P["lru_bx"][l][sl], P["lru_lambda"][l][sl]], axis=1)
    lru_w = np.concatenate([P["lru_wa"][l][ha], P["lru_wx"][l][ha]], axis=1)
    dn_cw = np.stack([P["dn_conv_w"][l][j, i * 1536 + h * 128:i * 1536 + (h + 1) * 128] for h in hb for i in range(3) for j in range(4)], axis=1)
    dn_hp = np.broadcast_to(np.array([P["dn_a_log"][l][hb[0]], P["dn_a_log"][l][hb[1]], P["dn_dt_bias"][l][hb[0]], P["dn_dt_bias"][l][hb[1]]], np.float32)[None, :], (128, 4))
    dn_nw = np.broadcast_to(P["dn_norm_w"][l][None, :], (128, 128))
    wq = P["mla_w_q_up"][l][:, list(hc), :].reshape(8, 128, 2 * 192).transpose(1, 0, 2)
    wkv = P["mla_w_kv_up"][l][:, list(hc), :].reshape(4, 128, 2 * 256).transpose(1, 0, 2)
    d = {"hT": hT_bf16, "w_ab": w_ab, "w_c": w_c, "w_tm": w_tm, "lru_p": lru_p, "lru_w": lru_w, "dn_cw": dn_cw, "dn_hp": dn_hp, "dn_nw": dn_nw,
         "qnw": colvec(P["mla_q_norm_w"][l]), "kvnw": colvec(P["mla_kv_norm_w"][l]), "wq": wq, "wkv": wkv, "pos": P["positions"].reshape(1, -1).astype(np.int32)}
    d.update(consts)
    return {kk: np.ascontiguousarray(v) for kk, v in d.items()}


NEGINF = -3.0e38


def build_ffn(tc):
    nc = _new_nc()
    TT_ = 256
    def din(name, shape, dt=F32):
        return nc.dram_tensor(name, list(shape), dt, kind="ExternalInput").ap()
    xT = din("xT", [D, tc]); yT = din("yT", [D, tc])
    vec = din("vec", [128, 9, NCH])
    w_out = din("w_out", [D, D]); w_qry = din("w_qry", [D, 2048]); keysT = din("keysT", [128, 16, 128])
    UT = din("UT", [D, 16384]); V = din("V", [16384, D]); c_ident = din("ident", [128, 128])
    oT = nc.dram_tensor("oT", [D, tc], F32, kind="ExternalOutput").ap()
    s_x1 = nc.dram_tensor("s_x1", [D, tc], F32, kind="Internal").ap()
    s_ut = nc.dram_tensor("s_ut", [128, 128, D], BF16, kind="Internal").ap()
    s_wo = nc.dram_tensor("s_wo", [NCH, 128, D], BF16, kind="Internal").ap()
    s_vb = nc.dram_tensor("s_vb", [16384, D], BF16, kind="Internal").ap()
    xv = xT.rearrange("(c p) t -> p c t", p=128); yv = yT.rearrange("(c p) t -> p c t", p=128)
    ov = oT.rearrange("(c p) t -> p c t", p=128); x1v = s_x1.rearrange("(c p) t -> p c t", p=128)
    wov = w_out.rearrange("(c p) n -> p c n", p=128); wqv = w_qry.rearrange("(c p) n -> p c n", p=128)
    utv = UT.rearrange("(c p) e -> p c e", p=128)
    with contextlib.ExitStack() as st:
        k = KB(nc, st)
        def barrier():
            for e in k.eng:
                for key in k.sem:
                    if k.cnt[key] > 0:
                        k._wait(e, (key, k.cnt[key]))
        ones = k.sb([128, 128]); k.op("dve", lambda e: e.memset(ones[:], 1.0), w=[ones])
        ident = k.sb([128, 128]); k.dma("sp", ident[:], c_ident[:, :], w=[ident])
        vt = k.sb([128, 9, NCH]); k.dma("sp", vt[:], vec[:, :, :], w=[vt])
        kT = k.sb([128, 16, 128]); k.dma("sp", kT[:], keysT[:, :, :], w=[kT])
        wmod = k.sb([128, NCH])
        k.op("dve", lambda e: e.scalar_tensor_tensor(out=wmod[:], in0=vt[:, 4, :], scalar=1.0, in1=vt[:, 3, :], op0=ALU.add, op1=ALU.mult), w=[wmod], r=[vt])
        bna = TV(vt, vt.h[:, 0, :], "bna"); bnc = TV(vt, vt.h[:, 1, :], "bnc"); shf = TV(vt, vt.h[:, 5, :], "shf")
        h2b = k.sb([128, NCH, TT_], BF16)
        qT = k.sb([128, 16, TT_])
        with contextlib.ExitStack() as pc_:
            cv = [k.sb([128, D], BF16, stack=pc_, name="cv%d" % i) for i in range(6)]
            ci = 0
            for n in range(NCH):
                t_ = cv[ci % 6]; ci += 1
                k.dma("pool", t_.h[:].rearrange("p (c n) -> p c n", n=128), wov[:, :, n * 128:(n + 1) * 128], w=[t_])
                k.dma("sp", s_wo[n], t_[:], r=[t_])
            for et in range(128):
                t_ = cv[ci % 6]; ci += 1
                k.dma("pool", t_.h[:].rearrange("p (c n) -> p c n", n=128), utv[:, :, et * 128:(et + 1) * 128], w=[t_])
                k.dma("sp", s_ut[et], t_[:], r=[t_])
                t_ = cv[ci % 6]; ci += 1
                k.dma("pool", t_[:], V[et * 128:(et + 1) * 128, :], w=[t_])
                k.dma("act", s_vb[et * 128:(et + 1) * 128, :], t_[:], r=[t_])
            barrier()
        for it in range(tc // TT_):
            tsl = slice(it * TT_, (it + 1) * TT_)
            with contextlib.ExitStack() as pa:
                yt = k.sb([128, NCH, TT_], stack=pa); xt = k.sb([128, NCH, TT_], stack=pa); ynb = k.sb([128, NCH, TT_], BF16, stack=pa)
                wst = [k.sb([128, NCH, 128], BF16, stack=pa, name="fw%d" % i) for i in range(3)]
                wqs = [k.sb([128, NCH, 128], stack=pa, name="fq%d" % i) for i in range(2)]
                scratch = [k.sb([128, TT_], stack=pa, name="fsq%d" % i) for i in range(2)]
                rs = k.sb([128, TT_], stack=pa); ps_ssq = k.ps([128, TT_], stack=pa)
                big = [k.ps([128, TT_], stack=pa, name="fbig%d" % i) for i in range(3)]
                for half in range(2):
                    cs = slice(half * 16, half * 16 + 16)
                    k.dma("sp", yt[:, cs, :], yv[:, cs, tsl], w=[yt]); k.dma("sp", xt[:, cs, :], xv[:, cs, tsl], w=[xt])
                emit_norm_mod(k, ones, yt, 8, TT_, bna, None, lambda c: (ynb[:, c, :], ynb), ps_ssq, scratch, rs, 1024.0, c0=0)
                emit_norm_mod(k, ones, yt, 12, TT_, bnc, None, lambda c: (ynb[:, 20 + c, :], ynb), ps_ssq, scratch, rs, 1536.0, c0=20)
                k.op("pool", lambda e: e.tensor_copy(out=ynb[:, 8:20, :], in_=yt[:, 8:20, :]), w=[ynb], r=[yt])
                for n in range(NCH):
                    wt = wst[n % 3]
                    k.dma("sp", wt.h[:].rearrange("p c n -> p (c n)"), s_wo[n], w=[wt])
                    ps = big[n % 3]
                    for fc in range(NCH):
                        k.op("pe", lambda e, fc=fc, ps=ps, wt=wt: e.matmul(ps[:], lhsT=wt[:, fc, :], rhs=ynb[:, fc, :], start=(fc == 0), stop=(fc == NCH - 1)), w=[ps], r=[wt, ynb])
                    k.op("dve", lambda e, n=n, ps=ps: e.scalar_tensor_tensor(out=xt[:, n, :], in0=ps[:], scalar=vt[:, 2, n:n + 1], in1=xt[:, n, :], op0=ALU.mult, op1=ALU.add),
                         w=[xt], r=[ps, vt, xt])
                k.dma("sp", x1v[:, :, tsl], xt[:], r=[xt])
                emit_norm_mod(k, ones, xt, NCH, TT_, wmod, shf, lambda c: (yt[:, c, :], yt), ps_ssq, scratch, rs, float(D))
                k.op("pool", lambda e: e.tensor_copy(out=h2b[:], in_=yt[:]), w=[h2b], r=[yt])
                for j in range(16):
                    wq_ = wqs[j % 2]
                    k.dma("sp", wq_[:], wqv[:, :, j * 128:(j + 1) * 128], w=[wq_])
                    ps = big[j % 3]
                    for fc in range(NCH):
                        k.op("pe", lambda e, fc=fc, ps=ps, wq_=wq_: e.matmul(ps[:], lhsT=wq_[:, fc, :], rhs=yt[:, fc, :], start=(fc == 0), stop=(fc == NCH - 1)), w=[ps], r=[wq_, yt])
                    k.op("act", lambda e, j=j, ps=ps: e.activation(out=qT[:, j, :], in_=ps[:], func=AF.Copy), w=[qT], r=[ps])
                barrier()
            with contextlib.ExitStack() as pb:
                NS = TT_ // 128
                sc = [k.sb([128, 16, 128], stack=pb, name="sc%d" % i) for i in range(NS)]
                tops = [k.sb([128, 16, 16], stack=pb, name="tops%d" % i) for i in range(NS)]
                tmp128 = k.sb([128, 128], stack=pb); cand = k.sb([128, 16, 16], stack=pb); cand2 = k.sb([128, 256], stack=pb)
                best = k.sb([128, 16], stack=pb); e16 = k.sb([128, 16], stack=pb)
                thr = [k.sb([128, 8], stack=pb, name="thr%d" % i) for i in range(NS)]
                negm = [k.sb([128, 8], stack=pb, name="negm%d" % i) for i in range(NS)]
                rZ = [k.sb([128, 8], stack=pb, name="rZ%d" % i) for i in range(NS)]
                gacc2 = [[k.sb([128, 16, 128], stack=pb, name="gacc%d_%d" % (pp, i)) for i in range(NS)] for pp in range(2)]
                S_ = k.sb([128, 16, 128], stack=pb); E_ = k.sb([128, 16, 128], stack=pb); M_ = E_
                ust = [k.sb([128, NCH, 128], BF16, stack=pb, name="ust%d" % i) for i in range(2)]
                vg = [k.sb([128, D], BF16, stack=pb, name="vg%d" % i) for i in range(4)]
                actT = k.sb([128, TT_], stack=pb); coefT = [k.sb([128, TT_], BF16, stack=pb, name="coefT%d" % i) for i in range(4)]
                acc_out = k.sb([128, NCH, TT_], stack=pb)
                psm = [k.ps([128, 512], stack=pb, name="fpsm%d" % i) for i in range(2)]
                pu = [k.ps([128, TT_], stack=pb, name="fpu%d" % i) for i in range(2)]
                pg = k.ps([128, TT_], stack=pb, name="fpg"); pv = [k.ps([128, TT_], stack=pb, name="fpv%d" % i) for i in range(2)]
                for sb_ in range(NS):
                    ssl = slice(sb_ * 128, (sb_ + 1) * 128)
                    for hp in range(16):
                        ps = psm[hp % 2]
                        k.op("pe", lambda e, hp=hp, ps=ps: e.matmul(ps[:, 0:128], lhsT=qT[:, hp, ssl], rhs=kT[:, hp, :], start=True, stop=True), w=[ps], r=[qT, kT])
                        k.op("act", lambda e, hp=hp, ps=ps: e.activation(out=sc[sb_][:, hp, :], in_=ps[:, 0:128], func=AF.Copy), w=[sc[sb_]], r=[ps])
                        k.op("dve", lambda e, hp=hp: e.max(out=tops[sb_][:, hp, 0:8], in_=sc[sb_][:, hp, :]), w=[tops[sb_]], r=[sc[sb_]])
                        k.op("dve", lambda e, hp=hp: e.match_replace(out=tmp128[:], in_to_replace=tops[sb_][:, hp, 0:8], in_values=sc[sb_][:, hp, :], imm_value=NEGINF),
                             w=[tmp128], r=[tops[sb_], sc[sb_]])
                        k.op("dve", lambda e, hp=hp: e.max(out=tops[sb_][:, hp, 8:16], in_=tmp128[:]), w=[tops[sb_]], r=[tmp128])
                    for h in range(8):
                        for a in range(16):
                            k.op("dve", lambda e, a=a, h=h: e.tensor_scalar(out=cand[:, a, :], in0=tops[sb_][:, 2 * h + 1, :], scalar1=tops[sb_][:, 2 * h, a:a + 1], scalar2=None, op0=ALU.add),
                                 w=[cand], r=[tops[sb_]])
                        cf = cand.h[:].rearrange("p a b -> p (a b)")
                        k.op("dve", lambda e: e.max(out=best[:, 0:8], in_=cf), w=[best], r=[cand])
                        k.op("dve", lambda e: e.match_replace(out=cand2[:], in_to_replace=best[:, 0:8], in_values=cf, imm_value=NEGINF), w=[cand2], r=[best, cand])
                        k.op("dve", lambda e: e.max(out=best[:, 8:16], in_=cand2[:]), w=[best], r=[cand2])
                        k.op("dve", lambda e, h=h: e.tensor_copy(out=thr[sb_][:, h:h + 1], in_=best[:, 15:16]), w=[thr[sb_]], r=[best])
                        k.op("dve", lambda e, h=h: e.tensor_scalar(out=negm[sb_][:, h:h + 1], in0=best[:, 0:1], scalar1=-1.0, scalar2=None, op0=ALU.mult), w=[negm[sb_]], r=[best])
                        k.op("act", lambda e, h=h: e.activation(out=e16[:], in_=best[:], func=AF.Exp, bias=negm[sb_][:, h:h + 1], scale=1.0), w=[e16], r=[best, negm[sb_]])
                        k.op("dve", lambda e, h=h: e.reduce_sum(out=rZ[sb_][:, h:h + 1], in_=e16[:], axis=AX.X), w=[rZ[sb_]], r=[e16])
                        k.op("dve", lambda e, h=h: e.reciprocal(out=rZ[sb_][:, h:h + 1], in_=rZ[sb_][:, h:h + 1]), w=[rZ[sb_]], r=[rZ[sb_]])
                def gate_gen(blk, par):
                    for sb_ in range(NS):
                        ga_ = gacc2[par][sb_]
                        for h in range(8):
                            for a in range(16):
                                i1 = blk * 16 + a
                                k.op("dve" if a % 2 == 0 else "pool", lambda e, a=a, i1=i1, h=h, sb_=sb_: e.tensor_scalar(out=S_[:, a, :], in0=sc[sb_][:, 2 * h + 1, :], scalar1=sc[sb_][:, 2 * h, i1:i1 + 1],
                                                                                                                     scalar2=None, op0=ALU.add), w=[S_], r=[sc[sb_]])
                            k.op("act", lambda e, h=h, sb_=sb_: e.activation(out=E_[:], in_=S_[:], func=AF.Exp, bias=negm[sb_][:, h:h + 1], scale=1.0), w=[E_], r=[S_, negm[sb_]])
                            k.op("dve", lambda e, h=h, sb_=sb_: e.scalar_tensor_tensor(out=M_[:], in0=S_[:], scalar=thr[sb_][:, h:h + 1], in1=E_[:], op0=ALU.is_ge, op1=ALU.mult), w=[M_], r=[S_, thr[sb_], E_])
                            if h == 0:
                                k.op("dve", lambda e, h=h, sb_=sb_, ga_=ga_: e.tensor_scalar(out=ga_[:], in0=M_[:], scalar1=rZ[sb_][:, h:h + 1], scalar2=None, op0=ALU.mult), w=[ga_], r=[M_, rZ[sb_]])
                            else:
                                k.op("dve", lambda e, h=h, sb_=sb_, ga_=ga_: e.scalar_tensor_tensor(out=ga_[:], in0=M_[:], scalar=rZ[sb_][:, h:h + 1], in1=ga_[:], op0=ALU.mult, op1=ALU.add),
                                     w=[ga_], r=[M_, rZ[sb_], ga_])
                            yield
                gi = 0
                for _ in gate_gen(0, 0):
                    pass
                for blk in range(8):
                    gacc = gacc2[blk % 2]
                    nxt = gate_gen(blk + 1, (blk + 1) % 2) if blk < 7 else None
                    for grp in range(4):
                        for ei in range(4):
                            a = grp * 4 + ei; et = blk * 16 + a
                            us = ust[et % 2]
                            k.dma("sp", us.h[:].rearrange("p c n -> p (c n)"), s_ut[et], w=[us])
                            k.dma("sp", vg[ei][:], s_vb[et * 128:(et + 1) * 128, :], w=[vg[ei]])
                            p_u = pu[et % 2]
                            for fc in range(NCH):
                                k.op("pe", lambda e, fc=fc, p_u=p_u, us=us: e.matmul(p_u[:], lhsT=us[:, fc, :], rhs=h2b[:, fc, :], start=(fc == 0), stop=(fc == NCH - 1)), w=[p_u], r=[us, h2b])
                            k.op("act", lambda e, p_u=p_u: e.activation(out=actT[:], in_=p_u[:], func=AF.Gelu_apprx_tanh), w=[actT], r=[p_u])
                            for sb_ in range(NS):
                                k.op("pe", lambda e, sb_=sb_, a=a: e.matmul(pg[:, sb_ * 128:(sb_ + 1) * 128], lhsT=gacc[sb_][:, a, :], rhs=ident[:], start=True, stop=True), w=[pg], r=[gacc[sb_], ident])
                            k.op("dve", lambda e, ei=ei: e.tensor_tensor(out=coefT[ei][:], in0=pg[:], in1=actT[:], op=ALU.mult), w=[coefT[ei]], r=[pg, actT])
                        for n in range(NCH):
                            p_v = pv[n % 2]
                            for ei in range(4):
                                k.op("pe", lambda e, ei=ei, n=n, p_v=p_v: e.matmul(p_v[:], lhsT=vg[ei][:, n * 128:(n + 1) * 128], rhs=coefT[ei][:], start=(ei == 0), stop=(ei == 3)), w=[p_v], r=[vg[ei], coefT[ei]])
                            if gi == 0:
                                k.op("act", lambda e, n=n, p_v=p_v: e.activation(out=acc_out[:, n, :], in_=p_v[:], func=AF.Copy), w=[acc_out], r=[p_v])
                            else:
                                k.op("dve", lambda e, n=n, p_v=p_v: e.tensor_tensor(out=acc_out[:, n, :], in0=p_v[:], in1=acc_out[:, n, :], op=ALU.add), w=[acc_out], r=[p_v, acc_out])
                        gi += 1
                        if nxt is not None:
                            for _ in range(NS * 8 // 4):
                                next(nxt, None)
                x1t = S_
                x1tv = x1t.h[:].rearrange("p a b -> p (a b)")
                for q4 in range(4):
                    cs = slice(q4 * 8, q4 * 8 + 8)
                    k.dma("sp", x1tv.rearrange("p (c t) -> p c t", t=TT_), x1v[:, cs, tsl], w=[x1t])
                    for c in range(8):
                        n = q4 * 8 + c
                        k.op("dve", lambda e, n=n, c=c: e.scalar_tensor_tensor(out=acc_out[:, n, :], in0=acc_out[:, n, :], scalar=vt[:, 6, n:n + 1], in1=x1tv[:, c * TT_:(c + 1) * TT_],
                                                                         op0=ALU.mult, op1=ALU.add), w=[acc_out], r=[acc_out, vt, x1t])
                k.out_dma("sp", ov[:, :, tsl], acc_out[:], r=[acc_out])
                barrier()
        k.finish()
    return nc


SEQ = 16384
_PROG = {}


def _prog(key, fn):
    if key not in _PROG:
        _PROG[key] = fn()
    return _PROG[key]


def kernel(**inp):
    P = {k_: np.asarray(v) for k_, v in inp.items()}
    T = SEQ
    tc = T // NCORES
    x = P["x"][0]
    ncols = 6 * D // NCORES
    mlf = P["mod_layer"].reshape(2, -1)
    res = run(_prog("mod", lambda: build_mod(ncols)),
              [{"ccol": colvec(P["c"][0]), "wm": np.ascontiguousarray(P["mod_w"][:, j * ncols:(j + 1) * ncols]),
                "ml": np.ascontiguousarray(mlf[:, j * ncols:(j + 1) * ncols])} for j in range(NCORES)])
    m = np.concatenate([r["out"] for r in res], axis=1).reshape(2, 6, D)
    xT = np.ascontiguousarray(x.T)
    consts = mix_consts()
    ident = np.eye(128, dtype=np.float32)
    for l in range(2):
        sh_a, sc_a, g_a, sh_f, sc_f, g_f = [m[l, i] for i in range(6)]
        res = run(_prog("pre_b", lambda: build_pre(tc, BF16)),
                  [{"xT": np.ascontiguousarray(xT[:, c * tc:(c + 1) * tc]), "wn": colvec(P["norm_mix_w"][l]), "scc": colvec(sc_a), "shc": colvec(sh_a)}
                   for c in range(NCORES)])
        hT = np.concatenate([r["hT"] for r in res], axis=1)
        res = run(_prog("mix", lambda: build_mix(T)), [mix_inputs(c, l, P, hT, consts) for c in range(NCORES)])
        del hT
        yT = np.empty((D, T), np.float32)
        for c in range(NCORES):
            ha, hb, hc = core_heads(c)
            yT[ha * 128:(ha + 1) * 128] = res[c]["yaT"]
            for i, h in enumerate(hb):
                yT[1024 + h * 128:1024 + (h + 1) * 128] = res[c]["yb"][:, i, :].T
            for i, h in enumerate(hc):
                yT[2560 + h * 128:2560 + (h + 1) * 128] = res[c]["yc"][:, i, :].T
        del res
        vec = np.zeros((128, 9, NCH), np.float32)
        vec[:, 0, :8] = colvec(P["branch_norm_a"][l]); vec[:, 1, :12] = colvec(P["branch_norm_c"][l])
        for i, v in ((2, g_a), (3, P["norm_ffn_w"][l]), (4, sc_f), (5, sh_f), (6, g_f)):
            vec[:, i, :] = colvec(v)
        keysT = np.ascontiguousarray(P["peer_sub_keys"][l].reshape(16, 128, 128).transpose(2, 0, 1))
        UT = np.ascontiguousarray(P["peer_u"][l].T)
        shared = {"vec": vec, "w_out": P["w_out"][l], "w_qry": P["peer_w_query"][l].reshape(D, 2048), "keysT": keysT, "UT": UT,
                  "V": P["peer_v"][l], "ident": ident}
        res = run(_prog("ffn", lambda: build_ffn(tc)),
                  [dict(shared, xT=np.ascontiguousarray(xT[:, c * tc:(c + 1) * tc]), yT=np.ascontiguousarray(yT[:, c * tc:(c + 1) * tc])) for c in range(NCORES)])
        del UT, shared, yT
        xT = np.concatenate([r["oT"] for r in res], axis=1)
        del res
    zeros = np.zeros(D, np.float32)
    res = run(_prog("pre_f", lambda: build_pre(tc, F32)),
              [{"xT": np.ascontiguousarray(xT[:, c * tc:(c + 1) * tc]), "wn": colvec(P["final_norm_w"]), "scc": colvec(zeros), "shc": colvec(zeros)}
               for c in range(NCORES)])
    oT = np.concatenate([r["hT"] for r in res], axis=1)
    return np.ascontiguousarray(oT.T)[None].astype(np.float32)
```
